# Optimizing a Trainium2 kernel written in Bass

```python
import math
import jax, jax.numpy as jnp
from jax import lax
import numpy as np

D_MODEL = 1024
BATCH = 8
SEQ = 2048
DEPTH = 2

N_META = 16
DN_HEADS = 4
DN_DK = 128
DN_DV = 128
DN_KEY = DN_HEADS * DN_DK
DN_VAL = DN_HEADS * DN_DV
CONV_W = 4
CHUNK = 64
SA_HEADS = 8
SA_DH = 64
SA_W = SA_HEADS * SA_DH
IDX_HEADS = 8
IDX_DH = 64
K_MAX = 256
Q_BLOCK = 128
ROPE_THETA = 10000.0
MIX_W = DN_VAL + SA_W
N_GROUPS = 4
EXP_PER_GROUP = 8
N_EXPERTS = N_GROUPS * EXP_PER_GROUP
TOP_K_IN_GROUP = 2
D_EXPERT = 256
DEEPNORM_ALPHA = (2.0 * DEPTH) ** 0.25
DEEPNORM_BETA = (8.0 * DEPTH) ** -0.25

SPLIT_SIZES = (DN_KEY, DN_KEY, DN_VAL, DN_VAL, DN_HEADS, DN_HEADS,
               SA_W, SA_W, SA_W, IDX_HEADS * IDX_DH, IDX_DH, IDX_HEADS)
SPLIT_POINTS = tuple(int(s) for s in np.cumsum(SPLIT_SIZES)[:-1])
D_IN_PROJ = int(sum(SPLIT_SIZES))
VALUE_COLS = (2, 8)

kernel_name = 'hybrid_deltanet_dsa_hmoe'

F32 = jnp.float32


def layer_norm(x, g, b, eps=1e-5):
    xf = x.astype(F32)
    mu = jnp.mean(xf, -1, keepdims=True)
    var = jnp.mean(jnp.square(xf - mu), -1, keepdims=True)
    return ((xf - mu) * lax.rsqrt(var + eps) * g + b).astype(x.dtype)


def rope_tables(n_pos, dim):
    inv = 1.0 / (ROPE_THETA ** (jnp.arange(0, dim, 2, dtype=F32) / dim))
    ang = jnp.arange(n_pos, dtype=F32)[:, None] * inv[None, :]
    ang = jnp.concatenate([ang, ang], -1)
    return jnp.cos(ang), jnp.sin(ang)


def apply_rope(x, cos, sin):
    half = x.shape[-1] // 2
    rot = jnp.concatenate([-x[..., half:], x[..., :half]], -1)
    shape = (1, cos.shape[0]) + (1,) * (x.ndim - 3) + (cos.shape[-1],)
    return (x * cos.reshape(shape) + rot * sin.reshape(shape)).astype(x.dtype)


def causal_dwconv(x, w):
    c = x.shape[-1]
    return lax.conv_general_dilated(x, w[:, None, :].astype(x.dtype), window_strides=(1,),
                                    padding=[(CONV_W - 1, 0)],
                                    dimension_numbers=('NWC', 'WIO', 'NWC'),
                                    feature_group_count=c)


def l2norm(x, eps=1e-6):
    return x * lax.rsqrt(jnp.sum(x * x, -1, keepdims=True) + eps)


def chunk_gated_delta_rule(q, k, v, g, beta):
    bsz, seq, h, dk = q.shape
    dv = v.shape[-1]
    n = seq // CHUNK

    def chunks(t):
        t = t.reshape((bsz, n, CHUNK, h) + t.shape[3:])
        return jnp.moveaxis(t, 3, 2)

    q, k, v, g, beta = (chunks(t) for t in (q, k, v, g, beta))
    q = q * dk ** -0.5
    gc = jnp.cumsum(g, axis=-1)
    pos = jnp.arange(CHUNK)
    tril = pos[:, None] >= pos[None, :]
    strict = pos[:, None] > pos[None, :]
    decay = jnp.exp(jnp.where(tril, gc[..., :, None] - gc[..., None, :], -jnp.inf))
    kb = k * beta[..., None]
    lower = jnp.where(strict, jnp.einsum('bnhid,bnhjd->bnhij', kb, k) * decay, 0.0)
    eye = jnp.eye(CHUNK, dtype=q.dtype)
    tmat = lax.linalg.triangular_solve(eye + lower, jnp.broadcast_to(eye, lower.shape),
                                       left_side=True, lower=True, unit_diagonal=True)
    u = tmat @ (v * beta[..., None])
    w = tmat @ (kb * jnp.exp(gc)[..., None])
    q_dec = q * jnp.exp(gc)[..., None]
    a_qk = jnp.where(tril, jnp.einsum('bnhid,bnhjd->bnhij', q, k) * decay, 0.0)
    g_last = gc[..., -1]
    k_dec = k * jnp.exp(g_last[..., None] - gc)[..., None]

    def step(state, inp):
        u_n, w_n, qd_n, a_n, kd_n, gl_n = inp
        v_new = u_n - jnp.einsum('bhck,bhkv->bhcv', w_n, state)
        o_n = jnp.einsum('bhck,bhkv->bhcv', qd_n, state) + jnp.einsum('bhij,bhjv->bhiv', a_n, v_new)
        state = state * jnp.exp(gl_n)[..., None, None] + jnp.einsum('bhck,bhcv->bhkv', kd_n, v_new)
        return state, o_n

    xs = tuple(jnp.moveaxis(t, 1, 0) for t in (u, w, q_dec, a_qk, k_dec, g_last))
    s0 = jnp.zeros((bsz, h, dk, dv), q.dtype)
    _, o = lax.scan(step, s0, xs)
    o = jnp.moveaxis(o, 0, 1)
    return jnp.moveaxis(o, 3, 2).reshape(bsz, seq, h, dv)


def deltanet_group(q, k, v, z, b_logit, a_logit, conv_w, a_log, dt_bias, norm_g):
    bsz, seq, _ = q.shape
    qkv = jax.nn.silu(causal_dwconv(jnp.concatenate([q, k, v], -1), conv_w)).astype(F32)
    q, k, v = jnp.split(qkv, [DN_KEY, 2 * DN_KEY], axis=-1)
    q = l2norm(q.reshape(bsz, seq, DN_HEADS, DN_DK))
    k = l2norm(k.reshape(bsz, seq, DN_HEADS, DN_DK))
    v = v.reshape(bsz, seq, DN_HEADS, DN_DV)
    beta = jax.nn.sigmoid(b_logit.astype(F32))
    g = -jnp.exp(a_log.astype(F32)) * jax.nn.softplus(a_logit.astype(F32) + dt_bias.astype(F32))
    pad = (-N_META) % CHUNK
    padf = lambda t: jnp.pad(t, ((0, 0), (pad, 0)) + ((0, 0),) * (t.ndim - 2))
    o = chunk_gated_delta_rule(padf(q), padf(k), padf(v), padf(g), padf(beta))[:, pad:]
    o = o * lax.rsqrt(jnp.mean(o * o, -1, keepdims=True) + 1e-6) * norm_g
    o = o.reshape(bsz, seq, DN_VAL) * jax.nn.silu(z.astype(F32))
    return o.astype(z.dtype)


def dsa_group(q, k, v, qi, ki, wi, cos, sin, cos_i, sin_i, k_top):
    bsz, seq, _ = q.shape
    q = apply_rope(q.reshape(bsz, seq, SA_HEADS, SA_DH), cos, sin)
    k = apply_rope(k.reshape(bsz, seq, SA_HEADS, SA_DH), cos, sin)
    v = v.reshape(bsz, seq, SA_HEADS, SA_DH)
    qi = apply_rope(qi.reshape(bsz, seq, IDX_HEADS, IDX_DH), cos_i, sin_i)
    ki = apply_rope(ki, cos_i, sin_i)
    wi = wi.astype(F32) * IDX_HEADS ** -0.5
    n_blk = -(-seq // Q_BLOCK)
    lq = n_blk * Q_BLOCK

    def blocks(t):
        t = jnp.pad(t, ((0, 0), (0, lq - seq)) + ((0, 0),) * (t.ndim - 2))
        return jnp.moveaxis(t.reshape((bsz, n_blk, Q_BLOCK) + t.shape[2:]), 1, 0)

    key_pos = jnp.arange(seq)
    q_pos = jnp.arange(lq).reshape(n_blk, Q_BLOCK)
    gather = jax.vmap(lambda arr, idx: arr[idx])

    def attend(args):
        qb, qib, wib, pos = args
        causal = key_pos[None, :] <= pos[:, None]
        rel = jax.nn.relu(jnp.einsum('bqhd,bsd->bqhs', qib, ki, preferred_element_type=F32) * IDX_DH ** -0.5)
        score = jnp.einsum('bqhs,bqh->bqs', rel, wib)
        score = jnp.where(causal[None], score, -jnp.inf)
        _, sel = lax.top_k(score, k_top)
        k_sel = gather(k, sel)
        v_sel = gather(v, sel)
        valid = sel <= pos[None, :, None]
        logits = jnp.einsum('bqhd,bqkhd->bhqk', qb, k_sel, preferred_element_type=F32) * SA_DH ** -0.5
        logits = jnp.where(valid[:, None], logits, -jnp.inf)
        p = jax.nn.softmax(logits, axis=-1)
        return jnp.einsum('bhqk,bqkhd->bqhd', p.astype(v.dtype), v_sel)

    out = lax.map(attend, (blocks(q), blocks(qi), blocks(wi), q_pos))
    return jnp.moveaxis(out, 0, 1).reshape(bsz, lq, SA_W)[:, :seq]


def hier_moe(h, w_grp, b_grp, w_rtr, b_rtr, w1, w3, w2):
    bsz, seq, d = h.shape
    t = h.reshape(-1, d)
    grp_prob = jax.nn.softmax((t @ w_grp).astype(F32) + b_grp, axis=-1)
    g_prob, g_sel = lax.top_k(grp_prob, 1)
    exp_logits = ((t @ w_rtr).astype(F32) + b_rtr).reshape(-1, N_GROUPS, EXP_PER_GROUP)
    in_grp = jnp.take_along_axis(exp_logits, g_sel[:, :, None], axis=1)[:, 0]
    e_prob, e_sel = lax.top_k(jax.nn.softmax(in_grp, axis=-1), TOP_K_IN_GROUP)
    gate = g_prob * e_prob / jnp.sum(e_prob, -1, keepdims=True)
    expert_id = g_sel * EXP_PER_GROUP + e_sel
    combine = jnp.sum(jax.nn.one_hot(expert_id, N_EXPERTS, dtype=F32) * gate[..., None], axis=1)
    hid = jax.nn.silu(jnp.einsum('td,edf->tef', t, w1)) * jnp.einsum('td,edf->tef', t, w3)
    y = jnp.einsum('tef,efd->td', hid * combine[..., None].astype(hid.dtype), w2)
    return y.reshape(bsz, seq, d)


def setup_inputs(seed: int = 0) -> dict:
    key = jax.random.key(seed)
    ks = jax.random.split(key, 20)
    nrm = lambda k, shape, s: jax.random.normal(k, shape, F32) * s
    d_sc = D_MODEL ** -0.5
    col_scale = np.concatenate([np.full((s,), DEEPNORM_BETA if i in VALUE_COLS else 1.0, np.float32)
                                for i, s in enumerate(SPLIT_SIZES)])
    dt = jnp.exp(jax.random.uniform(ks[5], (DEPTH, DN_HEADS), F32, math.log(1e-3), math.log(1e-1)))
    return {
        'x': nrm(ks[0], (BATCH, SEQ, D_MODEL), 1.0),
        'meta_tokens': nrm(ks[1], (N_META, D_MODEL), 1.0),
        'w_in': nrm(ks[2], (DEPTH, D_MODEL, D_IN_PROJ), d_sc) * jnp.asarray(col_scale),
        'conv_w': nrm(ks[3], (DEPTH, CONV_W, 2 * DN_KEY + DN_VAL), CONV_W ** -0.5),
        'a_log': jnp.log(jax.random.uniform(ks[4], (DEPTH, DN_HEADS), F32, 1.0, 16.0)),
        'dt_bias': dt + jnp.log(-jnp.expm1(-dt)),
        'dn_norm_g': 1.0 + nrm(ks[6], (DEPTH, DN_DV), 0.02),
        'w_out': nrm(ks[7], (DEPTH, MIX_W, D_MODEL), MIX_W ** -0.5 * DEEPNORM_BETA),
        'ln1_g': 1.0 + nrm(ks[8], (DEPTH, D_MODEL), 0.02),
        'ln1_b': nrm(ks[9], (DEPTH, D_MODEL), 0.02),
        'w_grp': nrm(ks[10], (DEPTH, D_MODEL, N_GROUPS), d_sc),
        'b_grp': nrm(ks[11], (DEPTH, N_GROUPS), 0.01),
        'w_rtr': nrm(ks[12], (DEPTH, D_MODEL, N_EXPERTS), d_sc),
        'b_rtr': nrm(ks[13], (DEPTH, N_EXPERTS), 0.01),
        'w1': nrm(ks[14], (DEPTH, N_EXPERTS, D_MODEL, D_EXPERT), d_sc),
        'w3': nrm(ks[15], (DEPTH, N_EXPERTS, D_MODEL, D_EXPERT), d_sc),
        'w2': nrm(ks[16], (DEPTH, N_EXPERTS, D_EXPERT, D_MODEL), D_EXPERT ** -0.5 * DEEPNORM_BETA),
        'ln2_g': 1.0 + nrm(ks[17], (DEPTH, D_MODEL), 0.02),
        'ln2_b': nrm(ks[18], (DEPTH, D_MODEL), 0.02),
    }


def reference(x, meta_tokens, w_in, conv_w, a_log, dt_bias, dn_norm_g, w_out, ln1_g, ln1_b,
              w_grp, b_grp, w_rtr, b_rtr, w1, w3, w2, ln2_g, ln2_b):
    bsz, seq, _ = x.shape
    total = seq + N_META
    k_top = min(K_MAX, seq // 4)
    meta = jnp.broadcast_to(meta_tokens[None].astype(x.dtype), (bsz, N_META, D_MODEL))
    h = jnp.concatenate([meta, x], axis=1)
    cos, sin = rope_tables(total, SA_DH)
    cos_i, sin_i = rope_tables(total, IDX_DH)
    for l in range(DEPTH):
        proj = h @ w_in[l]
        dq, dk, dv, dz, db, da, sq, sk, sv, iq, ik, iw = jnp.split(proj, SPLIT_POINTS, axis=-1)
        o_dn = deltanet_group(dq, dk, dv, dz, db, da, conv_w[l], a_log[l], dt_bias[l], dn_norm_g[l])
        o_sa = dsa_group(sq, sk, sv, iq, ik, iw, cos, sin, cos_i, sin_i, k_top)
        mix = jnp.concatenate([o_dn, o_sa.astype(o_dn.dtype)], axis=-1) @ w_out[l]
        h = layer_norm(DEEPNORM_ALPHA * h + mix, ln1_g[l], ln1_b[l])
        ffn = hier_moe(h, w_grp[l], b_grp[l], w_rtr[l], b_rtr[l], w1[l], w3[l], w2[l])
        h = layer_norm(DEEPNORM_ALPHA * h + ffn, ln2_g[l], ln2_b[l])
    return h[:, N_META:]
```

```python
import numpy as np
from contextlib import ExitStack
import concourse.bass as bass
import concourse.mybir as mybir
from concourse.bass_utils import run_bass_kernel_spmd

F32 = mybir.dt.float32
BF16 = mybir.dt.bfloat16
AF = mybir.ActivationFunctionType
ALU = mybir.AluOpType

ENGS = ("tensor", "vector", "scalar", "gpsimd", "sync")
N_DMA_SEMS = 12

D = 1024
SEQ = 2048
NMETA = 16
L = SEQ + NMETA
NT = 17
LP = NT * 128
DEPTH = 2
DIN = 4176
ALPHA = (2.0 * DEPTH) ** 0.25
NEG = -30000.0
KTOP = 256
NIT = 20
NE = 32
DN_CUT = 0
PREP_CUT = 0
NTILES = [(0, 512), (512, 512), (1024, 512), (1536, 512), (2048, 128)]


class Prog:
    def __init__(self, nc, stack):
        self.nc = nc
        self.streams = {e: [] for e in ENGS}
        self.esem = {e: stack.enter_context(nc.semaphore("s_" + e)) for e in ENGS}
        self.eseq = {e: 0 for e in ENGS}
        self.eval_ = {e: 0 for e in ENGS}
        self.dsem = {e: [stack.enter_context(nc.semaphore("d_%s%d" % (e, i)))
                         for i in range(N_DMA_SEMS)] for e in ("sync", "gpsimd", "scalar")}
        self.dcnt = {e: [0] * N_DMA_SEMS for e in self.dsem}
        self.drr = {e: 0 for e in self.dsem}
        self.waited = {e: {} for e in ENGS}
        self.last_w = {}
        self.readers = {}
        self.n_ops = 0
        self.excl = set()

    @staticmethod
    def _sk(src):
        return src if isinstance(src, str) else id(src)

    def _need(self, eng, ev, out):
        if ev is None:
            return
        src, val = ev
        if eng == "tensor" and src == "tensor":
            return
        k = self._sk(src)
        if self.waited[eng].get(k, 0) >= val:
            return
        self.waited[eng][k] = val
        out.append((src, val))

    def _deps(self, eng, reads, writes):
        waits = []
        for k in reads:
            self._need(eng, self.last_w.get(k), waits)
        for k in writes:
            self._need(eng, self.last_w.get(k), waits)
            for ev in self.readers.get(k, {}).values():
                self._need(eng, ev, waits)
        return waits

    def _commit(self, ev, reads, writes):
        for k in reads:
            self.readers.setdefault(k, {})[self._sk(ev[0])] = ev
        for k in writes:
            self.last_w[k] = ev
            self.readers[k] = {}

    def op(self, eng, meth, reads, writes, *args, **kw):
        if eng != "tensor":
            ex = [k for k in reads if isinstance(k, str) and k in self.excl and k not in writes]
            if ex:
                writes = list(writes) + ex
        waits = self._deps(eng, reads, writes)
        self.eseq[eng] += 1
        ev = (eng, self.eseq[eng])
        self.streams[eng].append((waits, meth, args, kw, "E", ev[1]))
        self._commit(ev, reads, writes)
        self.n_ops += 1

    def dma(self, q, reads, writes, out, in_, **kw):
        i = self.drr[q]
        self.drr[q] = (i + 1) % N_DMA_SEMS
        sem = self.dsem[q][i]
        waits = self._deps(q, reads, writes)
        if self.dcnt[q][i] > 0:
            self._need(q, (sem, 16 * self.dcnt[q][i]), waits)
        self.dcnt[q][i] += 1
        ev = (sem, 16 * self.dcnt[q][i])
        kw = dict(kw)
        kw["out"] = out
        kw["in_"] = in_
        self.streams[q].append((waits, "dma_start", (), kw, "D", sem))
        self._commit(ev, reads, writes)
        self.n_ops += 1

    def finish(self, final_keys):
        waits = []
        for k in final_keys:
            self._need("sync", self.last_w.get(k), waits)
        self.streams["sync"].append((waits, None, (), {}, None, None))
        self.flush()

    def barrier(self):
        evs = [(e, self.eseq[e]) for e in ENGS if self.eseq[e] > 0]
        for q in self.dsem:
            for i in range(N_DMA_SEMS):
                if self.dcnt[q][i] > 0:
                    evs.append((self.dsem[q][i], 16 * self.dcnt[q][i]))
        for e in ENGS:
            waits = []
            for ev in evs:
                self._need(e, ev, waits)
            if waits:
                self.streams[e].append((waits, None, (), {}, None, None))

    def flush(self):
        self.barrier()
        nc = self.nc
        streams = self.streams
        self.streams = {e: [] for e in ENGS}
        targets = {e: set() for e in ENGS}
        for e in ENGS:
            for waits, meth, args, kw, kind, x in streams[e]:
                for (src, val) in waits:
                    if isinstance(src, str):
                        targets[src].add(val)
        value_of = {}
        for e in ENGS:
            for waits, meth, args, kw, kind, x in streams[e]:
                if kind == "E" and x in targets[e]:
                    self.eval_[e] += 1
                    value_of[(e, x)] = self.eval_[e]
        for e in ENGS:
            for t in targets[e]:
                assert (e, t) in value_of, ("wait target from an earlier flush", e, t)
        esem = self.esem
        with nc.Block() as block:
            def run(engname):
                def body(eng):
                    for waits, meth, args, kw, kind, x in streams[engname]:
                        for (src, val) in waits:
                            if isinstance(src, str):
                                eng.wait_ge(esem[src], value_of[(src, val)])
                            else:
                                eng.wait_ge(src, val)
                        if meth is not None:
                            ins = getattr(eng, meth)(*args, **kw)
                            if kind == "D":
                                ins.then_inc(x, 16)
                            elif (engname, x) in value_of:
                                ins.then_inc(esem[engname], 1)
                return body
            block.tensor(run("tensor"))
            block.vector(run("vector"))
            block.scalar(run("scalar"))
            block.gpsimd(run("gpsimd"))
            block.sync(run("sync"))


class Ring:
    def __init__(self, alloc, name, shape, dt, n):
        self.bufs = [alloc(name + str(i), shape, dt) for i in range(n)]
        self.keys = [name + str(i) for i in range(n)]
        self.i = 0

    def next(self):
        i = self.i
        self.i = (i + 1) % len(self.bufs)
        return self.bufs[i], self.keys[i]


def interleave(gens):
    gens = list(gens)
    while gens:
        for g in list(gens):
            try:
                next(g)
            except StopIteration:
                gens.remove(g)


def build(debug=False, stop_after=None, n_layers=DEPTH, only=None):
    nc = bass.Bass("TRN2", target_bir_lowering=False)
    dk = "ExternalOutput" if debug else "Internal"

    def din(name, shape, dt=F32):
        return nc.dram_tensor(name, list(shape), dt, kind="ExternalInput").ap()

    def dscr(name, shape, dt=F32):
        return nc.dram_tensor(name, list(shape), dt, kind=dk).ap()

    h0 = din("h0", [LP, D])
    w_in = din("w_in", [DEPTH, D, DIN])
    w_out = din("w_out", [DEPTH, D, D])
    w1 = din("w1", [DEPTH, NE, D, 256])
    w3 = din("w3", [DEPTH, NE, D, 256])
    w2 = din("w2", [DEPTH, NE, 256, D])
    wr = din("wr", [DEPTH, D, 36])
    brep = din("brep", [DEPTH, 128, 36])
    cwT = din("cwT", [DEPTH, 128, 48])
    alog = din("alog", [DEPTH, 128, 68])
    dtb = din("dtb", [DEPTH, 128, 68])
    ngr = din("ngr", [DEPTH, 128, 512])
    ln1g = din("ln1g", [DEPTH, 128, D])
    ln1b = din("ln1b", [DEPTH, 128, D])
    ln2g = din("ln2g", [DEPTH, 128, D])
    ln2b = din("ln2b", [DEPTH, 128, D])
    ropec = din("ropec", [128, LP])
    ropes = din("ropes", [128, LP])
    y = nc.dram_tensor("y", [SEQ, D], F32, kind="ExternalOutput").ap()

    h_d = dscr("h_d", [LP, D])
    qkvT_d = dscr("qkvT_d", [12, 128, LP])
    ropeT_d = dscr("ropeT_d", [13, 128, LP], BF16)
    z_d = dscr("z_d", [LP, 512])
    sv_d = dscr("sv_d", [LP, 512], BF16)
    sm_d = dscr("sm_d", [LP, 16])
    mixT_d = dscr("mixT_d", [8, 128, LP], BF16)

    top = ExitStack()
    P = Prog(nc, top)

    uid = [0]

    def uname(name):
        uid[0] += 1
        return "%s_u%d" % (name, uid[0])

    def stage_allocs(st):
        def sb(name, shape, dt=F32):
            return st.enter_context(nc.sbuf_tensor(uname(name), list(shape), dt))

        def ps(name, shape=(128, 512), dt=F32):
            P.excl.add(name)
            return st.enter_context(nc.psum_tensor(uname(name), list(shape), dt))
        return sb, ps

    csb, _ = stage_allocs(top)
    ident = csb("ident", [128, 128])
    identb = csb("identb", [128, 128], BF16)
    ones = csb("ones", [128, 128])
    negones = csb("negones", [128, 128])
    P.op("gpsimd", "memset", [], ["ident"], ident[:], 1.0)
    P.op("gpsimd", "affine_select", ["ident"], ["ident"], out=ident[:], in_=ident[:], pattern=[[-1, 128]],
         compare_op=ALU.is_equal, fill=0.0, base=0, channel_multiplier=1)
    P.op("gpsimd", "tensor_copy", ["ident"], ["identb"], out=identb[:], in_=ident[:])
    P.op("gpsimd", "memset", [], ["ones"], ones[:], 1.0)
    P.op("gpsimd", "memset", [], ["negones"], negones[:], -1.0)

    def ln_tile(sb_t, tkey, g_rep, b_rep, outt, okey, scr, skey, st2, st2key):
        P.op("scalar", "activation", [tkey], [skey, st2key], out=scr[:], in_=sb_t[:], func=AF.Identity,
             accum_out=st2[:, 0:1])
        yield
        P.op("vector", "tensor_scalar", [st2key], [st2key], out=st2[:, 1:2], in0=st2[:, 0:1], scalar1=-1.0 / D,
             scalar2=None, op0=ALU.mult)
        yield
        P.op("scalar", "activation", [tkey, st2key], [skey, st2key], out=scr[:], in_=sb_t[:], func=AF.Square,
             bias=st2[:, 1:2], scale=1.0, accum_out=st2[:, 2:3])
        yield
        P.op("vector", "tensor_scalar", [st2key], [st2key], out=st2[:, 3:4], in0=st2[:, 2:3], scalar1=1.0 / D,
             scalar2=1e-5, op0=ALU.mult, op1=ALU.add)
        yield
        P.op("scalar", "activation", [st2key], [st2key], out=st2[:, 3:4], in_=st2[:, 3:4], func=AF.Sqrt)
        yield
        P.op("vector", "reciprocal", [st2key], [st2key], out=st2[:, 3:4], in_=st2[:, 3:4])
        yield
        P.op("vector", "tensor_scalar", [tkey, st2key], [okey], out=outt[:], in0=sb_t[:], scalar1=st2[:, 1:2],
             scalar2=st2[:, 3:4], op0=ALU.add, op1=ALU.mult)
        yield
        P.op("vector", "tensor_tensor", [okey, "lng"], [okey], out=outt[:], in0=outt[:], in1=g_rep[:], op=ALU.mult)
        yield
        P.op("vector", "tensor_tensor", [okey, "lnb"], [okey], out=outt[:], in0=outt[:], in1=b_rep[:], op=ALU.add)
        yield

    def stage_hT(src, hT, l_unused=None):
        with ExitStack() as st:
            sb, ps = stage_allocs(st)
            ht = Ring(sb, "ht_in", [128, D], F32, 2)
            pT = [ps("hT_ps%d" % i, [128, 1024]) for i in range(2)]
            for t in range(NT):
                a, ak = ht.next()
                P.dma("sync", [("h", t)], [ak], out=a[:], in_=src[t * 128:(t + 1) * 128, :])
                pt, pk = pT[t % 2], "hT_ps%d" % (t % 2)
                for c in range(8):
                    P.op("tensor", "transpose", [ak, "ident"], [pk], out=pt[:, c * 128:(c + 1) * 128],
                         in_=a[:, c * 128:(c + 1) * 128], identity=ident[:])
                eng = "vector" if t % 2 == 0 else "scalar"
                if eng == "vector":
                    P.op("vector", "tensor_copy", [pk], [("hT", t)], out=hT[:, :, t * 128:(t + 1) * 128],
                         in_=pt[:].rearrange("p (c t) -> p c t", c=8))
                else:
                    P.op("scalar", "copy", [pk], [("hT", t)], out=hT[:, :, t * 128:(t + 1) * 128],
                         in_=pt[:].rearrange("p (c t) -> p c t", c=8))
            P.flush()

    def stage_proj(l, hT):
        with ExitStack() as st:
            sb, ps = stage_allocs(st)
            NC_FM = 38 * 128
            W = sb("Wp", [128, 8, NC_FM + 1040], BF16)
            cosT = sb("cosT", [128, LP])
            sinT = sb("sinT", [128, LP])
            P.dma("sync", [], ["cosT"], out=cosT[:], in_=ropec[:, :])
            P.dma("sync", [], ["sinT"], out=sinT[:], in_=ropes[:, :])
            wl = w_in[l].rearrange("(c p) n -> p c n", p=128)

            wstg = Ring(sb, "pj_wst", [128, 8, 256], F32, 3)

            def wk(c0, n):
                return [("Wc", j) for j in range(c0 // 128, (c0 + n + 127) // 128)]

            def ld(dst0, src0, n, key):
                for o in range(0, n, 256):
                    nn_ = min(256, n - o)
                    sg, sgk = wstg.next()
                    P.dma("sync", [], [sgk], out=sg[:, :, 0:nn_], in_=wl[:, :, src0 + o:src0 + o + nn_])
                    P.op("gpsimd", "tensor_copy", [sgk], wk(dst0 + o, nn_), out=W[:, :, dst0 + o:dst0 + o + nn_], in_=sg[:, :, 0:nn_])

            def ld_perm(dst0, src0, nheads, key):
                for o in range(0, nheads, 4):
                    nh_ = min(4, nheads - o)
                    nn_ = nh_ * 64
                    sg, sgk = wstg.next()
                    P.dma("sync", [], [sgk], out=sg[:, :, 0:nn_], in_=wl[:, :, src0 + o * 64:src0 + o * 64 + nn_])
                    dv = W[:, :, dst0 + o * 64:dst0 + o * 64 + nn_].rearrange("p c (h two j) -> p c h two j", two=2, j=32)
                    sv = sg[:, :, 0:nn_].rearrange("p c (h two j) -> p c h two j", two=2, j=32)
                    for half in range(2):
                        P.op("gpsimd", "tensor_copy", [sgk], wk(dst0 + o * 64, nn_), out=dv[:, :, :, half, :], in_=sv[:, :, :, 1 - half, :])
            ld(0, 0, 1536, ("W", 0))
            ld(12 * 128, 2056, 512, ("W", 1))
            ld_perm(16 * 128, 2056, 8, ("W", 1))
            ld(20 * 128, 2568, 512, ("W", 2))
            ld_perm(24 * 128, 2568, 8, ("W", 2))
            ld(28 * 128, 3592, 512, ("W", 3))
            ld_perm(32 * 128, 3592, 8, ("W", 3))
            ld(36 * 128, 4104, 64, ("W", 4))
            ld(36 * 128 + 64, 4104, 64, ("W", 4))
            ld_perm(37 * 128, 4104, 1, ("W", 4))
            ld_perm(37 * 128 + 64, 4104, 1, ("W", 4))
            T0 = NC_FM
            ld(T0, 1536, 512, ("W", 5))
            ld(T0 + 512, 3080, 512, ("W", 5))
            ld(T0 + 1024, 2048, 8, ("W", 5))
            ld(T0 + 1032, 4168, 8, ("W", 5))
            wkeys = [("W", i) for i in range(6)]

            pbank = Ring(ps, "pj_ps", [128, 512], F32, 4)
            stg32 = Ring(sb, "pj_s32", [128, LP], F32, 2)
            stg16 = Ring(sb, "pj_s16", [128, LP], BF16, 2)
            tmp = Ring(sb, "pj_tmp", [128, 512], F32, 2)

            def wgrp(m):
                return [("Wc", m)]

            def mm_fm(m, n0, nn, pt, pk):
                for k in range(8):
                    P.op("tensor", "matmul", wgrp(m) + [("hT", i) for i in range(n0 // 128, (n0 + nn) // 128)], [pk],
                         pt[:, 0:nn], lhsT=W[:, k, m * 128:(m + 1) * 128], rhs=hT[:, k, n0:n0 + nn],
                         start=(k == 0), stop=(k == 7))
            for m in range(12):
                s, sk = stg32.next()
                for (n0, nn) in NTILES:
                    pt, pk = pbank.next()
                    mm_fm(m, n0, nn, pt, pk)
                    if (n0 // 512) % 2 == 0:
                        P.op("vector", "tensor_copy", [pk], [sk], out=s[:, n0:n0 + nn], in_=pt[:, 0:nn])
                    else:
                        P.op("scalar", "copy", [pk], [sk], out=s[:, n0:n0 + nn], in_=pt[:, 0:nn])
                P.dma("sync", [sk], [("qkvT", m)], out=qkvT_d[m, :, :], in_=s[:])
            rope_src = [12, 13, 14, 15, 20, 21, 22, 23, 28, 29, 30, 31, 36]
            rope_prm = [16, 17, 18, 19, 24, 25, 26, 27, 32, 33, 34, 35, 37]
            for r in range(13):
                s, sk = stg16.next()
                for (n0, nn) in NTILES:
                    pa, pak = pbank.next()
                    mm_fm(rope_src[r], n0, nn, pa, pak)
                    pb, pbk = pbank.next()
                    mm_fm(rope_prm[r], n0, nn, pb, pbk)
                    t1, t1k = tmp.next()
                    t2, t2k = tmp.next()
                    P.op("vector", "tensor_tensor", [pak, "cosT"], [t1k], out=t1[:, 0:nn], in0=pa[:, 0:nn],
                         in1=cosT[:, n0:n0 + nn], op=ALU.mult)
                    P.op("vector", "tensor_tensor", [pbk, "sinT"], [t2k], out=t2[:, 0:nn], in0=pb[:, 0:nn],
                         in1=sinT[:, n0:n0 + nn], op=ALU.mult)
                    P.op("gpsimd", "tensor_tensor", [t1k, t2k], [sk], out=s[:, n0:n0 + nn], in0=t1[:, 0:nn],
                         in1=t2[:, 0:nn], op=ALU.add)
                P.dma("sync", [sk], [("ropeT", r)], out=ropeT_d[r, :, :], in_=s[:])
            zst = Ring(sb, "pj_z", [128, 512], F32, 2)
            svst = Ring(sb, "pj_sv", [128, 512], BF16, 2)
            smst = Ring(sb, "pj_sm", [128, 16], F32, 2)
            for t in range(NT):
                for (c0, cn, kind) in ((T0, 512, "z"), (T0 + 512, 512, "sv"), (T0 + 1024, 16, "sm")):
                    pt, pk = pbank.next()
                    for k in range(8):
                        P.op("tensor", "matmul", wk(c0, cn) + [("hT", t)], [pk], pt[:, 0:cn],
                             lhsT=hT[:, k, t * 128:(t + 1) * 128], rhs=W[:, k, c0:c0 + cn], start=(k == 0), stop=(k == 7))
                    if kind == "z":
                        s, sk = zst.next()
                        P.op("scalar", "copy", [pk], [sk], out=s[:], in_=pt[:, 0:512])
                        P.dma("sync", [sk], [("z", t)], out=z_d[t * 128:(t + 1) * 128, :], in_=s[:])
                    elif kind == "sv":
                        s, sk = svst.next()
                        P.op("vector", "tensor_copy", [pk], [sk], out=s[:], in_=pt[:, 0:512])
                        P.dma("sync", [sk], [("sv", t)], out=sv_d[t * 128:(t + 1) * 128, :], in_=s[:])
                    else:
                        s, sk = smst.next()
                        P.op("vector", "tensor_copy", [pk], [sk], out=s[:], in_=pt[:, 0:16])
                        P.dma("sync", [sk], [("sm", t)], out=sm_d[t * 128:(t + 1) * 128, :], in_=s[:])
            P.flush()

    def stage_dn(l):
        with ExitStack() as st:
            sb, ps = stage_allocs(st)
            qT = sb("dn_qT", [128, 4, LP], BF16)
            kT = sb("dn_kT", [128, 4, LP], BF16)
            vT = sb("dn_vT", [128, 4, LP], BF16)
            cw = sb("dn_cw", [128, 48])
            P.dma("sync", [], ["cw"], out=cw[:], in_=cwT[l, :, :])
            with ExitStack() as st1:
                sb1, ps1 = stage_allocs(st1)
                xin = Ring(sb1, "dn_xin", [128, LP + 3], F32, 2)
                acc = Ring(sb1, "dn_acc", [128, LP], F32, 2)
                sq = Ring(sb1, "dn_sq", [128, LP], F32, 2)
                rn = Ring(sb1, "dn_rn", [128, 512], F32, 2)
                pss = Ring(ps1, "dn_ps", [128, 512], F32, 2)
                for m in range(12):
                    x, xk = xin.next()
                    P.op("gpsimd", "memset", [], [xk], x[:, 0:3], 0.0)
                    P.dma("sync", [("qkvT", m)], [xk], out=x[:, 3:LP + 3], in_=qkvT_d[m, :, :])
                    a, ak = acc.next()
                    P.op("vector", "tensor_scalar", [xk, "cw"], [ak], out=a[:], in0=x[:, 3:LP + 3],
                         scalar1=cw[:, m * 4 + 3:m * 4 + 4], scalar2=None, op0=ALU.mult)
                    for j in range(3):
                        P.op("vector", "scalar_tensor_tensor", [xk, "cw", ak], [ak], out=a[:],
                             in0=x[:, j:LP + j], scalar=cw[:, m * 4 + j:m * 4 + j + 1], in1=a[:], op0=ALU.mult, op1=ALU.add)
                    if m >= 8:
                        P.op("scalar", "activation", [ak], [("vT", m - 8)], out=vT[:, m - 8, :], in_=a[:], func=AF.Silu)
                        continue
                    P.op("scalar", "activation", [ak], [ak], out=a[:], in_=a[:], func=AF.Silu)
                    s, sk = sq.next()
                    P.op("gpsimd", "tensor_tensor", [ak], [sk], out=s[:], in0=a[:], in1=a[:], op=ALU.mult)
                    dst = qT if m < 4 else kT
                    dkey = ("qT", m) if m < 4 else ("kT", m - 4)
                    for (n0, nn) in NTILES:
                        pt, pk = pss.next()
                        P.op("tensor", "matmul", [sk, "ones"], [pk], pt[:, 0:nn], lhsT=ones[:], rhs=s[:, n0:n0 + nn],
                             start=True, stop=True)
                        r, rk = rn.next()
                        P.op("scalar", "activation", [pk], [rk], out=r[:, 0:nn], in_=pt[:, 0:nn], func=AF.Ln,
                             bias=1e-6, scale=1.0)
                        P.op("scalar", "activation", [rk], [rk], out=r[:, 0:nn], in_=r[:, 0:nn], func=AF.Exp, scale=-0.5)
                        P.op("vector", "scalar_tensor_tensor", [ak, rk], [dkey], out=dst[:, m % 4, n0:n0 + nn],
                             in0=a[:, n0:n0 + nn], scalar=(128.0 ** -0.5 if m < 4 else 1.0), in1=r[:, 0:nn],
                             op0=ALU.mult, op1=ALU.mult)
                P.flush()
            if DN_CUT == 1:
                return
            QK = [("qT", i) for i in range(4)]
            KK = [("kT", i) for i in range(4)]
            VK = [("vT", i) for i in range(4)]
            sm = sb("dn_sm", [128, NT, 16])
            P.dma("sync", [("sm", t) for t in range(NT)], ["smt"], out=sm[:],
                  in_=sm_d.rearrange("(t p) c -> p t c", p=128))
            alr = sb("dn_alr", [128, 68])
            dtr = sb("dn_dtr", [128, 68])
            P.dma("sync", [], ["alr"], out=alr[:], in_=alog[l, :, :])
            P.dma("sync", [], ["dtr"], out=dtr[:], in_=dtb[l, :, :])
            beta = sb("dn_beta", [128, NT, 4])
            g = sb("dn_g", [128, NT, 4])
            tmpg = sb("dn_tmpg", [128, NT, 4])
            v3 = lambda a: a[:].rearrange("p (t h) -> p t h", h=4)
            P.op("scalar", "activation", ["smt"], ["beta"], out=beta[:], in_=sm[:, :, 0:4], func=AF.Sigmoid)
            P.op("vector", "tensor_tensor", ["smt", "dtr"], ["tmpg"], out=tmpg[:], in0=sm[:, :, 4:8], in1=v3(dtr), op=ALU.add)
            P.op("scalar", "activation", ["tmpg"], ["tmpg"], out=tmpg[:], in_=tmpg[:], func=AF.Exp)
            P.op("scalar", "activation", ["tmpg"], ["tmpg"], out=tmpg[:], in_=tmpg[:], func=AF.Ln, bias=1.0, scale=1.0)
            P.op("scalar", "activation", ["alr"], ["alr"], out=alr[:], in_=alr[:], func=AF.Exp)
            P.op("vector", "scalar_tensor_tensor", ["tmpg", "alr"], ["g"], out=g[:], in0=tmpg[:], scalar=-1.0,
                 in1=v3(alr), op0=ALU.mult, op1=ALU.mult)
            U = sb("dn_U", [128, 128])
            Mst = sb("dn_Mst", [128, 4, 128])
            Mup = sb("dn_Mup", [128, 4, 128])
            P.op("gpsimd", "memset", [], ["U"], U[:], 1.0)
            P.op("gpsimd", "affine_select", ["U"], ["U"], out=U[:], in_=U[:], pattern=[[1, 128]],
                 compare_op=ALU.is_ge, fill=0.0, base=0, channel_multiplier=-1)
            P.op("gpsimd", "memset", [], ["Mst"], Mst[:], 0.0)
            P.op("gpsimd", "affine_select", ["Mst"], ["Mst"], out=Mst[:], in_=Mst[:], pattern=[[0, 4], [-1, 128]],
                 compare_op=ALU.is_ge, fill=NEG, base=-1, channel_multiplier=1)
            P.op("gpsimd", "memset", [], ["Mup"], Mup[:], 0.0)
            P.op("gpsimd", "affine_select", ["Mup"], ["Mup"], out=Mup[:], in_=Mup[:], pattern=[[0, 4], [1, 128]],
                 compare_op=ALU.is_ge, fill=NEG, base=0, channel_multiplier=-1)
            gc = sb("dn_gc", [128, 68])
            ngc = sb("dn_ngc", [128, 68])
            gl = sb("dn_gl", [128, 68])
            egc = sb("dn_egc", [128, 68])
            ekd = sb("dn_ekd", [128, 68])
            egl = sb("dn_egl", [128, 68])
            bgc = sb("dn_bgc", [128, 68])
            st_g = ExitStack()
            psg = st_g.enter_context(nc.psum_tensor(uname("dn_psg"), [128, 512], F32))
            g2 = g[:].rearrange("p t h -> p (t h)")
            P.op("tensor", "matmul", ["g", "U"], ["psg"], psg[:, 0:68], lhsT=U[:], rhs=g2, start=True, stop=True)
            P.op("tensor", "matmul", ["g", "ones"], ["psg"], psg[:, 128:196], lhsT=ones[:], rhs=g2, start=True, stop=True)
            P.op("vector", "tensor_copy", ["psg"], ["gc"], out=gc[:], in_=psg[:, 0:68])
            P.op("vector", "tensor_copy", ["psg"], ["gl"], out=gl[:], in_=psg[:, 128:196])
            P.op("vector", "tensor_scalar", ["gc"], ["ngc"], out=ngc[:], in0=gc[:], scalar1=-1.0, scalar2=None, op0=ALU.mult)
            P.op("scalar", "activation", ["gc"], ["egc"], out=egc[:], in_=gc[:], func=AF.Exp)
            P.op("scalar", "activation", ["gl"], ["egl"], out=egl[:], in_=gl[:], func=AF.Exp)
            P.op("vector", "tensor_tensor", ["gl", "gc"], ["ekd"], out=ekd[:], in0=gl[:], in1=gc[:], op=ALU.subtract)
            P.op("scalar", "activation", ["ekd"], ["ekd"], out=ekd[:], in_=ekd[:], func=AF.Exp)
            P.op("vector", "tensor_tensor", ["egc", "beta"], ["bgc"], out=bgc[:], in0=egc[:],
                 in1=beta[:].rearrange("p t h -> p (t h)"), op=ALU.mult)
            P.flush()
            st_g.close()
            if DN_CUT == 2:
                return

            NB = 4
            pA = [ps("dn_pA%d" % i) for i in range(2)]
            pB = [ps("dn_pB%d" % i) for i in range(2)]
            pS = [ps("dn_pS%d" % i) for i in range(2)]
            pTk = ps("dn_pTk")
            pTv = ps("dn_pTv")
            Dg_ = [Ring(sb, "dn_Dg%d_" % p_, [128, 4, 128], F32, 1) for p_ in range(2)]
            dec_ = [Ring(sb, "dn_dec%d_" % p_, [128, 4, 128], F32, 1) for p_ in range(2)]
            decT = Ring(sb, "dn_decT", [128, 4, 128], F32, NB)
            Abuf_ = [Ring(sb, "dn_A%d_" % p_, [128, 4, 128], F32, 2) for p_ in range(2)]
            Bbuf_ = [Ring(sb, "dn_B%d_" % p_, [128, 4, 128], F32, 2) for p_ in range(2)]
            Xbuf_ = [Ring(sb, "dn_X%d_" % p_, [128, 4, 128], F32, 2) for p_ in range(2)]
            bvb_ = [Ring(sb, "dn_bv%d_" % p_, [128, 4, 128], F32, 1) for p_ in range(2)]
            kbgb_ = [Ring(sb, "dn_kbg%d_" % p_, [128, 4, 128], F32, 1) for p_ in range(2)]
            u4b = Ring(sb, "dn_u4", [128, 4, 128], F32, NB)
            wT4b = Ring(sb, "dn_wT4", [128, 4, 128], BF16, NB)
            aqk4b = Ring(sb, "dn_aqk", [128, 4, 128], BF16, NB)
            kd4b = Ring(sb, "dn_kd4", [128, 4, 128], BF16, NB)

            def slot(ring, t):
                i_ = t % len(ring.bufs)
                return ring.bufs[i_], ring.keys[i_]
            S4 = sb("dn_S4", [128, 4, 128])
            S4b = sb("dn_S4b", [128, 4, 128], BF16)
            P.op("vector", "memset", [], ["S4"], S4[:], 0.0)
            P.op("gpsimd", "memset", [], ["S4b"], S4b[:], 0.0)
            vn4b = Ring(sb, "dn_vn4", [128, 4, 128], BF16, 2)
            qs4b = Ring(sb, "dn_qs4", [128, 4, 128], F32, 2)
            o4b = Ring(sb, "dn_o4", [128, 4, 128], F32, 2)
            ztb = Ring(sb, "dn_zt", [128, 512], F32, 2)
            ogb = Ring(sb, "dn_og", [128, 512], BF16, 2)
            ssb = Ring(sb, "dn_ss", [128, 8], F32, 2)
            junk = sb("dn_junk", [128, 128])
            odT = sb("dn_odT", [128, 4, LP], BF16)
            ng = sb("dn_ng", [128, 512])
            P.dma("sync", [], ["ng"], out=ng[:], in_=ngr[l, :, :])
            prep_out = {}

            def F(ap):
                return ap[:].rearrange("p h c -> p (h c)")

            def prep(t):
                par = t % 2
                Dg, dec, Abuf, Bbuf, Xbuf, bvb, kbgb = Dg_[par], dec_[par], Abuf_[par], Bbuf_[par], Xbuf_[par], bvb_[par], kbgb_[par]
                cs = slice(t * 128, (t + 1) * 128)
                col = lambda a, h: a[:, t * 4 + h:t * 4 + h + 1]
                dg, dgk = Dg.next()
                for h in range(4):
                    P.op("gpsimd", "tensor_scalar", ["ident", "gc"], [dgk], out=dg[:, h, :], in0=ident[:],
                         scalar1=col(gc, h), scalar2=None, op0=ALU.mult)
                a_ps, ak_ps = pA[par], "dn_pA%d" % par
                b_ps, bk_ps = pB[par], "dn_pB%d" % par
                x_ps, xk_ps = b_ps, bk_ps
                P.op("tensor", "matmul", [dgk, "negones"], [ak_ps], a_ps[:], lhsT=negones[:], rhs=F(dg), start=True, stop=False)
                P.op("tensor", "matmul", ["Mst", "ident"], [ak_ps], a_ps[:], lhsT=ident[:], rhs=F(Mst), start=False, stop=True)
                P.op("tensor", "matmul", [dgk, "ones"], [bk_ps], b_ps[:], lhsT=ones[:], rhs=F(dg), start=True, stop=False)
                P.op("tensor", "matmul", ["Mup", "ident"], [bk_ps], b_ps[:], lhsT=ident[:], rhs=F(Mup), start=False, stop=True)
                de, dek = dec.next()
                deT, deTk = slot(decT, t)
                for h in range(4):
                    P.op("scalar", "activation", [ak_ps, "gc"], [dek], out=de[:, h, :], in_=a_ps[:, h * 128:(h + 1) * 128],
                         func=AF.Exp, bias=col(gc, h), scale=1.0)
                    P.op("scalar", "activation", [bk_ps, "ngc"], [deTk], out=deT[:, h, :], in_=b_ps[:, h * 128:(h + 1) * 128],
                         func=AF.Exp, bias=col(ngc, h), scale=1.0)
                yield
                if PREP_CUT == 1:
                    return
                for h in range(4):
                    P.op("tensor", "matmul", KK, [xk_ps], x_ps[:, h * 128:(h + 1) * 128], lhsT=kT[:, h, cs], rhs=kT[:, h, cs],
                         start=True, stop=True)
                A, Ak = Abuf.next()
                for h in range(4):
                    P.op("vector", "scalar_tensor_tensor", [xk_ps, "beta", dek], [Ak], out=A[:, h, :],
                         in0=x_ps[:, h * 128:(h + 1) * 128], scalar=beta[:, t, h:h + 1], in1=de[:, h, :],
                         op0=ALU.mult, op1=ALU.mult)
                for h in range(4):
                    P.op("tensor", "transpose", [Ak, "ident"], [ak_ps], out=a_ps[:, h * 128:(h + 1) * 128], in_=A[:, h, :],
                         identity=ident[:])
                Bm, Bk = Bbuf.next()
                P.op("scalar", "copy", [ak_ps], [Bk], out=F(Bm), in_=a_ps[:])
                X, Xk = Xbuf.next()
                for h in range(4):
                    P.op("gpsimd", "tensor_tensor", ["ident", Bk], [Xk], out=X[:, h, :], in0=ident[:], in1=Bm[:, h, :],
                         op=ALU.subtract)
                if PREP_CUT == 2:
                    return
                for h in range(4):
                    P.op("tensor", "matmul", KK + ["identb"], ["dn_pTk"], pTk[:, h * 128:(h + 1) * 128], lhsT=kT[:, h, cs],
                         rhs=identb[:], start=True, stop=True)
                    P.op("tensor", "matmul", VK + ["identb"], ["dn_pTv"], pTv[:, h * 128:(h + 1) * 128], lhsT=vT[:, h, cs],
                         rhs=identb[:], start=True, stop=True)
                kbg, kbgk = kbgb.next()
                kd4, kd4k = slot(kd4b, t)
                bv, bvk = bvb.next()
                if PREP_CUT == 31:
                    return
                for h in range(4):
                    P.op("vector", "tensor_scalar", ["dn_pTk", "bgc"], [kbgk], out=kbg[:, h, :], in0=pTk[:, h * 128:(h + 1) * 128],
                         scalar1=col(bgc, h), scalar2=None, op0=ALU.mult)
                    if PREP_CUT == 32:
                        continue
                    P.op("scalar", "activation", ["dn_pTk", "ekd"], [kd4k], out=kd4[:, h, :], in_=pTk[:, h * 128:(h + 1) * 128],
                         func=AF.Copy, scale=col(ekd, h))
                    if PREP_CUT == 33:
                        continue
                    P.op("vector", "tensor_scalar", ["dn_pTv", "beta"], [bvk], out=bv[:, h, :],
                         in0=pTv[:, h * 128:(h + 1) * 128], scalar1=beta[:, t, h:h + 1], scalar2=None, op0=ALU.mult)
                if PREP_CUT in (32, 33):
                    return
                yield
                if PREP_CUT == 3:
                    return
                for n in range(1, 7):
                    A2, A2k = Abuf.next()
                    for h in range(4):
                        P.op("tensor", "matmul", [Ak, Bk], [ak_ps], a_ps[:, h * 128:(h + 1) * 128], lhsT=Bm[:, h, :], rhs=A[:, h, :],
                             start=True, stop=True)
                    if n < 6:
                        B2, B2k = Bbuf.next()
                        for h in range(4):
                            P.op("tensor", "matmul", [Ak, Bk], [bk_ps], b_ps[:, h * 128:(h + 1) * 128], lhsT=A[:, h, :], rhs=Bm[:, h, :],
                                 start=True, stop=True)
                    P.op("scalar", "copy", [ak_ps], [A2k], out=F(A2), in_=a_ps[:])
                    if n < 6:
                        P.op("vector", "tensor_copy", [bk_ps], [B2k], out=F(B2), in_=b_ps[:])
                    for h in range(4):
                        P.op("tensor", "matmul", [A2k, Xk], [ak_ps], a_ps[:, h * 128:(h + 1) * 128], lhsT=A2[:, h, :], rhs=X[:, h, :],
                             start=True, stop=True)
                    X2, X2k = Xbuf.next()
                    P.op("vector", "tensor_tensor", [ak_ps, Xk], [X2k], out=F(X2), in0=a_ps[:], in1=F(X), op=ALU.add)
                    A, Ak = A2, A2k
                    if n < 6:
                        Bm, Bk = B2, B2k
                    X, Xk = X2, X2k
                    yield
                if PREP_CUT == 4:
                    return
                u4, u4k = slot(u4b, t)
                wT4, wT4k = slot(wT4b, t)
                aqk, aqkk = slot(aqk4b, t)
                for h in range(4):
                    P.op("tensor", "matmul", [Xk, bvk], [ak_ps], a_ps[:, h * 128:(h + 1) * 128], lhsT=X[:, h, :], rhs=bv[:, h, :],
                         start=True, stop=True)
                    P.op("tensor", "matmul", [Xk, kbgk], [bk_ps], b_ps[:, h * 128:(h + 1) * 128], lhsT=kbg[:, h, :], rhs=X[:, h, :],
                         start=True, stop=True)
                P.op("scalar", "copy", [ak_ps], [u4k], out=F(u4), in_=a_ps[:])
                P.op("scalar", "copy", [bk_ps], [wT4k], out=F(wT4), in_=b_ps[:])
                for h in range(4):
                    P.op("tensor", "matmul", KK + QK, [ak_ps], a_ps[:, h * 128:(h + 1) * 128], lhsT=kT[:, h, cs], rhs=qT[:, h, cs],
                         start=True, stop=True)
                P.op("vector", "tensor_tensor", [ak_ps, deTk], [aqkk], out=F(aqk), in0=a_ps[:], in1=F(deT), op=ALU.mult)
                prep_out[t] = (u4, u4k, wT4, wT4k, aqk, aqkk, kd4, kd4k)
                yield

            def scan(ts):
                for t in ts:
                    u4, u4k, wT4, wT4k, aqk, aqkk, kd4, kd4k = prep_out.pop(t)
                    cs = slice(t * 128, (t + 1) * 128)
                    col = lambda a, h: a[:, t * 4 + h:t * 4 + h + 1]
                    p1, p1k = pS[0], "dn_pS0"
                    p2, p2k = pS[1], "dn_pS1"
                    for h in range(4):
                        P.op("tensor", "matmul", [wT4k, "S4b"], [p1k], p1[:, h * 128:(h + 1) * 128], lhsT=wT4[:, h, :], rhs=S4b[:, h, :],
                             start=True, stop=True)
                    for h in range(4):
                        P.op("tensor", "matmul", QK + ["S4b"], [p2k], p2[:, h * 128:(h + 1) * 128], lhsT=qT[:, h, cs], rhs=S4b[:, h, :],
                             start=True, stop=True)
                    vn, vnk = vn4b.next()
                    P.op("vector", "tensor_tensor", [u4k, p1k], [vnk], out=F(vn), in0=F(u4), in1=p1[:], op=ALU.subtract)
                    qs, qsk = qs4b.next()
                    for h in range(4):
                        P.op("scalar", "activation", [p2k, "egc"], [qsk], out=qs[:, h, :], in_=p2[:, h * 128:(h + 1) * 128],
                             func=AF.Copy, scale=col(egc, h))
                    yield
                    for h in range(4):
                        P.op("tensor", "matmul", [aqkk, vnk], [p1k], p1[:, h * 128:(h + 1) * 128], lhsT=aqk[:, h, :], rhs=vn[:, h, :],
                             start=True, stop=True)
                    for h in range(4):
                        P.op("tensor", "matmul", [kd4k, vnk], [p2k], p2[:, h * 128:(h + 1) * 128], lhsT=kd4[:, h, :], rhs=vn[:, h, :],
                             start=True, stop=True)
                    o4, o4k = o4b.next()
                    P.op("vector", "tensor_tensor", [p1k, qsk], [o4k], out=F(o4), in0=p1[:], in1=F(qs), op=ALU.add)
                    for h in range(4):
                        P.op("vector", "scalar_tensor_tensor", ["S4", "egl", p2k], ["S4"], out=S4[:, h, :], in0=S4[:, h, :],
                             scalar=col(egl, h), in1=p2[:, h * 128:(h + 1) * 128], op0=ALU.mult, op1=ALU.add)
                    P.op("gpsimd", "tensor_copy", ["S4"], ["S4b"], out=F(S4b), in_=F(S4))
                    yield
                    zt, ztk = ztb.next()
                    P.dma("sync", [("z", t)], [ztk], out=zt[:], in_=z_d[t * 128:(t + 1) * 128, :])
                    ss, ssk = ssb.next()
                    for h in range(4):
                        P.op("scalar", "activation", [o4k], ["dn_junk", ssk], out=junk[:], in_=o4[:, h, :], func=AF.Square,
                             accum_out=ss[:, h:h + 1])
                    P.op("vector", "tensor_scalar", [ssk], [ssk], out=ss[:, 4:8], in0=ss[:, 0:4], scalar1=1.0 / 128, scalar2=1e-6,
                         op0=ALU.mult, op1=ALU.add)
                    P.op("scalar", "activation", [ssk], [ssk], out=ss[:, 4:8], in_=ss[:, 4:8], func=AF.Sqrt)
                    P.op("vector", "reciprocal", [ssk], [ssk], out=ss[:, 4:8], in_=ss[:, 4:8])
                    P.op("scalar", "activation", [ztk], [ztk], out=zt[:], in_=zt[:], func=AF.Silu)
                    P.op("gpsimd", "tensor_tensor", [ztk, "ng"], [ztk], out=zt[:], in0=zt[:], in1=ng[:], op=ALU.mult)
                    og, ogk = ogb.next()
                    for h in range(4):
                        P.op("gpsimd", "tensor_scalar", [o4k, ssk], [o4k], out=o4[:, h, :], in0=o4[:, h, :],
                             scalar1=ss[:, 4 + h:5 + h], scalar2=None, op0=ALU.mult)
                        P.op("gpsimd", "tensor_tensor", [o4k, ztk], [ogk], out=og[:, h * 128:(h + 1) * 128], in0=o4[:, h, :],
                             in1=zt[:, h * 128:(h + 1) * 128], op=ALU.mult)
                    for h in range(4):
                        P.op("tensor", "matmul", [ogk, "identb"], ["dn_pTk"], pTk[:, h * 128:(h + 1) * 128],
                             lhsT=og[:, h * 128:(h + 1) * 128], rhs=identb[:], start=True, stop=True)
                    P.op("scalar", "copy", ["dn_pTk"], [("odT", t)], out=odT[:, :, cs],
                         in_=pTk[:, 0:512].rearrange("p (h c) -> p h c", h=4))
                    yield

            for s_ in range(0, NT + 2, 2):
                if DN_CUT == 3 and s_ >= 2:
                    break
                gens = [prep(t) for t in (s_, s_ + 1) if t < NT]
                prev = [t for t in (s_ - 2, s_ - 1) if 0 <= t < NT]
                if prev:
                    gens.append(scan(prev))
                interleave(gens)
            for h in range(4):
                P.dma("sync", [("odT", t) for t in range(NT)], [("mixT", h)], out=mixT_d[h, :, :], in_=odT[:, h, :])
            P.flush()

    def stage_dsa(l):
        with ExitStack() as st:
            sb, ps = stage_allocs(st)
            saq = sb("sa_q", [128, 4, LP], BF16)
            sak = sb("sa_k", [128, 4, LP], BF16)
            iq = sb("sa_iq", [128, 4, LP], BF16)
            ik = sb("sa_ik", [128, LP], BF16)
            for c in range(4):
                P.dma("sync", [("ropeT", c)], ["saq"], out=saq[:, c, :], in_=ropeT_d[c, :, :])
                P.dma("sync", [("ropeT", 4 + c)], ["sak"], out=sak[:, c, :], in_=ropeT_d[4 + c, :, :])
                P.dma("sync", [("ropeT", 8 + c)], ["iq"], out=iq[:, c, :], in_=ropeT_d[8 + c, :, :])
            P.dma("sync", [("ropeT", 12)], ["ik"], out=ik[:], in_=ropeT_d[12, :, :])
            va = sb("sa_va", [128, NT, 8, 65], BF16)
            P.op("gpsimd", "memset", [], ["va"], va[:], 1.0)
            for t in range(NT):
                P.dma("sync", [("sv", t)], ["va"], out=va[:, t, :, 0:64],
                      in_=sv_d[t * 128:(t + 1) * 128, :].rearrange("p (h d) -> p h d", d=64))
            sm = sb("sa_sm", [128, NT, 16])
            P.dma("sync", [("sm", t) for t in range(NT)], ["sasm"], out=sm[:], in_=sm_d.rearrange("(t p) c -> p t c", p=128))
            cmask = sb("sa_cmask", [128, 128], BF16)
            P.op("gpsimd", "memset", [], ["cmask"], cmask[:], 0.0)
            P.op("gpsimd", "affine_select", ["cmask"], ["cmask"], out=cmask[:], in_=cmask[:], pattern=[[-1, 128]],
                 compare_op=ALU.is_ge, fill=NEG, base=0, channel_multiplier=1)
            osT = sb("sa_osT", [128, 4, LP], BF16)
            sc_ps = Ring(ps, "sa_scps", [128, 512], F32, 2)
            s_ps = Ring(ps, "sa_sps", [128, 512], F32, 3)
            o_ps = [ps("sa_ops%d" % i, [128, 512]) for i in range(2)]
            pTb = ps("sa_pTb")
            relu_b = Ring(sb, "sa_relu", [128, 512], F32, 3)
            accb = Ring(sb, "sa_acc", [128, LP], F32, 2)
            mbb = Ring(sb, "sa_mb", [128, LP], BF16, 4)
            junkb = Ring(sb, "sa_junk", [128, LP], BF16, 2)
            ptb = Ring(sb, "sa_pt", [128, 512], BF16, 3)
            stt = Ring(sb, "sa_st", [128, 8], F32, 2)
            wtb = Ring(sb, "sa_wt", [128, NIT + 2], F32, 2)
            ftab = sb("sa_ftab", [128, NIT + 2])
            for n_ in range(NIT + 2):
                P.op("gpsimd", "memset", [], ["ftab"], ftab[:, n_:n_ + 1], 2.0 ** (-n_))
            osb = Ring(sb, "sa_os", [128, 512], BF16, 2)
            rdb = Ring(sb, "sa_rd", [128, 8], F32, 2)

            def hrows(h):
                return slice(0, 64) if h % 2 == 0 else slice(64, 128)

            mb_of = {}

            def scores(i):
                nk = 128 * (i + 1)
                qs_ = slice(i * 128, (i + 1) * 128)
                acc, acck = accb.next()
                for h in range(8):
                    for k0 in range(0, nk, 512):
                        kn = min(512, nk - k0)
                        pt, pk = sc_ps.next()
                        P.op("tensor", "matmul", ["iq", "ik"], [pk], pt[:, 0:kn], lhsT=iq[hrows(h), h // 2, qs_],
                             rhs=ik[hrows(h), k0:k0 + kn], start=True, stop=True)
                        if h == 0:
                            r, rk = relu_b.next()
                            P.op("scalar", "activation", [pk], [rk], out=r[:, 0:kn], in_=pt[:, 0:kn], func=AF.Relu)
                            P.op("vector", "tensor_scalar", [rk, "sasm"], [acck], out=acc[:, k0:k0 + kn], in0=r[:, 0:kn],
                                 scalar1=sm[:, i, 8:9], scalar2=None, op0=ALU.mult)
                        else:
                            r, rk = relu_b.next()
                            P.op("scalar", "activation", [pk], [rk], out=r[:, 0:kn], in_=pt[:, 0:kn], func=AF.Relu)
                            P.op("vector", "scalar_tensor_tensor", [rk, "sasm", acck], [acck], out=acc[:, k0:k0 + kn], in0=r[:, 0:kn],
                                 scalar=sm[:, i, 8 + h:9 + h], in1=acc[:, k0:k0 + kn], op0=ALU.mult, op1=ALU.add)
                    yield
                P.op("gpsimd", "affine_select", [acck], [acck], out=acc[:, i * 128:nk], in_=acc[:, i * 128:nk], pattern=[[-1, 128]],
                     compare_op=ALU.is_ge, fill=-1e30, base=0, channel_multiplier=1)
                mb, mbk = mbb.next()
                s8, s8k = stt.next()
                if nk <= KTOP:
                    P.op("vector", "memset", [], [s8k], s8[:, 0:1], -1e29)
                else:
                    jk, jkk = junkb.next()
                    P.op("vector", "tensor_reduce", [acck], [s8k], out=s8[:, 5:6], in_=acc[:, 0:nk], axis=mybir.AxisListType.X, op=ALU.max)
                    P.op("vector", "tensor_reduce", [acck], [s8k], out=s8[:, 0:1], in_=acc[:, 0:i * 128], axis=mybir.AxisListType.X, op=ALU.min)
                    P.op("vector", "tensor_tensor", [s8k], [s8k], out=s8[:, 1:2], in0=s8[:, 5:6], in1=s8[:, 0:1], op=ALU.subtract)
                    P.op("vector", "tensor_scalar", [s8k], [s8k], out=s8[:, 1:2], in0=s8[:, 1:2], scalar1=1.0001, scalar2=1e-6,
                         op0=ALU.mult, op1=ALU.add)
                    wt, wtk = wtb.next()
                    P.op("vector", "tensor_scalar", ["ftab", s8k], [wtk], out=wt[:], in0=ftab[:], scalar1=s8[:, 1:2], scalar2=None,
                         op0=ALU.mult)
                    P.op("vector", "tensor_tensor", [s8k, wtk], [s8k], out=s8[:, 2:3], in0=s8[:, 0:1], in1=wt[:, 1:2], op=ALU.add)
                    for it in range(1, NIT + 1):
                        P.op("vector", "tensor_scalar", [acck, s8k], [jkk, s8k], out=jk[:, 0:nk], in0=acc[:, 0:nk], scalar1=s8[:, 2:3],
                             scalar2=None, op0=ALU.is_ge, op1=ALU.add, accum_out=s8[:, 3:4])
                        P.op("vector", "tensor_scalar", [s8k], [s8k], out=s8[:, 4:5], in0=s8[:, 3:4], scalar1=KTOP - 0.5, scalar2=0.5,
                             op0=ALU.is_ge, op1=ALU.subtract)
                        P.op("vector", "scalar_tensor_tensor", [s8k, wtk], [s8k], out=s8[:, 2:3], in0=s8[:, 4:5], scalar=wt[:, it:it + 1],
                             in1=s8[:, 2:3], op0=ALU.mult, op1=ALU.add)
                        yield
                    P.op("vector", "tensor_tensor", [s8k, wtk], [s8k], out=s8[:, 0:1], in0=s8[:, 2:3], in1=wt[:, NIT + 1:NIT + 2],
                         op=ALU.subtract)
                P.op("vector", "tensor_scalar", [acck, s8k], [mbk], out=mb[:, 0:nk], in0=acc[:, 0:nk], scalar1=s8[:, 0:1], scalar2=NEG,
                     op0=ALU.is_lt, op1=ALU.mult)
                mb_of[i] = (mb, mbk)
                yield

            def attention(i):
                mb, mbk = mb_of.pop(i)
                qs_ = slice(i * 128, (i + 1) * 128)
                groups = [(kt, hg) for kt in range(i + 1) for hg in range(2)]
                pend = None
                for g in groups + [None]:
                    cur = None
                    if g is not None:
                        kt, hg = g
                        ks_ = slice(kt * 128, (kt + 1) * 128)
                        sp, spk = s_ps.next()
                        for pair in ((0, 2), (1, 3)):
                            for hh in pair:
                                h = hg * 4 + hh
                                P.op("tensor", "matmul", ["sak", "saq"], [spk], sp[:, hh * 128:(hh + 1) * 128],
                                     lhsT=sak[hrows(h), h // 2, ks_], rhs=saq[hrows(h), h // 2, qs_], start=(hh == 0), stop=False,
                                     skip_group_check=True)
                            for hh in pair:
                                P.op("tensor", "matmul", [mbk, "identb"], [spk], sp[:, hh * 128:(hh + 1) * 128],
                                     lhsT=mb[:, ks_], rhs=identb[:], start=False, stop=True, skip_group_check=True)
                        pt, ptk = ptb.next()
                        P.op("scalar", "activation", [spk], [ptk], out=pt[:], in_=sp[:], func=AF.Exp, scale=0.125)
                        cur = (kt, hg, pt, ptk)
                    if pend is not None:
                        kt, hg, pt, ptk = pend
                        for hh in range(4):
                            h = hg * 4 + hh
                            P.op("tensor", "matmul", [ptk, "va"], ["sa_ops%d" % hg], o_ps[hg][:, hh * 65:(hh + 1) * 65],
                                 lhsT=pt[:, hh * 128:(hh + 1) * 128], rhs=va[:, kt, h, :], start=(kt == 0 and hh == 0), stop=(kt == i),
                                 skip_group_check=True)
                    pend = cur
                    yield
                rd, rdk = rdb.next()
                for hg in range(2):
                    P.op("vector", "reciprocal", ["sa_ops%d" % hg], [rdk], out=rd[:, hg * 4:(hg + 1) * 4],
                         in_=o_ps[hg][:, 0:260].rearrange("p (h d) -> p h d", d=65)[:, :, 64])
                os_, osk = osb.next()
                for h in range(8):
                    hg, hh = h // 4, h % 4
                    P.op("vector", "tensor_scalar", ["sa_ops%d" % hg, rdk], [osk], out=os_[:, h * 64:(h + 1) * 64],
                         in0=o_ps[hg][:, hh * 65:hh * 65 + 64], scalar1=rd[:, h:h + 1], scalar2=None, op0=ALU.mult)
                for c in range(4):
                    P.op("tensor", "matmul", [osk, "identb"], ["sa_pTb"], pTb[:, c * 128:(c + 1) * 128],
                         lhsT=os_[:, c * 128:(c + 1) * 128], rhs=identb[:], start=True, stop=True)
                P.op("scalar", "copy", ["sa_pTb"], [("osT", i)], out=osT[:, :, qs_],
                     in_=pTb[:, 0:512].rearrange("p (h c) -> p h c", h=4))
                yield

            def att_chain(ts):
                for t_ in ts:
                    yield from attention(t_)

            for i in range(0, NT + 2, 2):
                gens = [scores(t_) for t_ in (i, i + 1) if t_ < NT]
                prev = [t_ for t_ in (i - 2, i - 1) if 0 <= t_ < NT]
                if prev:
                    gens.append(att_chain(prev))
                interleave(gens)
            for c in range(4):
                P.dma("sync", [("osT", t) for t in range(NT)], [("mixT", 4 + c)], out=mixT_d[4 + c, :, :], in_=osT[:, c, :])
            P.flush()

    def stage_mix_ln1(l, src, hT, comb):
        with ExitStack() as st:
            sb, ps = stage_allocs(st)
            mixT = sb("mx_mixT", [128, 8, LP], BF16)
            for c in range(8):
                P.dma("sync", [("mixT", c)], ["mixT"], out=mixT[:, c, :], in_=mixT_d[c, :, :])
            wo = sb("mx_wo", [128, 8, D], BF16)
            wol = w_out[l].rearrange("(c p) n -> p c n", p=128)
            for c in range(8):
                P.dma("gpsimd", [], ["wo"], out=wo[:, c, :], in_=wol[:, c, :])
            g_rep = sb("mx_g", [128, D])
            b_rep = sb("mx_b", [128, D])
            P.dma("sync", [], ["lng"], out=g_rep[:], in_=ln1g[l, :, :])
            P.dma("sync", [], ["lnb"], out=b_rep[:], in_=ln1b[l, :, :])
            wrs = sb("mx_wr", [128, 8, 36])
            P.dma("sync", [], ["wrs"], out=wrs[:], in_=wr[l].rearrange("(c p) n -> p c n", p=128))
            br = sb("mx_br", [128, 36])
            P.dma("sync", [], ["br"], out=br[:], in_=brep[l, :, :])
            hin = Ring(sb, "mx_hin", [128, D], F32, 2)
            tb = Ring(sb, "mx_t", [128, D], F32, 2)
            ob = Ring(sb, "mx_o", [128, D], F32, 2)
            scr = sb("mx_scr", [128, D])
            stb = Ring(sb, "mx_st", [128, 4], F32, 2)
            hTf = Ring(sb, "mx_hTf", [128, 8, 128], F32, 2)
            pM = [ps("mx_pM%d" % i, [128, 1024]) for i in range(2)]
            pT = [ps("mx_pT%d" % i, [128, 1024]) for i in range(1)]
            pLs = [ps("mx_pL%d" % i, [128, 512]) for i in range(2)]
            rt = Ring(sb, "mx_rt", [128, 160], F32, 2)
            def mtile(t):
                cs = slice(t * 128, (t + 1) * 128)
                pL, pLk = pLs[t % 2], "mx_pL%d" % (t % 2)
                pm, pmk = pM[t % 2], "mx_pM%d" % (t % 2)
                for n in range(2):
                    for c in range(8):
                        P.op("tensor", "matmul", ["mixT", "wo"], [pmk], pm[:, n * 512:(n + 1) * 512], lhsT=mixT[:, c, cs],
                             rhs=wo[:, c, n * 512:(n + 1) * 512], start=(c == 0), stop=(c == 7))
                hi_, hik = hin.next()
                P.dma("sync", [("h", t)], [hik], out=hi_[:], in_=src[t * 128:(t + 1) * 128, :])
                tt, ttk = tb.next()
                P.op("vector", "scalar_tensor_tensor", [hik, pmk], [ttk], out=tt[:], in0=hi_[:], scalar=ALPHA, in1=pm[:],
                     op0=ALU.mult, op1=ALU.add)
                o, ok = ob.next()
                s4, s4k = stb.next()
                yield
                yield from ln_tile(tt, ttk, g_rep, b_rep, o, ok, scr, "mx_scr", s4, s4k)
                P.dma("sync", [ok], [("h", t)], out=h_d[t * 128:(t + 1) * 128, :], in_=o[:])
                yield
                pt, ptk = pT[0], "mx_pT0"
                for c in range(8):
                    P.op("tensor", "transpose", [ok, "ident"], [ptk], out=pt[:, c * 128:(c + 1) * 128], in_=o[:, c * 128:(c + 1) * 128],
                         identity=ident[:])
                hf, hfk = hTf.next()
                P.op("scalar", "copy", [ptk], [hfk], out=hf[:].rearrange("p c t -> p (c t)"), in_=pt[:])
                P.op("gpsimd", "tensor_copy", [hfk], [("hT", t)], out=hT[:, :, cs], in_=hf[:])
                yield
                for c in range(8):
                    P.op("tensor", "matmul", [hfk, "wrs"], [pLk], pL[:, 0:36], lhsT=hf[:, c, :], rhs=wrs[:, c, :],
                         start=(c == 0), stop=(c == 7))
                yield
                r, rk = rt.next()
                P.op("vector", "tensor_tensor", [pLk, "br"], [rk], out=r[:, 0:36], in0=pL[:, 0:36], in1=br[:], op=ALU.add)
                P.op("vector", "tensor_reduce", [rk], [rk], out=r[:, 148:149], in_=r[:, 0:4], axis=mybir.AxisListType.X, op=ALU.max)
                P.op("vector", "tensor_scalar", [rk], [rk], out=r[:, 149:150], in0=r[:, 148:149], scalar1=-1.0, scalar2=None, op0=ALU.mult)
                P.op("scalar", "activation", [rk], [rk], out=r[:, 36:40], in_=r[:, 0:4], func=AF.Exp, bias=r[:, 149:150], scale=1.0,
                     accum_out=r[:, 150:151])
                P.op("vector", "reciprocal", [rk], [rk], out=r[:, 151:152], in_=r[:, 150:151])
                P.op("vector", "tensor_scalar", [rk], [rk], out=r[:, 44:48], in0=r[:, 0:4], scalar1=r[:, 148:149], scalar2=None,
                     op0=ALU.is_ge)
                P.op("vector", "tensor_scalar", [rk], [rk], out=r[:, 48:52], in0=r[:, 44:48], scalar1=-1.0, scalar2=1e30,
                     op0=ALU.add, op1=ALU.mult)
                for gq in range(4):
                    P.op("vector", "tensor_scalar", [rk], [rk], out=r[:, 52 + gq * 8:60 + gq * 8], in0=r[:, 4 + gq * 8:12 + gq * 8],
                         scalar1=r[:, 48 + gq:49 + gq], scalar2=None, op0=ALU.add)
                yield
                P.op("vector", "max", [rk], [rk], out=r[:, 36:44], in_=r[:, 52:84])
                P.op("vector", "tensor_scalar", [rk], [rk], out=r[:, 84:116], in0=r[:, 52:84], scalar1=r[:, 36:37], scalar2=None,
                     op0=ALU.is_equal)
                P.op("vector", "tensor_scalar", [rk], [rk], out=r[:, 116:148], in0=r[:, 52:84], scalar1=r[:, 37:38], scalar2=None,
                     op0=ALU.is_equal)
                P.op("vector", "tensor_tensor", [rk], [rk], out=r[:, 152:153], in0=r[:, 37:38], in1=r[:, 36:37], op=ALU.subtract)
                yield
                P.op("scalar", "activation", [rk], [rk], out=r[:, 153:154], in_=r[:, 152:153], func=AF.Exp)
                P.op("vector", "tensor_scalar", [rk], [rk], out=r[:, 154:155], in0=r[:, 153:154], scalar1=1.0, scalar2=None, op0=ALU.add)
                P.op("vector", "reciprocal", [rk], [rk], out=r[:, 154:155], in_=r[:, 154:155])
                P.op("vector", "tensor_tensor", [rk], [rk], out=r[:, 155:156], in0=r[:, 154:155], in1=r[:, 151:152], op=ALU.mult)
                P.op("vector", "tensor_tensor", [rk], [rk], out=r[:, 156:157], in0=r[:, 155:156], in1=r[:, 153:154], op=ALU.mult)
                P.op("vector", "tensor_scalar", [rk], [("comb", t)], out=comb[:, t, :], in0=r[:, 84:116], scalar1=r[:, 155:156],
                     scalar2=None, op0=ALU.mult)
                P.op("vector", "scalar_tensor_tensor", [rk, ("comb", t)], [("comb", t)], out=comb[:, t, :], in0=r[:, 116:148],
                     scalar=r[:, 156:157], in1=comb[:, t, :], op0=ALU.mult, op1=ALU.add)
            for t0 in range(0, NT, 2):
                interleave([mtile(t) for t in (t0, t0 + 1) if t < NT])
            P.flush()

    def stage_moe(l, hT, comb, yacc):
        with ExitStack() as st:
            sb, ps = stage_allocs(st)
            w1b = Ring(sb, "mo_w1", [128, 8, 256], BF16, 3)
            w3b = Ring(sb, "mo_w3", [128, 8, 256], BF16, 3)
            w2b = Ring(sb, "mo_w2", [128, 2, D], BF16, 3)
            hidb = Ring(sb, "mo_hid", [128, 2, LP], BF16, 2)
            silb = Ring(sb, "mo_sil", [128, 512], F32, 3)
            stg = Ring(sb, "mo_stg", [128, 2048], F32, 3)
            p1 = Ring(ps, "mo_p1", [128, 512], F32, 2)
            p3 = Ring(ps, "mo_p3", [128, 512], F32, 2)
            pY = [ps("mo_pY%d" % i, [128, 1024]) for i in range(2)]
            HT = [("hT", t) for t in range(NT)]
            yi = 0
            for e in range(NE):
                a1, a1k = w1b.next()
                a3, a3k = w3b.next()
                a2, a2k = w2b.next()
                for (dst_, dkey_, src_, c_) in ((a1, a1k, w1[l, e].rearrange("(c p) f -> p c f", p=128), 8),
                                                (a3, a3k, w3[l, e].rearrange("(c p) f -> p c f", p=128), 8),
                                                (a2, a2k, w2[l, e].rearrange("(c p) n -> p c n", p=128), 2)):
                    sg, sgk = stg.next()
                    sv_ = sg[:].rearrange("p (c f) -> p c f", c=c_)
                    P.dma("sync", [], [sgk], out=sv_, in_=src_)
                    P.op("gpsimd", "tensor_copy", [sgk], [dkey_], out=dst_[:], in_=sv_)
                hid, hidk = hidb.next()
                for fc in range(2):
                    for (n0, nn) in NTILES:
                        q1, q1k = p1.next()
                        q3, q3k = p3.next()
                        for k in range(8):
                            P.op("tensor", "matmul", [a1k] + HT, [q1k], q1[:, 0:nn], lhsT=a1[:, k, fc * 128:(fc + 1) * 128],
                                 rhs=hT[:, k, n0:n0 + nn], start=(k == 0), stop=(k == 7))
                        for k in range(8):
                            P.op("tensor", "matmul", [a3k] + HT, [q3k], q3[:, 0:nn], lhsT=a3[:, k, fc * 128:(fc + 1) * 128],
                                 rhs=hT[:, k, n0:n0 + nn], start=(k == 0), stop=(k == 7))
                        s, sk = silb.next()
                        P.op("scalar", "activation", [q1k], [sk], out=s[:, 0:nn], in_=q1[:, 0:nn], func=AF.Silu)
                        P.op("vector", "tensor_tensor", [sk, q3k], [hidk], out=hid[:, fc, n0:n0 + nn], in0=s[:, 0:nn], in1=q3[:, 0:nn],
                             op=ALU.mult)
                for t in range(NT):
                    cs = slice(t * 128, (t + 1) * 128)
                    py, pyk = pY[yi % 2], "mo_pY%d" % (yi % 2)
                    yi += 1
                    for n in range(2):
                        for fc in range(2):
                            P.op("tensor", "matmul", [hidk, a2k], [pyk], py[:, n * 512:(n + 1) * 512], lhsT=hid[:, fc, cs],
                                 rhs=a2[:, fc, n * 512:(n + 1) * 512], start=(fc == 0), stop=(fc == 1))
                    if e == 0:
                        P.op("vector", "tensor_scalar", [pyk, ("comb", t)], [("yacc", t)], out=yacc[:, t, :], in0=py[:],
                             scalar1=comb[:, t, e:e + 1], scalar2=None, op0=ALU.mult)
                    else:
                        P.op("vector", "scalar_tensor_tensor", [pyk, ("comb", t), ("yacc", t)], [("yacc", t)], out=yacc[:, t, :],
                             in0=py[:], scalar=comb[:, t, e:e + 1], in1=yacc[:, t, :], op0=ALU.mult, op1=ALU.add)
            P.flush()

    def stage_ln2(l, yacc, last):
        with ExitStack() as st:
            sb, ps = stage_allocs(st)
            g_rep = sb("l2_g", [128, D])
            b_rep = sb("l2_b", [128, D])
            P.dma("sync", [], ["lng"], out=g_rep[:], in_=ln2g[l, :, :])
            P.dma("sync", [], ["lnb"], out=b_rep[:], in_=ln2b[l, :, :])
            hin = Ring(sb, "l2_hin", [128, D], F32, 2)
            tb = Ring(sb, "l2_t", [128, D], F32, 2)
            ob = Ring(sb, "l2_o", [128, D], F32, 2)
            scr = sb("l2_scr", [128, D])
            stb = Ring(sb, "l2_st", [128, 4], F32, 2)
            def ltile(t):
                hi_, hik = hin.next()
                P.dma("sync", [("h", t)], [hik], out=hi_[:], in_=h_d[t * 128:(t + 1) * 128, :])
                tt, ttk = tb.next()
                P.op("vector", "scalar_tensor_tensor", [hik, ("yacc", t)], [ttk], out=tt[:], in0=hi_[:], scalar=ALPHA,
                     in1=yacc[:, t, :], op0=ALU.mult, op1=ALU.add)
                o, ok = ob.next()
                s4, s4k = stb.next()
                yield
                yield from ln_tile(tt, ttk, g_rep, b_rep, o, ok, scr, "l2_scr", s4, s4k)
                if not last:
                    P.dma("sync", [ok], [("h", t)], out=h_d[t * 128:(t + 1) * 128, :], in_=o[:])
                else:
                    if t == 0:
                        P.dma("sync", [ok], [("y", t)], out=y[0:112, :], in_=o[16:128, :])
                    elif t < 16:
                        P.dma("sync", [ok], [("y", t)], out=y[t * 128 - 16:t * 128 + 112, :], in_=o[:])
                    else:
                        P.dma("sync", [ok], [("y", t)], out=y[2032:2048, :], in_=o[0:16, :])
            for t0 in range(0, NT, 2):
                interleave([ltile(t) for t in (t0, t0 + 1) if t < NT])
            P.flush()

    stages = []
    res = ExitStack()
    rsb, _ = stage_allocs(res)
    hT = rsb("hT", [128, 8, LP], BF16)
    done = False

    def want(name, l):
        nonlocal done
        if done:
            return False
        if only is not None and (name, l) not in only:
            return False
        if stop_after is not None and stop_after == (name, l):
            done = True
        return True

    for l in range(n_layers):
        src = h0 if l == 0 else h_d
        if want("hT", l):
            stage_hT(src, hT)
        if want("proj", l):
            stage_proj(l, hT)
        if want("dn", l):
            stage_dn(l)
        if want("dsa", l):
            stage_dsa(l)
        moe_st = ExitStack()
        msb, _ = stage_allocs(moe_st)
        comb = msb("comb", [128, NT, NE])
        if want("mix", l):
            stage_mix_ln1(l, src, hT, comb)
        yacc = msb("yacc", [128, NT, D])
        if want("moe", l):
            stage_moe(l, hT, comb, yacc)
        if want("ln2", l):
            stage_ln2(l, yacc, last=(l == n_layers - 1))
        moe_st.close()
    P.finish([("y", t) for t in range(NT)])
    res.close()
    top.close()
    return nc


def _rope_tables():
    inv = 1.0 / (10000.0 ** (np.arange(0, 64, 2, dtype=np.float32) / np.float32(64)))
    pos = np.arange(LP, dtype=np.float32)
    ang = pos[:, None] * inv[None, :].astype(np.float32)
    ang = np.concatenate([ang, ang], -1)
    cos = np.cos(ang).astype(np.float32)
    sin = np.sin(ang).astype(np.float32)
    sgn = np.concatenate([-np.ones(32, np.float32), np.ones(32, np.float32)])
    sins = sin * sgn[None, :]
    cosT = np.ascontiguousarray(np.concatenate([cos.T, cos.T], 0))
    sinT = np.ascontiguousarray(np.concatenate([sins.T, sins.T], 0))
    return cosT, sinT


def make_shared(inp):
    f = lambda a: np.ascontiguousarray(np.asarray(a, dtype=np.float32))
    rep = lambda a: f(np.broadcast_to(np.asarray(a, np.float32)[:, None, :], (DEPTH, 128, np.asarray(a).shape[-1])))
    cosT, sinT = _rope_tables()
    cw = np.asarray(inp["conv_w"], np.float32)
    cwT = f(cw.reshape(DEPTH, 4, 12, 128).transpose(0, 3, 2, 1).reshape(DEPTH, 128, 48))
    sh = {
        "w_in": f(inp["w_in"]), "w_out": f(inp["w_out"]),
        "w1": f(inp["w1"]), "w3": f(inp["w3"]), "w2": f(inp["w2"]),
        "wr": f(np.concatenate([np.asarray(inp["w_grp"], np.float32), np.asarray(inp["w_rtr"], np.float32)], -1)),
        "brep": rep(np.concatenate([np.asarray(inp["b_grp"], np.float32), np.asarray(inp["b_rtr"], np.float32)], -1)),
        "cwT": cwT,
        "alog": rep(np.tile(np.asarray(inp["a_log"], np.float32), (1, NT))),
        "dtb": rep(np.tile(np.asarray(inp["dt_bias"], np.float32), (1, NT))),
        "ngr": rep(np.tile(np.asarray(inp["dn_norm_g"], np.float32), (1, 4))),
        "ln1g": rep(inp["ln1_g"]), "ln1b": rep(inp["ln1_b"]), "ln2g": rep(inp["ln2_g"]), "ln2b": rep(inp["ln2_b"]),
        "ropec": cosT, "ropes": sinT,
    }
    return sh


def make_h0(x_b, meta):
    h0 = np.zeros((LP, D), np.float32)
    h0[:NMETA] = meta
    h0[NMETA:L] = x_b
    return h0


_NC_CACHE = {}


def kernel(**inputs):
    x = np.asarray(inputs["x"], np.float32)
    meta = np.asarray(inputs["meta_tokens"], np.float32)
    sh = make_shared(inputs)
    if "nc" not in _NC_CACHE:
        _NC_CACHE["nc"] = build()
    nc = _NC_CACHE["nc"]
    in_maps = []
    for b in range(8):
        m = dict(sh)
        m["h0"] = make_h0(x[b], meta)
        in_maps.append(m)
    res = run_bass_kernel_spmd(nc, in_maps, core_ids=list(range(8)))
    return np.stack([np.asarray(r["y"], np.float32) for r in res.results], 0)
```

```python
import numpy as np
from contextlib import ExitStack
import concourse.bass as bass
import concourse.mybir as mybir
from concourse.bass_utils import run_bass_kernel_spmd

F32 = mybir.dt.float32
BF16 = mybir.dt.bfloat16
AF = mybir.ActivationFunctionType
ALU = mybir.AluOpType

ENGS = ("tensor", "vector", "scalar", "gpsimd", "sync")
N_DMA_SEMS = 12

D = 1024
SEQ = 2048
NMETA = 16
L = SEQ + NMETA
NT = 17
LP = NT * 128
DEPTH = 2
DIN = 4176
ALPHA = (2.0 * DEPTH) ** 0.25
NEG = -30000.0
KTOP = 256
NIT = 20
NE = 32
DN_CUT = 0
PREP_CUT = 0
NTILES = [(0, 512), (512, 512), (1024, 512), (1536, 512), (2048, 128)]


class Prog:
    def __init__(self, nc, stack):
        self.nc = nc
        self.streams = {e: [] for e in ENGS}
        self.esem = {e: stack.enter_context(nc.semaphore("s_" + e)) for e in ENGS}
        self.eseq = {e: 0 for e in ENGS}
        self.eval_ = {e: 0 for e in ENGS}
        self.dsem = {e: [stack.enter_context(nc.semaphore("d_%s%d" % (e, i)))
                         for i in range(N_DMA_SEMS)] for e in ("sync", "gpsimd", "scalar")}
        self.dcnt = {e: [0] * N_DMA_SEMS for e in self.dsem}
        self.drr = {e: 0 for e in self.dsem}
        self.waited = {e: {} for e in ENGS}
        self.last_w = {}
        self.readers = {}
        self.n_ops = 0
        self.excl = set()

    @staticmethod
    def _sk(src):
        return src if isinstance(src, str) else id(src)

    def _need(self, eng, ev, out):
        if ev is None:
            return
        src, val = ev
        if eng == "tensor" and src == "tensor":
            return
        k = self._sk(src)
        if self.waited[eng].get(k, 0) >= val:
            return
        self.waited[eng][k] = val
        out.append((src, val))

    def _deps(self, eng, reads, writes):
        waits = []
        for k in reads:
            self._need(eng, self.last_w.get(k), waits)
        for k in writes:
            self._need(eng, self.last_w.get(k), waits)
            for ev in self.readers.get(k, {}).values():
                self._need(eng, ev, waits)
        return waits

    def _commit(self, ev, reads, writes):
        for k in reads:
            self.readers.setdefault(k, {})[self._sk(ev[0])] = ev
        for k in writes:
            self.last_w[k] = ev
            self.readers[k] = {}

    def op(self, eng, meth, reads, writes, *args, **kw):
        if eng != "tensor":
            ex = [k for k in reads if isinstance(k, str) and k in self.excl and k not in writes]
            if ex:
                writes = list(writes) + ex
        waits = self._deps(eng, reads, writes)
        self.eseq[eng] += 1
        ev = (eng, self.eseq[eng])
        self.streams[eng].append((waits, meth, args, kw, "E", ev[1]))
        self._commit(ev, reads, writes)
        self.n_ops += 1

    def dma(self, q, reads, writes, out, in_, **kw):
        i = self.drr[q]
        self.drr[q] = (i + 1) % N_DMA_SEMS
        sem = self.dsem[q][i]
        waits = self._deps(q, reads, writes)
        if self.dcnt[q][i] > 0:
            self._need(q, (sem, 16 * self.dcnt[q][i]), waits)
        self.dcnt[q][i] += 1
        ev = (sem, 16 * self.dcnt[q][i])
        kw = dict(kw)
        kw["out"] = out
        kw["in_"] = in_
        self.streams[q].append((waits, "dma_start", (), kw, "D", sem))
        self._commit(ev, reads, writes)
        self.n_ops += 1

    def finish(self, final_keys):
        waits = []
        for k in final_keys:
            self._need("sync", self.last_w.get(k), waits)
        self.streams["sync"].append((waits, None, (), {}, None, None))
        self.flush()

    def barrier(self):
        evs = [(e, self.eseq[e]) for e in ENGS if self.eseq[e] > 0]
        for q in self.dsem:
            for i in range(N_DMA_SEMS):
                if self.dcnt[q][i] > 0:
                    evs.append((self.dsem[q][i], 16 * self.dcnt[q][i]))
        for e in ENGS:
            waits = []
            for ev in evs:
                self._need(e, ev, waits)
            if waits:
                self.streams[e].append((waits, None, (), {}, None, None))

    def flush(self):
        self.barrier()
        nc = self.nc
        streams = self.streams
        self.streams = {e: [] for e in ENGS}
        targets = {e: set() for e in ENGS}
        for e in ENGS:
            for waits, meth, args, kw, kind, x in streams[e]:
                for (src, val) in waits:
                    if isinstance(src, str):
                        targets[src].add(val)
        value_of = {}
        for e in ENGS:
            for waits, meth, args, kw, kind, x in streams[e]:
                if kind == "E" and x in targets[e]:
                    self.eval_[e] += 1
                    value_of[(e, x)] = self.eval_[e]
        for e in ENGS:
            for t in targets[e]:
                assert (e, t) in value_of, ("wait target from an earlier flush", e, t)
        esem = self.esem
        with nc.Block() as block:
            def run(engname):
                def body(eng):
                    for waits, meth, args, kw, kind, x in streams[engname]:
                        for (src, val) in waits:
                            if isinstance(src, str):
                                eng.wait_ge(esem[src], value_of[(src, val)])
                            else:
                                eng.wait_ge(src, val)
                        if meth is not None:
                            ins = getattr(eng, meth)(*args, **kw)
                            if kind == "D":
                                ins.then_inc(x, 16)
                            elif (engname, x) in value_of:
                                ins.then_inc(esem[engname], 1)
                return body
            block.tensor(run("tensor"))
            block.vector(run("vector"))
            block.scalar(run("scalar"))
            block.gpsimd(run("gpsimd"))
            block.sync(run("sync"))


class Ring:
    def __init__(self, alloc, name, shape, dt, n):
        self.bufs = [alloc(name + str(i), shape, dt) for i in range(n)]
        self.keys = [name + str(i) for i in range(n)]
        self.i = 0

    def next(self):
        i = self.i
        self.i = (i + 1) % len(self.bufs)
        return self.bufs[i], self.keys[i]


def interleave(gens):
    gens = list(gens)
    while gens:
        for g in list(gens):
            try:
                next(g)
            except StopIteration:
                gens.remove(g)


def build(debug=False, stop_after=None, n_layers=DEPTH, only=None):
    nc = bass.Bass("TRN2", target_bir_lowering=False)
    dk = "ExternalOutput" if debug else "Internal"

    def din(name, shape, dt=F32):
        return nc.dram_tensor(name, list(shape), dt, kind="ExternalInput").ap()

    def dscr(name, shape, dt=F32):
        return nc.dram_tensor(name, list(shape), dt, kind=dk).ap()

    h0 = din("h0", [LP, D])
    w_in = din("w_in", [DEPTH, D, DIN])
    w_out = din("w_out", [DEPTH, D, D])
    w1 = din("w1", [DEPTH, NE, D, 256])
    w3 = din("w3", [DEPTH, NE, D, 256])
    w2 = din("w2", [DEPTH, NE, 256, D])
    wr = din("wr", [DEPTH, D, 36])
    brep = din("brep", [DEPTH, 128, 36])
    cwT = din("cwT", [DEPTH, 128, 48])
    alog = din("alog", [DEPTH, 128, 68])
    dtb = din("dtb", [DEPTH, 128, 68])
    ngr = din("ngr", [DEPTH, 128, 512])
    ln1g = din("ln1g", [DEPTH, 128, D])
    ln1b = din("ln1b", [DEPTH, 128, D])
    ln2g = din("ln2g", [DEPTH, 128, D])
    ln2b = din("ln2b", [DEPTH, 128, D])
    ropec = din("ropec", [128, LP])
    ropes = din("ropes", [128, LP])
    y = nc.dram_tensor("y", [SEQ, D], F32, kind="ExternalOutput").ap()

    h_d = dscr("h_d", [LP, D])
    qkvT_d = dscr("qkvT_d", [12, 128, LP])
    ropeT_d = dscr("ropeT_d", [13, 128, LP], BF16)
    z_d = dscr("z_d", [LP, 512])
    sv_d = dscr("sv_d", [LP, 512], BF16)
    sm_d = dscr("sm_d", [LP, 16])
    mixT_d = dscr("mixT_d", [8, 128, LP], BF16)

    top = ExitStack()
    P = Prog(nc, top)

    uid = [0]

    def uname(name):
        uid[0] += 1
        return "%s_u%d" % (name, uid[0])

    def stage_allocs(st):
        def sb(name, shape, dt=F32):
            return st.enter_context(nc.sbuf_tensor(uname(name), list(shape), dt))

        def ps(name, shape=(128, 512), dt=F32):
            P.excl.add(name)
            return st.enter_context(nc.psum_tensor(uname(name), list(shape), dt))
        return sb, ps

    csb, _ = stage_allocs(top)
    ident = csb("ident", [128, 128])
    identb = csb("identb", [128, 128], BF16)
    ones = csb("ones", [128, 128])
    negones = csb("negones", [128, 128])
    P.op("gpsimd", "memset", [], ["ident"], ident[:], 1.0)
    P.op("gpsimd", "affine_select", ["ident"], ["ident"], out=ident[:], in_=ident[:], pattern=[[-1, 128]],
         compare_op=ALU.is_equal, fill=0.0, base=0, channel_multiplier=1)
    P.op("gpsimd", "tensor_copy", ["ident"], ["identb"], out=identb[:], in_=ident[:])
    P.op("gpsimd", "memset", [], ["ones"], ones[:], 1.0)
    P.op("gpsimd", "memset", [], ["negones"], negones[:], -1.0)

    def ln_tile(sb_t, tkey, g_rep, b_rep, outt, okey, scr, skey, st2, st2key):
        P.op("scalar", "activation", [tkey], [skey, st2key], out=scr[:], in_=sb_t[:], func=AF.Identity,
             accum_out=st2[:, 0:1])
        yield
        P.op("vector", "tensor_scalar", [st2key], [st2key], out=st2[:, 1:2], in0=st2[:, 0:1], scalar1=-1.0 / D,
             scalar2=None, op0=ALU.mult)
        yield
        P.op("scalar", "activation", [tkey, st2key], [skey, st2key], out=scr[:], in_=sb_t[:], func=AF.Square,
             bias=st2[:, 1:2], scale=1.0, accum_out=st2[:, 2:3])
        yield
        P.op("vector", "tensor_scalar", [st2key], [st2key], out=st2[:, 3:4], in0=st2[:, 2:3], scalar1=1.0 / D,
             scalar2=1e-5, op0=ALU.mult, op1=ALU.add)
        yield
        P.op("scalar", "activation", [st2key], [st2key], out=st2[:, 3:4], in_=st2[:, 3:4], func=AF.Ln)
        P.op("scalar", "activation", [st2key], [st2key], out=st2[:, 3:4], in_=st2[:, 3:4], func=AF.Exp, scale=-0.5)
        yield
        P.op("vector", "tensor_scalar", [tkey, st2key], [okey], out=outt[:], in0=sb_t[:], scalar1=st2[:, 1:2],
             scalar2=st2[:, 3:4], op0=ALU.add, op1=ALU.mult)
        yield
        P.op("vector", "tensor_tensor", [okey, "lng"], [okey], out=outt[:], in0=outt[:], in1=g_rep[:], op=ALU.mult)
        yield
        P.op("vector", "tensor_tensor", [okey, "lnb"], [okey], out=outt[:], in0=outt[:], in1=b_rep[:], op=ALU.add)
        yield

    def stage_hT(src, hT, l_unused=None):
        with ExitStack() as st:
            sb, ps = stage_allocs(st)
            ht = Ring(sb, "ht_in", [128, D], F32, 2)
            pT = [ps("hT_ps%d" % i, [128, 1024]) for i in range(2)]
            for t in range(NT):
                a, ak = ht.next()
                P.dma("sync", [("h", t)], [ak], out=a[:], in_=src[t * 128:(t + 1) * 128, :])
                pt, pk = pT[t % 2], "hT_ps%d" % (t % 2)
                for c in range(8):
                    P.op("tensor", "transpose", [ak, "ident"], [pk], out=pt[:, c * 128:(c + 1) * 128],
                         in_=a[:, c * 128:(c + 1) * 128], identity=ident[:])
                eng = "vector" if t % 2 == 0 else "scalar"
                if eng == "vector":
                    P.op("vector", "tensor_copy", [pk], [("hT", t)], out=hT[:, :, t * 128:(t + 1) * 128],
                         in_=pt[:].rearrange("p (c t) -> p c t", c=8))
                else:
                    P.op("scalar", "copy", [pk], [("hT", t)], out=hT[:, :, t * 128:(t + 1) * 128],
                         in_=pt[:].rearrange("p (c t) -> p c t", c=8))
            P.flush()

    def stage_proj(l, hT):
        with ExitStack() as st:
            sb, ps = stage_allocs(st)
            NC_FM = 38 * 128
            W = sb("Wp", [128, 8, NC_FM + 1040], BF16)
            cosT = sb("cosT", [128, LP])
            sinT = sb("sinT", [128, LP])
            P.dma("sync", [], ["cosT"], out=cosT[:], in_=ropec[:, :])
            P.dma("sync", [], ["sinT"], out=sinT[:], in_=ropes[:, :])
            wl = w_in[l].rearrange("(c p) n -> p c n", p=128)

            wstg = Ring(sb, "pj_wst", [128, 8, 256], F32, 3)

            def wk(c0, n):
                return [("Wc", j) for j in range(c0 // 128, (c0 + n + 127) // 128)]

            def ld(dst0, src0, n, key):
                for o in range(0, n, 256):
                    nn_ = min(256, n - o)
                    sg, sgk = wstg.next()
                    P.dma("sync", [], [sgk], out=sg[:, :, 0:nn_], in_=wl[:, :, src0 + o:src0 + o + nn_])
                    P.op("gpsimd", "tensor_copy", [sgk], wk(dst0 + o, nn_), out=W[:, :, dst0 + o:dst0 + o + nn_], in_=sg[:, :, 0:nn_])

            def ld_perm(dst0, src0, nheads, key):
                for o in range(0, nheads, 4):
                    nh_ = min(4, nheads - o)
                    nn_ = nh_ * 64
                    sg, sgk = wstg.next()
                    P.dma("sync", [], [sgk], out=sg[:, :, 0:nn_], in_=wl[:, :, src0 + o * 64:src0 + o * 64 + nn_])
                    dv = W[:, :, dst0 + o * 64:dst0 + o * 64 + nn_].rearrange("p c (h two j) -> p c h two j", two=2, j=32)
                    sv = sg[:, :, 0:nn_].rearrange("p c (h two j) -> p c h two j", two=2, j=32)
                    for half in range(2):
                        P.op("gpsimd", "tensor_copy", [sgk], wk(dst0 + o * 64, nn_), out=dv[:, :, :, half, :], in_=sv[:, :, :, 1 - half, :])
            ld(0, 0, 1536, ("W", 0))
            ld(12 * 128, 2056, 512, ("W", 1))
            ld_perm(16 * 128, 2056, 8, ("W", 1))
            ld(20 * 128, 2568, 512, ("W", 2))
            ld_perm(24 * 128, 2568, 8, ("W", 2))
            ld(28 * 128, 3592, 512, ("W", 3))
            ld_perm(32 * 128, 3592, 8, ("W", 3))
            ld(36 * 128, 4104, 64, ("W", 4))
            ld(36 * 128 + 64, 4104, 64, ("W", 4))
            ld_perm(37 * 128, 4104, 1, ("W", 4))
            ld_perm(37 * 128 + 64, 4104, 1, ("W", 4))
            T0 = NC_FM
            ld(T0, 1536, 512, ("W", 5))
            ld(T0 + 512, 3080, 512, ("W", 5))
            ld(T0 + 1024, 2048, 8, ("W", 5))
            ld(T0 + 1032, 4168, 8, ("W", 5))
            wkeys = [("W", i) for i in range(6)]

            pbank = Ring(ps, "pj_ps", [128, 512], F32, 4)
            stg32 = Ring(sb, "pj_s32", [128, LP], F32, 2)
            stg16 = Ring(sb, "pj_s16", [128, LP], BF16, 2)
            tmp = Ring(sb, "pj_tmp", [128, 512], F32, 2)

            def wgrp(m):
                return [("Wc", m)]

            def mm_fm(m, n0, nn, pt, pk):
                for k in range(8):
                    P.op("tensor", "matmul", wgrp(m) + [("hT", i) for i in range(n0 // 128, (n0 + nn) // 128)], [pk],
                         pt[:, 0:nn], lhsT=W[:, k, m * 128:(m + 1) * 128], rhs=hT[:, k, n0:n0 + nn],
                         start=(k == 0), stop=(k == 7))
            for m in range(12):
                s, sk = stg32.next()
                for (n0, nn) in NTILES:
                    pt, pk = pbank.next()
                    mm_fm(m, n0, nn, pt, pk)
                    if (n0 // 512) % 2 == 0:
                        P.op("vector", "tensor_copy", [pk], [sk], out=s[:, n0:n0 + nn], in_=pt[:, 0:nn])
                    else:
                        P.op("scalar", "copy", [pk], [sk], out=s[:, n0:n0 + nn], in_=pt[:, 0:nn])
                P.dma("sync", [sk], [("qkvT", m)], out=qkvT_d[m, :, :], in_=s[:])
            rope_src = [12, 13, 14, 15, 20, 21, 22, 23, 28, 29, 30, 31, 36]
            rope_prm = [16, 17, 18, 19, 24, 25, 26, 27, 32, 33, 34, 35, 37]
            for r in range(13):
                s, sk = stg16.next()
                for (n0, nn) in NTILES:
                    pa, pak = pbank.next()
                    mm_fm(rope_src[r], n0, nn, pa, pak)
                    pb, pbk = pbank.next()
                    mm_fm(rope_prm[r], n0, nn, pb, pbk)
                    t1, t1k = tmp.next()
                    t2, t2k = tmp.next()
                    P.op("vector", "tensor_tensor", [pak, "cosT"], [t1k], out=t1[:, 0:nn], in0=pa[:, 0:nn],
                         in1=cosT[:, n0:n0 + nn], op=ALU.mult)
                    P.op("vector", "tensor_tensor", [pbk, "sinT"], [t2k], out=t2[:, 0:nn], in0=pb[:, 0:nn],
                         in1=sinT[:, n0:n0 + nn], op=ALU.mult)
                    P.op("gpsimd", "tensor_tensor", [t1k, t2k], [sk], out=s[:, n0:n0 + nn], in0=t1[:, 0:nn],
                         in1=t2[:, 0:nn], op=ALU.add)
                P.dma("sync", [sk], [("ropeT", r)], out=ropeT_d[r, :, :], in_=s[:])
            zst = Ring(sb, "pj_z", [128, 512], F32, 2)
            svst = Ring(sb, "pj_sv", [128, 512], BF16, 2)
            smst = Ring(sb, "pj_sm", [128, 16], F32, 2)
            for t in range(NT):
                for (c0, cn, kind) in ((T0, 512, "z"), (T0 + 512, 512, "sv"), (T0 + 1024, 16, "sm")):
                    pt, pk = pbank.next()
                    for k in range(8):
                        P.op("tensor", "matmul", wk(c0, cn) + [("hT", t)], [pk], pt[:, 0:cn],
                             lhsT=hT[:, k, t * 128:(t + 1) * 128], rhs=W[:, k, c0:c0 + cn], start=(k == 0), stop=(k == 7))
                    if kind == "z":
                        s, sk = zst.next()
                        P.op("scalar", "copy", [pk], [sk], out=s[:], in_=pt[:, 0:512])
                        P.dma("sync", [sk], [("z", t)], out=z_d[t * 128:(t + 1) * 128, :], in_=s[:])
                    elif kind == "sv":
                        s, sk = svst.next()
                        P.op("vector", "tensor_copy", [pk], [sk], out=s[:], in_=pt[:, 0:512])
                        P.dma("sync", [sk], [("sv", t)], out=sv_d[t * 128:(t + 1) * 128, :], in_=s[:])
                    else:
                        s, sk = smst.next()
                        P.op("vector", "tensor_copy", [pk], [sk], out=s[:], in_=pt[:, 0:16])
                        P.dma("sync", [sk], [("sm", t)], out=sm_d[t * 128:(t + 1) * 128, :], in_=s[:])
            P.flush()

    def stage_dn(l):
        with ExitStack() as st:
            sb, ps = stage_allocs(st)
            qT = sb("dn_qT", [128, 4, LP], BF16)
            kT = sb("dn_kT", [128, 4, LP], BF16)
            vT = sb("dn_vT", [128, 4, LP], BF16)
            cw = sb("dn_cw", [128, 48])
            P.dma("sync", [], ["cw"], out=cw[:], in_=cwT[l, :, :])
            with ExitStack() as st1:
                sb1, ps1 = stage_allocs(st1)
                xin = Ring(sb1, "dn_xin", [128, LP + 3], F32, 2)
                acc = Ring(sb1, "dn_acc", [128, LP], F32, 2)
                sq = Ring(sb1, "dn_sq", [128, LP], F32, 2)
                rn = Ring(sb1, "dn_rn", [128, 512], F32, 2)
                pss = Ring(ps1, "dn_ps", [128, 512], F32, 2)
                for m in range(12):
                    x, xk = xin.next()
                    P.op("gpsimd", "memset", [], [xk], x[:, 0:3], 0.0)
                    P.dma("sync", [("qkvT", m)], [xk], out=x[:, 3:LP + 3], in_=qkvT_d[m, :, :])
                    a, ak = acc.next()
                    P.op("vector", "tensor_scalar", [xk, "cw"], [ak], out=a[:], in0=x[:, 3:LP + 3],
                         scalar1=cw[:, m * 4 + 3:m * 4 + 4], scalar2=None, op0=ALU.mult)
                    for j in range(3):
                        P.op("vector", "scalar_tensor_tensor", [xk, "cw", ak], [ak], out=a[:],
                             in0=x[:, j:LP + j], scalar=cw[:, m * 4 + j:m * 4 + j + 1], in1=a[:], op0=ALU.mult, op1=ALU.add)
                    if m >= 8:
                        P.op("scalar", "activation", [ak], [("vT", m - 8)], out=vT[:, m - 8, :], in_=a[:], func=AF.Silu)
                        continue
                    P.op("scalar", "activation", [ak], [ak], out=a[:], in_=a[:], func=AF.Silu)
                    s, sk = sq.next()
                    P.op("gpsimd", "tensor_tensor", [ak], [sk], out=s[:], in0=a[:], in1=a[:], op=ALU.mult)
                    dst = qT if m < 4 else kT
                    dkey = ("qT", m) if m < 4 else ("kT", m - 4)
                    for (n0, nn) in NTILES:
                        pt, pk = pss.next()
                        P.op("tensor", "matmul", [sk, "ones"], [pk], pt[:, 0:nn], lhsT=ones[:], rhs=s[:, n0:n0 + nn],
                             start=True, stop=True)
                        r, rk = rn.next()
                        P.op("scalar", "activation", [pk], [rk], out=r[:, 0:nn], in_=pt[:, 0:nn], func=AF.Ln,
                             bias=1e-6, scale=1.0)
                        P.op("scalar", "activation", [rk], [rk], out=r[:, 0:nn], in_=r[:, 0:nn], func=AF.Exp, scale=-0.5)
                        P.op("vector", "scalar_tensor_tensor", [ak, rk], [dkey], out=dst[:, m % 4, n0:n0 + nn],
                             in0=a[:, n0:n0 + nn], scalar=(128.0 ** -0.5 if m < 4 else 1.0), in1=r[:, 0:nn],
                             op0=ALU.mult, op1=ALU.mult)
                P.flush()
            if DN_CUT == 1:
                return
            QK = [("qT", i) for i in range(4)]
            KK = [("kT", i) for i in range(4)]
            VK = [("vT", i) for i in range(4)]
            sm = sb("dn_sm", [128, NT, 16])
            P.dma("sync", [("sm", t) for t in range(NT)], ["smt"], out=sm[:],
                  in_=sm_d.rearrange("(t p) c -> p t c", p=128))
            alr = sb("dn_alr", [128, 68])
            dtr = sb("dn_dtr", [128, 68])
            P.dma("sync", [], ["alr"], out=alr[:], in_=alog[l, :, :])
            P.dma("sync", [], ["dtr"], out=dtr[:], in_=dtb[l, :, :])
            beta = sb("dn_beta", [128, NT, 4])
            g = sb("dn_g", [128, NT, 4])
            tmpg = sb("dn_tmpg", [128, NT, 4])
            v3 = lambda a: a[:].rearrange("p (t h) -> p t h", h=4)
            P.op("scalar", "activation", ["smt"], ["beta"], out=beta[:], in_=sm[:, :, 0:4], func=AF.Sigmoid)
            P.op("vector", "tensor_tensor", ["smt", "dtr"], ["tmpg"], out=tmpg[:], in0=sm[:, :, 4:8], in1=v3(dtr), op=ALU.add)
            P.op("scalar", "activation", ["tmpg"], ["tmpg"], out=tmpg[:], in_=tmpg[:], func=AF.Exp)
            P.op("scalar", "activation", ["tmpg"], ["tmpg"], out=tmpg[:], in_=tmpg[:], func=AF.Ln, bias=1.0, scale=1.0)
            P.op("scalar", "activation", ["alr"], ["alr"], out=alr[:], in_=alr[:], func=AF.Exp)
            P.op("vector", "scalar_tensor_tensor", ["tmpg", "alr"], ["g"], out=g[:], in0=tmpg[:], scalar=-1.0,
                 in1=v3(alr), op0=ALU.mult, op1=ALU.mult)
            U = sb("dn_U", [128, 128])
            Mst = sb("dn_Mst", [128, 4, 128])
            Mup = sb("dn_Mup", [128, 4, 128])
            P.op("gpsimd", "memset", [], ["U"], U[:], 1.0)
            P.op("gpsimd", "affine_select", ["U"], ["U"], out=U[:], in_=U[:], pattern=[[1, 128]],
                 compare_op=ALU.is_ge, fill=0.0, base=0, channel_multiplier=-1)
            P.op("gpsimd", "memset", [], ["Mst"], Mst[:], 0.0)
            P.op("gpsimd", "affine_select", ["Mst"], ["Mst"], out=Mst[:], in_=Mst[:], pattern=[[0, 4], [-1, 128]],
                 compare_op=ALU.is_ge, fill=NEG, base=-1, channel_multiplier=1)
            P.op("gpsimd", "memset", [], ["Mup"], Mup[:], 0.0)
            P.op("gpsimd", "affine_select", ["Mup"], ["Mup"], out=Mup[:], in_=Mup[:], pattern=[[0, 4], [1, 128]],
                 compare_op=ALU.is_ge, fill=NEG, base=0, channel_multiplier=-1)
            gc = sb("dn_gc", [128, 68])
            ngc = sb("dn_ngc", [128, 68])
            gl = sb("dn_gl", [128, 68])
            egc = sb("dn_egc", [128, 68])
            ekd = sb("dn_ekd", [128, 68])
            egl = sb("dn_egl", [128, 68])
            bgc = sb("dn_bgc", [128, 68])
            st_g = ExitStack()
            psg = st_g.enter_context(nc.psum_tensor(uname("dn_psg"), [128, 512], F32))
            g2 = g[:].rearrange("p t h -> p (t h)")
            P.op("tensor", "matmul", ["g", "U"], ["psg"], psg[:, 0:68], lhsT=U[:], rhs=g2, start=True, stop=True)
            P.op("tensor", "matmul", ["g", "ones"], ["psg"], psg[:, 128:196], lhsT=ones[:], rhs=g2, start=True, stop=True)
            P.op("vector", "tensor_copy", ["psg"], ["gc"], out=gc[:], in_=psg[:, 0:68])
            P.op("vector", "tensor_copy", ["psg"], ["gl"], out=gl[:], in_=psg[:, 128:196])
            P.op("vector", "tensor_scalar", ["gc"], ["ngc"], out=ngc[:], in0=gc[:], scalar1=-1.0, scalar2=None, op0=ALU.mult)
            P.op("scalar", "activation", ["gc"], ["egc"], out=egc[:], in_=gc[:], func=AF.Exp)
            P.op("scalar", "activation", ["gl"], ["egl"], out=egl[:], in_=gl[:], func=AF.Exp)
            P.op("vector", "tensor_tensor", ["gl", "gc"], ["ekd"], out=ekd[:], in0=gl[:], in1=gc[:], op=ALU.subtract)
            P.op("scalar", "activation", ["ekd"], ["ekd"], out=ekd[:], in_=ekd[:], func=AF.Exp)
            P.op("vector", "tensor_tensor", ["egc", "beta"], ["bgc"], out=bgc[:], in0=egc[:],
                 in1=beta[:].rearrange("p t h -> p (t h)"), op=ALU.mult)
            P.flush()
            st_g.close()
            if DN_CUT == 2:
                return

            NB = 4
            pA = [ps("dn_pA%d" % i) for i in range(2)]
            pB = [ps("dn_pB%d" % i) for i in range(2)]
            pS = [ps("dn_pS%d" % i) for i in range(2)]
            pTk = ps("dn_pTk")
            pTv = ps("dn_pTv")
            Dg_ = [Ring(sb, "dn_Dg%d_" % p_, [128, 4, 128], F32, 1) for p_ in range(2)]
            dec_ = [Ring(sb, "dn_dec%d_" % p_, [128, 4, 128], F32, 1) for p_ in range(2)]
            decT = Ring(sb, "dn_decT", [128, 4, 128], F32, NB)
            Abuf_ = [Ring(sb, "dn_A%d_" % p_, [128, 4, 128], F32, 2) for p_ in range(2)]
            Bbuf_ = [Ring(sb, "dn_B%d_" % p_, [128, 4, 128], F32, 2) for p_ in range(2)]
            Xbuf_ = [Ring(sb, "dn_X%d_" % p_, [128, 4, 128], F32, 2) for p_ in range(2)]
            bvb_ = [Ring(sb, "dn_bv%d_" % p_, [128, 4, 128], F32, 1) for p_ in range(2)]
            kbgb_ = [Ring(sb, "dn_kbg%d_" % p_, [128, 4, 128], F32, 1) for p_ in range(2)]
            u4b = Ring(sb, "dn_u4", [128, 4, 128], F32, NB)
            wT4b = Ring(sb, "dn_wT4", [128, 4, 128], BF16, NB)
            aqk4b = Ring(sb, "dn_aqk", [128, 4, 128], BF16, NB)
            kd4b = Ring(sb, "dn_kd4", [128, 4, 128], BF16, NB)

            def slot(ring, t):
                i_ = t % len(ring.bufs)
                return ring.bufs[i_], ring.keys[i_]
            S4 = sb("dn_S4", [128, 4, 128])
            S4b = sb("dn_S4b", [128, 4, 128], BF16)
            P.op("vector", "memset", [], ["S4"], S4[:], 0.0)
            P.op("gpsimd", "memset", [], ["S4b"], S4b[:], 0.0)
            vn4b = Ring(sb, "dn_vn4", [128, 4, 128], BF16, 2)
            qs4b = Ring(sb, "dn_qs4", [128, 4, 128], F32, 2)
            o4b = Ring(sb, "dn_o4", [128, 4, 128], F32, 2)
            ztb = Ring(sb, "dn_zt", [128, 512], F32, 2)
            ogb = Ring(sb, "dn_og", [128, 512], BF16, 2)
            ssb = Ring(sb, "dn_ss", [128, 8], F32, 2)
            junk = sb("dn_junk", [128, 128])
            odT = sb("dn_odT", [128, 4, LP], BF16)
            ng = sb("dn_ng", [128, 512])
            P.dma("sync", [], ["ng"], out=ng[:], in_=ngr[l, :, :])
            prep_out = {}

            def F(ap):
                return ap[:].rearrange("p h c -> p (h c)")

            def prep(t):
                par = t % 2
                Dg, dec, Abuf, Bbuf, Xbuf, bvb, kbgb = Dg_[par], dec_[par], Abuf_[par], Bbuf_[par], Xbuf_[par], bvb_[par], kbgb_[par]
                cs = slice(t * 128, (t + 1) * 128)
                col = lambda a, h: a[:, t * 4 + h:t * 4 + h + 1]
                dg, dgk = Dg.next()
                for h in range(4):
                    P.op("gpsimd", "tensor_scalar", ["ident", "gc"], [dgk], out=dg[:, h, :], in0=ident[:],
                         scalar1=col(gc, h), scalar2=None, op0=ALU.mult)
                a_ps, ak_ps = pA[par], "dn_pA%d" % par
                b_ps, bk_ps = pB[par], "dn_pB%d" % par
                x_ps, xk_ps = b_ps, bk_ps
                P.op("tensor", "matmul", [dgk, "negones"], [ak_ps], a_ps[:], lhsT=negones[:], rhs=F(dg), start=True, stop=False)
                P.op("tensor", "matmul", ["Mst", "ident"], [ak_ps], a_ps[:], lhsT=ident[:], rhs=F(Mst), start=False, stop=True)
                P.op("tensor", "matmul", [dgk, "ones"], [bk_ps], b_ps[:], lhsT=ones[:], rhs=F(dg), start=True, stop=False)
                P.op("tensor", "matmul", ["Mup", "ident"], [bk_ps], b_ps[:], lhsT=ident[:], rhs=F(Mup), start=False, stop=True)
                de, dek = dec.next()
                deT, deTk = slot(decT, t)
                for h in range(4):
                    P.op("scalar", "activation", [ak_ps, "gc"], [dek], out=de[:, h, :], in_=a_ps[:, h * 128:(h + 1) * 128],
                         func=AF.Exp, bias=col(gc, h), scale=1.0)
                    P.op("scalar", "activation", [bk_ps, "ngc"], [deTk], out=deT[:, h, :], in_=b_ps[:, h * 128:(h + 1) * 128],
                         func=AF.Exp, bias=col(ngc, h), scale=1.0)
                yield
                if PREP_CUT == 1:
                    return
                for h in range(4):
                    P.op("tensor", "matmul", KK, [xk_ps], x_ps[:, h * 128:(h + 1) * 128], lhsT=kT[:, h, cs], rhs=kT[:, h, cs],
                         start=True, stop=True)
                A, Ak = Abuf.next()
                for h in range(4):
                    P.op("vector", "scalar_tensor_tensor", [xk_ps, "beta", dek], [Ak], out=A[:, h, :],
                         in0=x_ps[:, h * 128:(h + 1) * 128], scalar=beta[:, t, h:h + 1], in1=de[:, h, :],
                         op0=ALU.mult, op1=ALU.mult)
                for h in range(4):
                    P.op("tensor", "transpose", [Ak, "ident"], [ak_ps], out=a_ps[:, h * 128:(h + 1) * 128], in_=A[:, h, :],
                         identity=ident[:])
                Bm, Bk = Bbuf.next()
                P.op("scalar", "copy", [ak_ps], [Bk], out=F(Bm), in_=a_ps[:])
                X, Xk = Xbuf.next()
                for h in range(4):
                    P.op("gpsimd", "tensor_tensor", ["ident", Bk], [Xk], out=X[:, h, :], in0=ident[:], in1=Bm[:, h, :],
                         op=ALU.subtract)
                if PREP_CUT == 2:
                    return
                for h in range(4):
                    P.op("tensor", "matmul", KK + ["identb"], ["dn_pTk"], pTk[:, h * 128:(h + 1) * 128], lhsT=kT[:, h, cs],
                         rhs=identb[:], start=True, stop=True)
                    P.op("tensor", "matmul", VK + ["identb"], ["dn_pTv"], pTv[:, h * 128:(h + 1) * 128], lhsT=vT[:, h, cs],
                         rhs=identb[:], start=True, stop=True)
                kbg, kbgk = kbgb.next()
                kd4, kd4k = slot(kd4b, t)
                bv, bvk = bvb.next()
                if PREP_CUT == 31:
                    return
                for h in range(4):
                    P.op("vector", "tensor_scalar", ["dn_pTk", "bgc"], [kbgk], out=kbg[:, h, :], in0=pTk[:, h * 128:(h + 1) * 128],
                         scalar1=col(bgc, h), scalar2=None, op0=ALU.mult)
                    if PREP_CUT == 32:
                        continue
                    P.op("scalar", "activation", ["dn_pTk", "ekd"], [kd4k], out=kd4[:, h, :], in_=pTk[:, h * 128:(h + 1) * 128],
                         func=AF.Copy, scale=col(ekd, h))
                    if PREP_CUT == 33:
                        continue
                    P.op("vector", "tensor_scalar", ["dn_pTv", "beta"], [bvk], out=bv[:, h, :],
                         in0=pTv[:, h * 128:(h + 1) * 128], scalar1=beta[:, t, h:h + 1], scalar2=None, op0=ALU.mult)
                if PREP_CUT in (32, 33):
                    return
                yield
                if PREP_CUT == 3:
                    return
                for n in range(1, 7):
                    A2, A2k = Abuf.next()
                    for h in range(4):
                        P.op("tensor", "matmul", [Ak, Bk], [ak_ps], a_ps[:, h * 128:(h + 1) * 128], lhsT=Bm[:, h, :], rhs=A[:, h, :],
                             start=True, stop=True)
                    if n < 6:
                        B2, B2k = Bbuf.next()
                        for h in range(4):
                            P.op("tensor", "matmul", [Ak, Bk], [bk_ps], b_ps[:, h * 128:(h + 1) * 128], lhsT=A[:, h, :], rhs=Bm[:, h, :],
                                 start=True, stop=True)
                    P.op("scalar", "copy", [ak_ps], [A2k], out=F(A2), in_=a_ps[:])
                    if n < 6:
                        P.op("vector", "tensor_copy", [bk_ps], [B2k], out=F(B2), in_=b_ps[:])
                    for h in range(4):
                        P.op("tensor", "matmul", [A2k, Xk], [ak_ps], a_ps[:, h * 128:(h + 1) * 128], lhsT=A2[:, h, :], rhs=X[:, h, :],
                             start=True, stop=True)
                    X2, X2k = Xbuf.next()
                    P.op("vector", "tensor_tensor", [ak_ps, Xk], [X2k], out=F(X2), in0=a_ps[:], in1=F(X), op=ALU.add)
                    A, Ak = A2, A2k
                    if n < 6:
                        Bm, Bk = B2, B2k
                    X, Xk = X2, X2k
                    yield
                if PREP_CUT == 4:
                    return
                u4, u4k = slot(u4b, t)
                wT4, wT4k = slot(wT4b, t)
                aqk, aqkk = slot(aqk4b, t)
                for h in range(4):
                    P.op("tensor", "matmul", [Xk, bvk], [ak_ps], a_ps[:, h * 128:(h + 1) * 128], lhsT=X[:, h, :], rhs=bv[:, h, :],
                         start=True, stop=True)
                    P.op("tensor", "matmul", [Xk, kbgk], [bk_ps], b_ps[:, h * 128:(h + 1) * 128], lhsT=kbg[:, h, :], rhs=X[:, h, :],
                         start=True, stop=True)
                P.op("scalar", "copy", [ak_ps], [u4k], out=F(u4), in_=a_ps[:])
                P.op("scalar", "copy", [bk_ps], [wT4k], out=F(wT4), in_=b_ps[:])
                for h in range(4):
                    P.op("tensor", "matmul", KK + QK, [ak_ps], a_ps[:, h * 128:(h + 1) * 128], lhsT=kT[:, h, cs], rhs=qT[:, h, cs],
                         start=True, stop=True)
                P.op("vector", "tensor_tensor", [ak_ps, deTk], [aqkk], out=F(aqk), in0=a_ps[:], in1=F(deT), op=ALU.mult)
                prep_out[t] = (u4, u4k, wT4, wT4k, aqk, aqkk, kd4, kd4k)
                yield

            def scan(ts):
                for t in ts:
                    u4, u4k, wT4, wT4k, aqk, aqkk, kd4, kd4k = prep_out.pop(t)
                    cs = slice(t * 128, (t + 1) * 128)
                    col = lambda a, h: a[:, t * 4 + h:t * 4 + h + 1]
                    p1, p1k = pS[0], "dn_pS0"
                    p2, p2k = pS[1], "dn_pS1"
                    for h in range(4):
                        P.op("tensor", "matmul", [wT4k, "S4b"], [p1k], p1[:, h * 128:(h + 1) * 128], lhsT=wT4[:, h, :], rhs=S4b[:, h, :],
                             start=True, stop=True)
                    for h in range(4):
                        P.op("tensor", "matmul", QK + ["S4b"], [p2k], p2[:, h * 128:(h + 1) * 128], lhsT=qT[:, h, cs], rhs=S4b[:, h, :],
                             start=True, stop=True)
                    vn, vnk = vn4b.next()
                    P.op("vector", "tensor_tensor", [u4k, p1k], [vnk], out=F(vn), in0=F(u4), in1=p1[:], op=ALU.subtract)
                    qs, qsk = qs4b.next()
                    for h in range(4):
                        P.op("scalar", "activation", [p2k, "egc"], [qsk], out=qs[:, h, :], in_=p2[:, h * 128:(h + 1) * 128],
                             func=AF.Copy, scale=col(egc, h))
                    yield
                    for h in range(4):
                        P.op("tensor", "matmul", [aqkk, vnk], [p1k], p1[:, h * 128:(h + 1) * 128], lhsT=aqk[:, h, :], rhs=vn[:, h, :],
                             start=True, stop=True)
                    for h in range(4):
                        P.op("tensor", "matmul", [kd4k, vnk], [p2k], p2[:, h * 128:(h + 1) * 128], lhsT=kd4[:, h, :], rhs=vn[:, h, :],
                             start=True, stop=True)
                    o4, o4k = o4b.next()
                    P.op("vector", "tensor_tensor", [p1k, qsk], [o4k], out=F(o4), in0=p1[:], in1=F(qs), op=ALU.add)
                    for h in range(4):
                        P.op("vector", "scalar_tensor_tensor", ["S4", "egl", p2k], ["S4"], out=S4[:, h, :], in0=S4[:, h, :],
                             scalar=col(egl, h), in1=p2[:, h * 128:(h + 1) * 128], op0=ALU.mult, op1=ALU.add)
                    P.op("gpsimd", "tensor_copy", ["S4"], ["S4b"], out=F(S4b), in_=F(S4))
                    yield
                    zt, ztk = ztb.next()
                    P.dma("sync", [("z", t)], [ztk], out=zt[:], in_=z_d[t * 128:(t + 1) * 128, :])
                    ss, ssk = ssb.next()
                    for h in range(4):
                        P.op("scalar", "activation", [o4k], ["dn_junk", ssk], out=junk[:], in_=o4[:, h, :], func=AF.Square,
                             accum_out=ss[:, h:h + 1])
                    P.op("vector", "tensor_scalar", [ssk], [ssk], out=ss[:, 4:8], in0=ss[:, 0:4], scalar1=1.0 / 128, scalar2=1e-6,
                         op0=ALU.mult, op1=ALU.add)
                    P.op("scalar", "activation", [ssk], [ssk], out=ss[:, 4:8], in_=ss[:, 4:8], func=AF.Sqrt)
                    P.op("vector", "reciprocal", [ssk], [ssk], out=ss[:, 4:8], in_=ss[:, 4:8])
                    P.op("scalar", "activation", [ztk], [ztk], out=zt[:], in_=zt[:], func=AF.Silu)
                    P.op("gpsimd", "tensor_tensor", [ztk, "ng"], [ztk], out=zt[:], in0=zt[:], in1=ng[:], op=ALU.mult)
                    og, ogk = ogb.next()
                    for h in range(4):
                        P.op("gpsimd", "tensor_scalar", [o4k, ssk], [o4k], out=o4[:, h, :], in0=o4[:, h, :],
                             scalar1=ss[:, 4 + h:5 + h], scalar2=None, op0=ALU.mult)
                        P.op("gpsimd", "tensor_tensor", [o4k, ztk], [ogk], out=og[:, h * 128:(h + 1) * 128], in0=o4[:, h, :],
                             in1=zt[:, h * 128:(h + 1) * 128], op=ALU.mult)
                    for h in range(4):
                        P.op("tensor", "matmul", [ogk, "identb"], ["dn_pTk"], pTk[:, h * 128:(h + 1) * 128],
                             lhsT=og[:, h * 128:(h + 1) * 128], rhs=identb[:], start=True, stop=True)
                    P.op("scalar", "copy", ["dn_pTk"], [("odT", t)], out=odT[:, :, cs],
                         in_=pTk[:, 0:512].rearrange("p (h c) -> p h c", h=4))
                    yield

            for s_ in range(0, NT + 2, 2):
                if DN_CUT == 3 and s_ >= 2:
                    break
                gens = [prep(t) for t in (s_, s_ + 1) if t < NT]
                prev = [t for t in (s_ - 2, s_ - 1) if 0 <= t < NT]
                if prev:
                    gens.append(scan(prev))
                interleave(gens)
            for h in range(4):
                P.dma("sync", [("odT", t) for t in range(NT)], [("mixT", h)], out=mixT_d[h, :, :], in_=odT[:, h, :])
            P.flush()

    def stage_dsa(l):
        with ExitStack() as st:
            sb, ps = stage_allocs(st)
            saq = sb("sa_q", [128, 4, LP], BF16)
            sak = sb("sa_k", [128, 4, LP], BF16)
            iq = sb("sa_iq", [128, 4, LP], BF16)
            ik = sb("sa_ik", [128, LP], BF16)
            for c in range(4):
                P.dma("sync", [("ropeT", c)], ["saq"], out=saq[:, c, :], in_=ropeT_d[c, :, :])
                P.dma("sync", [("ropeT", 4 + c)], ["sak"], out=sak[:, c, :], in_=ropeT_d[4 + c, :, :])
                P.dma("sync", [("ropeT", 8 + c)], ["iq"], out=iq[:, c, :], in_=ropeT_d[8 + c, :, :])
            P.dma("sync", [("ropeT", 12)], ["ik"], out=ik[:], in_=ropeT_d[12, :, :])
            va = sb("sa_va", [128, NT, 8, 65], BF16)
            P.op("gpsimd", "memset", [], ["va"], va[:], 1.0)
            for t in range(NT):
                P.dma("sync", [("sv", t)], ["va"], out=va[:, t, :, 0:64],
                      in_=sv_d[t * 128:(t + 1) * 128, :].rearrange("p (h d) -> p h d", d=64))
            sm = sb("sa_sm", [128, NT, 16])
            P.dma("sync", [("sm", t) for t in range(NT)], ["sasm"], out=sm[:], in_=sm_d.rearrange("(t p) c -> p t c", p=128))
            cmask = sb("sa_cmask", [128, 128], BF16)
            P.op("gpsimd", "memset", [], ["cmask"], cmask[:], 0.0)
            P.op("gpsimd", "affine_select", ["cmask"], ["cmask"], out=cmask[:], in_=cmask[:], pattern=[[-1, 128]],
                 compare_op=ALU.is_ge, fill=NEG, base=0, channel_multiplier=1)
            osT = sb("sa_osT", [128, 4, LP], BF16)
            sc_ps = Ring(ps, "sa_scps", [128, 512], F32, 2)
            s_ps = Ring(ps, "sa_sps", [128, 512], F32, 3)
            o_ps = [ps("sa_ops%d" % i, [128, 512]) for i in range(2)]
            pTb = ps("sa_pTb")
            relu_b = Ring(sb, "sa_relu", [128, 512], F32, 3)
            accb = Ring(sb, "sa_acc", [128, LP], F32, 2)
            mbb = Ring(sb, "sa_mb", [128, LP], BF16, 4)
            junkb = Ring(sb, "sa_junk", [128, LP], BF16, 2)
            ptb = Ring(sb, "sa_pt", [128, 512], BF16, 3)
            stt = Ring(sb, "sa_st", [128, 8], F32, 2)
            wtb = Ring(sb, "sa_wt", [128, NIT + 2], F32, 2)
            ftab = sb("sa_ftab", [128, NIT + 2])
            for n_ in range(NIT + 2):
                P.op("gpsimd", "memset", [], ["ftab"], ftab[:, n_:n_ + 1], 2.0 ** (-n_))
            osb = Ring(sb, "sa_os", [128, 512], BF16, 2)
            rdb = Ring(sb, "sa_rd", [128, 8], F32, 2)

            def hrows(h):
                return slice(0, 64) if h % 2 == 0 else slice(64, 128)

            mb_of = {}

            def scores(i):
                nk = 128 * (i + 1)
                qs_ = slice(i * 128, (i + 1) * 128)
                acc, acck = accb.next()
                for h in range(8):
                    for k0 in range(0, nk, 512):
                        kn = min(512, nk - k0)
                        pt, pk = sc_ps.next()
                        P.op("tensor", "matmul", ["iq", "ik"], [pk], pt[:, 0:kn], lhsT=iq[hrows(h), h // 2, qs_],
                             rhs=ik[hrows(h), k0:k0 + kn], start=True, stop=True)
                        if h == 0:
                            r, rk = relu_b.next()
                            P.op("scalar", "activation", [pk], [rk], out=r[:, 0:kn], in_=pt[:, 0:kn], func=AF.Relu)
                            P.op("vector", "tensor_scalar", [rk, "sasm"], [acck], out=acc[:, k0:k0 + kn], in0=r[:, 0:kn],
                                 scalar1=sm[:, i, 8:9], scalar2=None, op0=ALU.mult)
                        else:
                            r, rk = relu_b.next()
                            P.op("scalar", "activation", [pk], [rk], out=r[:, 0:kn], in_=pt[:, 0:kn], func=AF.Relu)
                            P.op("vector", "scalar_tensor_tensor", [rk, "sasm", acck], [acck], out=acc[:, k0:k0 + kn], in0=r[:, 0:kn],
                                 scalar=sm[:, i, 8 + h:9 + h], in1=acc[:, k0:k0 + kn], op0=ALU.mult, op1=ALU.add)
                    yield
                P.op("gpsimd", "affine_select", [acck], [acck], out=acc[:, i * 128:nk], in_=acc[:, i * 128:nk], pattern=[[-1, 128]],
                     compare_op=ALU.is_ge, fill=-1e30, base=0, channel_multiplier=1)
                mb, mbk = mbb.next()
                s8, s8k = stt.next()
                if nk <= KTOP:
                    P.op("vector", "memset", [], [s8k], s8[:, 0:1], -1e29)
                else:
                    jk, jkk = junkb.next()
                    P.op("vector", "tensor_reduce", [acck], [s8k], out=s8[:, 5:6], in_=acc[:, 0:nk], axis=mybir.AxisListType.X, op=ALU.max)
                    P.op("vector", "tensor_reduce", [acck], [s8k], out=s8[:, 0:1], in_=acc[:, 0:i * 128], axis=mybir.AxisListType.X, op=ALU.min)
                    P.op("vector", "tensor_tensor", [s8k], [s8k], out=s8[:, 1:2], in0=s8[:, 5:6], in1=s8[:, 0:1], op=ALU.subtract)
                    P.op("vector", "tensor_scalar", [s8k], [s8k], out=s8[:, 1:2], in0=s8[:, 1:2], scalar1=1.0001, scalar2=1e-6,
                         op0=ALU.mult, op1=ALU.add)
                    wt, wtk = wtb.next()
                    P.op("vector", "tensor_scalar", ["ftab", s8k], [wtk], out=wt[:], in0=ftab[:], scalar1=s8[:, 1:2], scalar2=None,
                         op0=ALU.mult)
                    P.op("vector", "tensor_tensor", [s8k, wtk], [s8k], out=s8[:, 2:3], in0=s8[:, 0:1], in1=wt[:, 1:2], op=ALU.add)
                    for it in range(1, NIT + 1):
                        P.op("vector", "tensor_scalar", [acck, s8k], [jkk, s8k], out=jk[:, 0:nk], in0=acc[:, 0:nk], scalar1=s8[:, 2:3],
                             scalar2=None, op0=ALU.is_ge, op1=ALU.add, accum_out=s8[:, 3:4])
                        P.op("vector", "tensor_scalar", [s8k], [s8k], out=s8[:, 4:5], in0=s8[:, 3:4], scalar1=KTOP - 0.5, scalar2=0.5,
                             op0=ALU.is_ge, op1=ALU.subtract)
                        P.op("vector", "scalar_tensor_tensor", [s8k, wtk], [s8k], out=s8[:, 2:3], in0=s8[:, 4:5], scalar=wt[:, it:it + 1],
                             in1=s8[:, 2:3], op0=ALU.mult, op1=ALU.add)
                        yield
                    P.op("vector", "tensor_tensor", [s8k, wtk], [s8k], out=s8[:, 0:1], in0=s8[:, 2:3], in1=wt[:, NIT + 1:NIT + 2],
                         op=ALU.subtract)
                P.op("vector", "tensor_scalar", [acck, s8k], [mbk], out=mb[:, 0:nk], in0=acc[:, 0:nk], scalar1=s8[:, 0:1], scalar2=NEG,
                     op0=ALU.is_lt, op1=ALU.mult)
                mb_of[i] = (mb, mbk)
                yield

            def attention(i):
                mb, mbk = mb_of.pop(i)
                qs_ = slice(i * 128, (i + 1) * 128)
                groups = [(kt, hg) for kt in range(i + 1) for hg in range(2)]
                pend = None
                for g in groups + [None]:
                    cur = None
                    if g is not None:
                        kt, hg = g
                        ks_ = slice(kt * 128, (kt + 1) * 128)
                        sp, spk = s_ps.next()
                        for pair in ((0, 2), (1, 3)):
                            for hh in pair:
                                h = hg * 4 + hh
                                P.op("tensor", "matmul", ["sak", "saq"], [spk], sp[:, hh * 128:(hh + 1) * 128],
                                     lhsT=sak[hrows(h), h // 2, ks_], rhs=saq[hrows(h), h // 2, qs_], start=(hh == 0), stop=False,
                                     skip_group_check=True)
                            for hh in pair:
                                P.op("tensor", "matmul", [mbk, "identb"], [spk], sp[:, hh * 128:(hh + 1) * 128],
                                     lhsT=mb[:, ks_], rhs=identb[:], start=False, stop=True, skip_group_check=True)
                        pt, ptk = ptb.next()
                        P.op("scalar", "activation", [spk], [ptk], out=pt[:], in_=sp[:], func=AF.Exp, scale=0.125)
                        cur = (kt, hg, pt, ptk)
                    if pend is not None:
                        kt, hg, pt, ptk = pend
                        for hh in range(4):
                            h = hg * 4 + hh
                            P.op("tensor", "matmul", [ptk, "va"], ["sa_ops%d" % hg], o_ps[hg][:, hh * 65:(hh + 1) * 65],
                                 lhsT=pt[:, hh * 128:(hh + 1) * 128], rhs=va[:, kt, h, :], start=(kt == 0 and hh == 0), stop=(kt == i),
                                 skip_group_check=True)
                    pend = cur
                    yield
                rd, rdk = rdb.next()
                for hg in range(2):
                    P.op("vector", "reciprocal", ["sa_ops%d" % hg], [rdk], out=rd[:, hg * 4:(hg + 1) * 4],
                         in_=o_ps[hg][:, 0:260].rearrange("p (h d) -> p h d", d=65)[:, :, 64])
                os_, osk = osb.next()
                for h in range(8):
                    hg, hh = h // 4, h % 4
                    P.op("vector", "tensor_scalar", ["sa_ops%d" % hg, rdk], [osk], out=os_[:, h * 64:(h + 1) * 64],
                         in0=o_ps[hg][:, hh * 65:hh * 65 + 64], scalar1=rd[:, h:h + 1], scalar2=None, op0=ALU.mult)
                for c in range(4):
                    P.op("tensor", "matmul", [osk, "identb"], ["sa_pTb"], pTb[:, c * 128:(c + 1) * 128],
                         lhsT=os_[:, c * 128:(c + 1) * 128], rhs=identb[:], start=True, stop=True)
                P.op("scalar", "copy", ["sa_pTb"], [("osT", i)], out=osT[:, :, qs_],
                     in_=pTb[:, 0:512].rearrange("p (h c) -> p h c", h=4))
                yield

            def att_chain(ts):
                for t_ in ts:
                    yield from attention(t_)

            for i in range(0, NT + 2, 2):
                gens = [scores(t_) for t_ in (i, i + 1) if t_ < NT]
                prev = [t_ for t_ in (i - 2, i - 1) if 0 <= t_ < NT]
                if prev:
                    gens.append(att_chain(prev))
                interleave(gens)
            for c in range(4):
                P.dma("sync", [("osT", t) for t in range(NT)], [("mixT", 4 + c)], out=mixT_d[4 + c, :, :], in_=osT[:, c, :])
            P.flush()

    def stage_mix_ln1(l, src, hT, comb):
        with ExitStack() as st:
            sb, ps = stage_allocs(st)
            mixT = sb("mx_mixT", [128, 8, LP], BF16)
            for c in range(8):
                P.dma("sync", [("mixT", c)], ["mixT"], out=mixT[:, c, :], in_=mixT_d[c, :, :])
            wo = sb("mx_wo", [128, 8, D], BF16)
            wol = w_out[l].rearrange("(c p) n -> p c n", p=128)
            for c in range(8):
                P.dma("gpsimd", [], ["wo"], out=wo[:, c, :], in_=wol[:, c, :])
            g_rep = sb("mx_g", [128, D])
            b_rep = sb("mx_b", [128, D])
            P.dma("sync", [], ["lng"], out=g_rep[:], in_=ln1g[l, :, :])
            P.dma("sync", [], ["lnb"], out=b_rep[:], in_=ln1b[l, :, :])
            wrs = sb("mx_wr", [128, 8, 36])
            P.dma("sync", [], ["wrs"], out=wrs[:], in_=wr[l].rearrange("(c p) n -> p c n", p=128))
            br = sb("mx_br", [128, 36])
            P.dma("sync", [], ["br"], out=br[:], in_=brep[l, :, :])
            hin = Ring(sb, "mx_hin", [128, D], F32, 2)
            tb = Ring(sb, "mx_t", [128, D], F32, 2)
            ob = Ring(sb, "mx_o", [128, D], F32, 2)
            scr = sb("mx_scr", [128, D])
            stb = Ring(sb, "mx_st", [128, 4], F32, 2)
            hTf = Ring(sb, "mx_hTf", [128, 8, 128], F32, 2)
            pM = [ps("mx_pM%d" % i, [128, 1024]) for i in range(2)]
            pT = [ps("mx_pT%d" % i, [128, 1024]) for i in range(1)]
            pLs = [ps("mx_pL%d" % i, [128, 512]) for i in range(2)]
            rt = Ring(sb, "mx_rt", [128, 160], F32, 2)
            def mtile(t):
                cs = slice(t * 128, (t + 1) * 128)
                pL, pLk = pLs[t % 2], "mx_pL%d" % (t % 2)
                pm, pmk = pM[t % 2], "mx_pM%d" % (t % 2)
                for n in range(2):
                    for c in range(8):
                        P.op("tensor", "matmul", ["mixT", "wo"], [pmk], pm[:, n * 512:(n + 1) * 512], lhsT=mixT[:, c, cs],
                             rhs=wo[:, c, n * 512:(n + 1) * 512], start=(c == 0), stop=(c == 7))
                hi_, hik = hin.next()
                P.dma("sync", [("h", t)], [hik], out=hi_[:], in_=src[t * 128:(t + 1) * 128, :])
                tt, ttk = tb.next()
                P.op("vector", "scalar_tensor_tensor", [hik, pmk], [ttk], out=tt[:], in0=hi_[:], scalar=ALPHA, in1=pm[:],
                     op0=ALU.mult, op1=ALU.add)
                o, ok = ob.next()
                s4, s4k = stb.next()
                yield
                yield from ln_tile(tt, ttk, g_rep, b_rep, o, ok, scr, "mx_scr", s4, s4k)
                P.dma("sync", [ok], [("h", t)], out=h_d[t * 128:(t + 1) * 128, :], in_=o[:])
                yield
                pt, ptk = pT[0], "mx_pT0"
                for c in range(8):
                    P.op("tensor", "transpose", [ok, "ident"], [ptk], out=pt[:, c * 128:(c + 1) * 128], in_=o[:, c * 128:(c + 1) * 128],
                         identity=ident[:])
                hf, hfk = hTf.next()
                P.op("scalar", "copy", [ptk], [hfk], out=hf[:].rearrange("p c t -> p (c t)"), in_=pt[:])
                P.op("gpsimd", "tensor_copy", [hfk], [("hT", t)], out=hT[:, :, cs], in_=hf[:])
                yield
                for c in range(8):
                    P.op("tensor", "matmul", [hfk, "wrs"], [pLk], pL[:, 0:36], lhsT=hf[:, c, :], rhs=wrs[:, c, :],
                         start=(c == 0), stop=(c == 7))
                yield
                r, rk = rt.next()
                P.op("vector", "tensor_tensor", [pLk, "br"], [rk], out=r[:, 0:36], in0=pL[:, 0:36], in1=br[:], op=ALU.add)
                P.op("vector", "tensor_reduce", [rk], [rk], out=r[:, 148:149], in_=r[:, 0:4], axis=mybir.AxisListType.X, op=ALU.max)
                P.op("vector", "tensor_scalar", [rk], [rk], out=r[:, 149:150], in0=r[:, 148:149], scalar1=-1.0, scalar2=None, op0=ALU.mult)
                P.op("scalar", "activation", [rk], [rk], out=r[:, 36:40], in_=r[:, 0:4], func=AF.Exp, bias=r[:, 149:150], scale=1.0,
                     accum_out=r[:, 150:151])
                P.op("vector", "reciprocal", [rk], [rk], out=r[:, 151:152], in_=r[:, 150:151])
                P.op("vector", "tensor_scalar", [rk], [rk], out=r[:, 44:48], in0=r[:, 0:4], scalar1=r[:, 148:149], scalar2=None,
                     op0=ALU.is_ge)
                P.op("vector", "tensor_scalar", [rk], [rk], out=r[:, 48:52], in0=r[:, 44:48], scalar1=-1.0, scalar2=1e30,
                     op0=ALU.add, op1=ALU.mult)
                for gq in range(4):
                    P.op("vector", "tensor_scalar", [rk], [rk], out=r[:, 52 + gq * 8:60 + gq * 8], in0=r[:, 4 + gq * 8:12 + gq * 8],
                         scalar1=r[:, 48 + gq:49 + gq], scalar2=None, op0=ALU.add)
                yield
                P.op("vector", "max", [rk], [rk], out=r[:, 36:44], in_=r[:, 52:84])
                P.op("vector", "tensor_scalar", [rk], [rk], out=r[:, 84:116], in0=r[:, 52:84], scalar1=r[:, 36:37], scalar2=None,
                     op0=ALU.is_equal)
                P.op("vector", "tensor_scalar", [rk], [rk], out=r[:, 116:148], in0=r[:, 52:84], scalar1=r[:, 37:38], scalar2=None,
                     op0=ALU.is_equal)
                P.op("vector", "tensor_tensor", [rk], [rk], out=r[:, 152:153], in0=r[:, 37:38], in1=r[:, 36:37], op=ALU.subtract)
                yield
                P.op("scalar", "activation", [rk], [rk], out=r[:, 153:154], in_=r[:, 152:153], func=AF.Exp)
                P.op("vector", "tensor_scalar", [rk], [rk], out=r[:, 154:155], in0=r[:, 153:154], scalar1=1.0, scalar2=None, op0=ALU.add)
                P.op("vector", "reciprocal", [rk], [rk], out=r[:, 154:155], in_=r[:, 154:155])
                P.op("vector", "tensor_tensor", [rk], [rk], out=r[:, 155:156], in0=r[:, 154:155], in1=r[:, 151:152], op=ALU.mult)
                P.op("vector", "tensor_tensor", [rk], [rk], out=r[:, 156:157], in0=r[:, 155:156], in1=r[:, 153:154], op=ALU.mult)
                P.op("vector", "tensor_scalar", [rk], [("comb", t)], out=comb[:, t, :], in0=r[:, 84:116], scalar1=r[:, 155:156],
                     scalar2=None, op0=ALU.mult)
                P.op("vector", "scalar_tensor_tensor", [rk, ("comb", t)], [("comb", t)], out=comb[:, t, :], in0=r[:, 116:148],
                     scalar=r[:, 156:157], in1=comb[:, t, :], op0=ALU.mult, op1=ALU.add)
            for t0 in range(0, NT, 2):
                interleave([mtile(t) for t in (t0, t0 + 1) if t < NT])
            P.flush()

    def stage_moe(l, hT, comb, yacc):
        with ExitStack() as st:
            sb, ps = stage_allocs(st)
            w1b = Ring(sb, "mo_w1", [128, 8, 256], BF16, 3)
            w3b = Ring(sb, "mo_w3", [128, 8, 256], BF16, 3)
            w2b = Ring(sb, "mo_w2", [128, 2, D], BF16, 3)
            hidb = Ring(sb, "mo_hid", [128, 2, LP], BF16, 2)
            silb = Ring(sb, "mo_sil", [128, 512], F32, 3)
            stg = Ring(sb, "mo_stg", [128, 2048], F32, 3)
            p1 = Ring(ps, "mo_p1", [128, 512], F32, 2)
            p3 = Ring(ps, "mo_p3", [128, 512], F32, 2)
            pY = [ps("mo_pY%d" % i, [128, 1024]) for i in range(2)]
            HT = [("hT", t) for t in range(NT)]
            yi = 0
            for e in range(NE):
                a1, a1k = w1b.next()
                a3, a3k = w3b.next()
                a2, a2k = w2b.next()
                for (dst_, dkey_, src_, c_) in ((a1, a1k, w1[l, e].rearrange("(c p) f -> p c f", p=128), 8),
                                                (a3, a3k, w3[l, e].rearrange("(c p) f -> p c f", p=128), 8),
                                                (a2, a2k, w2[l, e].rearrange("(c p) n -> p c n", p=128), 2)):
                    sg, sgk = stg.next()
                    sv_ = sg[:].rearrange("p (c f) -> p c f", c=c_)
                    P.dma("sync", [], [sgk], out=sv_, in_=src_)
                    P.op("gpsimd", "tensor_copy", [sgk], [dkey_], out=dst_[:], in_=sv_)
                hid, hidk = hidb.next()
                for fc in range(2):
                    for (n0, nn) in NTILES:
                        q1, q1k = p1.next()
                        q3, q3k = p3.next()
                        for k in range(8):
                            P.op("tensor", "matmul", [a1k] + HT, [q1k], q1[:, 0:nn], lhsT=a1[:, k, fc * 128:(fc + 1) * 128],
                                 rhs=hT[:, k, n0:n0 + nn], start=(k == 0), stop=(k == 7))
                        for k in range(8):
                            P.op("tensor", "matmul", [a3k] + HT, [q3k], q3[:, 0:nn], lhsT=a3[:, k, fc * 128:(fc + 1) * 128],
                                 rhs=hT[:, k, n0:n0 + nn], start=(k == 0), stop=(k == 7))
                        s, sk = silb.next()
                        P.op("scalar", "activation", [q1k], [sk], out=s[:, 0:nn], in_=q1[:, 0:nn], func=AF.Silu)
                        P.op("vector", "tensor_tensor", [sk, q3k], [hidk], out=hid[:, fc, n0:n0 + nn], in0=s[:, 0:nn], in1=q3[:, 0:nn],
                             op=ALU.mult)
                for t in range(NT):
                    cs = slice(t * 128, (t + 1) * 128)
                    py, pyk = pY[yi % 2], "mo_pY%d" % (yi % 2)
                    yi += 1
                    for n in range(2):
                        for fc in range(2):
                            P.op("tensor", "matmul", [hidk, a2k], [pyk], py[:, n * 512:(n + 1) * 512], lhsT=hid[:, fc, cs],
                                 rhs=a2[:, fc, n * 512:(n + 1) * 512], start=(fc == 0), stop=(fc == 1))
                    if e == 0:
                        P.op("vector", "tensor_scalar", [pyk, ("comb", t)], [("yacc", t)], out=yacc[:, t, :], in0=py[:],
                             scalar1=comb[:, t, e:e + 1], scalar2=None, op0=ALU.mult)
                    else:
                        P.op("vector", "scalar_tensor_tensor", [pyk, ("comb", t), ("yacc", t)], [("yacc", t)], out=yacc[:, t, :],
                             in0=py[:], scalar=comb[:, t, e:e + 1], in1=yacc[:, t, :], op0=ALU.mult, op1=ALU.add)
            P.flush()

    def stage_ln2(l, yacc, last):
        with ExitStack() as st:
            sb, ps = stage_allocs(st)
            g_rep = sb("l2_g", [128, D])
            b_rep = sb("l2_b", [128, D])
            P.dma("sync", [], ["lng"], out=g_rep[:], in_=ln2g[l, :, :])
            P.dma("sync", [], ["lnb"], out=b_rep[:], in_=ln2b[l, :, :])
            hin = Ring(sb, "l2_hin", [128, D], F32, 2)
            tb = Ring(sb, "l2_t", [128, D], F32, 2)
            ob = Ring(sb, "l2_o", [128, D], F32, 2)
            scr = sb("l2_scr", [128, D])
            stb = Ring(sb, "l2_st", [128, 4], F32, 2)
            def ltile(t):
                hi_, hik = hin.next()
                P.dma("sync", [("h", t)], [hik], out=hi_[:], in_=h_d[t * 128:(t + 1) * 128, :])
                tt, ttk = tb.next()
                P.op("vector", "scalar_tensor_tensor", [hik, ("yacc", t)], [ttk], out=tt[:], in0=hi_[:], scalar=ALPHA,
                     in1=yacc[:, t, :], op0=ALU.mult, op1=ALU.add)
                o, ok = ob.next()
                s4, s4k = stb.next()
                yield
                yield from ln_tile(tt, ttk, g_rep, b_rep, o, ok, scr, "l2_scr", s4, s4k)
                if not last:
                    P.dma("sync", [ok], [("h", t)], out=h_d[t * 128:(t + 1) * 128, :], in_=o[:])
                else:
                    if t == 0:
                        P.dma("sync", [ok], [("y", t)], out=y[0:112, :], in_=o[16:128, :])
                    elif t < 16:
                        P.dma("sync", [ok], [("y", t)], out=y[t * 128 - 16:t * 128 + 112, :], in_=o[:])
                    else:
                        P.dma("sync", [ok], [("y", t)], out=y[2032:2048, :], in_=o[0:16, :])
            for t0 in range(0, NT, 2):
                interleave([ltile(t) for t in (t0, t0 + 1) if t < NT])
            P.flush()

    stages = []
    res = ExitStack()
    rsb, _ = stage_allocs(res)
    hT = rsb("hT", [128, 8, LP], BF16)
    done = False

    def want(name, l):
        nonlocal done
        if done:
            return False
        if only is not None and (name, l) not in only:
            return False
        if stop_after is not None and stop_after == (name, l):
            done = True
        return True

    for l in range(n_layers):
        src = h0 if l == 0 else h_d
        if want("hT", l):
            stage_hT(src, hT)
        if want("proj", l):
            stage_proj(l, hT)
        if want("dn", l):
            stage_dn(l)
        if want("dsa", l):
            stage_dsa(l)
        moe_st = ExitStack()
        msb, _ = stage_allocs(moe_st)
        comb = msb("comb", [128, NT, NE])
        if want("mix", l):
            stage_mix_ln1(l, src, hT, comb)
        yacc = msb("yacc", [128, NT, D])
        if want("moe", l):
            stage_moe(l, hT, comb, yacc)
        if want("ln2", l):
            stage_ln2(l, yacc, last=(l == n_layers - 1))
        moe_st.close()
    P.finish([("y", t) for t in range(NT)])
    res.close()
    top.close()
    return nc


def _rope_tables():
    inv = 1.0 / (10000.0 ** (np.arange(0, 64, 2, dtype=np.float32) / np.float32(64)))
    pos = np.arange(LP, dtype=np.float32)
    ang = pos[:, None] * inv[None, :].astype(np.float32)
    ang = np.concatenate([ang, ang], -1)
    cos = np.cos(ang).astype(np.float32)
    sin = np.sin(ang).astype(np.float32)
    sgn = np.concatenate([-np.ones(32, np.float32), np.ones(32, np.float32)])
    sins = sin * sgn[None, :]
    cosT = np.ascontiguousarray(np.concatenate([cos.T, cos.T], 0))
    sinT = np.ascontiguousarray(np.concatenate([sins.T, sins.T], 0))
    return cosT, sinT


def make_shared(inp):
    f = lambda a: np.ascontiguousarray(np.asarray(a, dtype=np.float32))
    rep = lambda a: f(np.broadcast_to(np.asarray(a, np.float32)[:, None, :], (DEPTH, 128, np.asarray(a).shape[-1])))
    cosT, sinT = _rope_tables()
    cw = np.asarray(inp["conv_w"], np.float32)
    cwT = f(cw.reshape(DEPTH, 4, 12, 128).transpose(0, 3, 2, 1).reshape(DEPTH, 128, 48))
    sh = {
        "w_in": f(inp["w_in"]), "w_out": f(inp["w_out"]),
        "w1": f(inp["w1"]), "w3": f(inp["w3"]), "w2": f(inp["w2"]),
        "wr": f(np.concatenate([np.asarray(inp["w_grp"], np.float32), np.asarray(inp["w_rtr"], np.float32)], -1)),
        "brep": rep(np.concatenate([np.asarray(inp["b_grp"], np.float32), np.asarray(inp["b_rtr"], np.float32)], -1)),
        "cwT": cwT,
        "alog": rep(np.tile(np.asarray(inp["a_log"], np.float32), (1, NT))),
        "dtb": rep(np.tile(np.asarray(inp["dt_bias"], np.float32), (1, NT))),
        "ngr": rep(np.tile(np.asarray(inp["dn_norm_g"], np.float32), (1, 4))),
        "ln1g": rep(inp["ln1_g"]), "ln1b": rep(inp["ln1_b"]), "ln2g": rep(inp["ln2_g"]), "ln2b": rep(inp["ln2_b"]),
        "ropec": cosT, "ropes": sinT,
    }
    return sh


def make_h0(x_b, meta):
    h0 = np.zeros((LP, D), np.float32)
    h0[:NMETA] = meta
    h0[NMETA:L] = x_b
    return h0


_NC_CACHE = {}


def kernel(**inputs):
    x = np.asarray(inputs["x"], np.float32)
    meta = np.asarray(inputs["meta_tokens"], np.float32)
    sh = make_shared(inputs)
    if "nc" not in _NC_CACHE:
        _NC_CACHE["nc"] = build()
    nc = _NC_CACHE["nc"]
    in_maps = []
    for b in range(8):
        m = dict(sh)
        m["h0"] = make_h0(x[b], meta)
        in_maps.append(m)
    res = run_bass_kernel_spmd(nc, in_maps, core_ids=list(range(8)))
    return np.stack([np.asarray(r["y"], np.float32) for r in res.results], 0)
```

```python
import numpy as np
from contextlib import ExitStack
import concourse.bass as bass
import concourse.mybir as mybir
from concourse.bass_utils import run_bass_kernel_spmd

F32 = mybir.dt.float32
BF16 = mybir.dt.bfloat16
AF = mybir.ActivationFunctionType
ALU = mybir.AluOpType

ENGS = ("tensor", "vector", "scalar", "gpsimd", "sync")
N_DMA_SEMS = 12

D = 1024
SEQ = 2048
NMETA = 16
L = SEQ + NMETA
NT = 17
LP = NT * 128
DEPTH = 2
DIN = 4176
ALPHA = (2.0 * DEPTH) ** 0.25
NEG = -30000.0
KTOP = 256
NIT = 20
NE = 32
DN_CUT = 0
PREP_CUT = 0
NTILES = [(0, 512), (512, 512), (1024, 512), (1536, 512), (2048, 128)]


class Prog:
    def __init__(self, nc, stack):
        self.nc = nc
        self.streams = {e: [] for e in ENGS}
        self.esem = {e: stack.enter_context(nc.semaphore("s_" + e)) for e in ENGS}
        self.eseq = {e: 0 for e in ENGS}
        self.eval_ = {e: 0 for e in ENGS}
        self.dsem = {e: [stack.enter_context(nc.semaphore("d_%s%d" % (e, i)))
                         for i in range(N_DMA_SEMS)] for e in ("sync", "gpsimd", "scalar")}
        self.dcnt = {e: [0] * N_DMA_SEMS for e in self.dsem}
        self.drr = {e: 0 for e in self.dsem}
        self.waited = {e: {} for e in ENGS}
        self.last_w = {}
        self.readers = {}
        self.n_ops = 0
        self.excl = set()

    @staticmethod
    def _sk(src):
        return src if isinstance(src, str) else id(src)

    def _need(self, eng, ev, out):
        if ev is None:
            return
        src, val = ev
        if eng == "tensor" and src == "tensor":
            return
        k = self._sk(src)
        if self.waited[eng].get(k, 0) >= val:
            return
        self.waited[eng][k] = val
        out.append((src, val))

    def _deps(self, eng, reads, writes):
        waits = []
        for k in reads:
            self._need(eng, self.last_w.get(k), waits)
        for k in writes:
            self._need(eng, self.last_w.get(k), waits)
            for ev in self.readers.get(k, {}).values():
                self._need(eng, ev, waits)
        return waits

    def _commit(self, ev, reads, writes):
        for k in reads:
            self.readers.setdefault(k, {})[self._sk(ev[0])] = ev
        for k in writes:
            self.last_w[k] = ev
            self.readers[k] = {}

    def op(self, eng, meth, reads, writes, *args, **kw):
        if eng != "tensor":
            ex = [k for k in reads if isinstance(k, str) and k in self.excl and k not in writes]
            if ex:
                writes = list(writes) + ex
        waits = self._deps(eng, reads, writes)
        self.eseq[eng] += 1
        ev = (eng, self.eseq[eng])
        self.streams[eng].append((waits, meth, args, kw, "E", ev[1]))
        self._commit(ev, reads, writes)
        self.n_ops += 1

    def dma(self, q, reads, writes, out, in_, **kw):
        i = self.drr[q]
        self.drr[q] = (i + 1) % N_DMA_SEMS
        sem = self.dsem[q][i]
        waits = self._deps(q, reads, writes)
        if self.dcnt[q][i] > 0:
            self._need(q, (sem, 16 * self.dcnt[q][i]), waits)
        self.dcnt[q][i] += 1
        ev = (sem, 16 * self.dcnt[q][i])
        kw = dict(kw)
        kw["out"] = out
        kw["in_"] = in_
        self.streams[q].append((waits, "dma_start", (), kw, "D", sem))
        self._commit(ev, reads, writes)
        self.n_ops += 1

    def finish(self, final_keys):
        waits = []
        for k in final_keys:
            self._need("sync", self.last_w.get(k), waits)
        self.streams["sync"].append((waits, None, (), {}, None, None))
        self.flush()

    def barrier(self):
        evs = [(e, self.eseq[e]) for e in ENGS if self.eseq[e] > 0]
        for q in self.dsem:
            for i in range(N_DMA_SEMS):
                if self.dcnt[q][i] > 0:
                    evs.append((self.dsem[q][i], 16 * self.dcnt[q][i]))
        for e in ENGS:
            waits = []
            for ev in evs:
                self._need(e, ev, waits)
            if waits:
                self.streams[e].append((waits, None, (), {}, None, None))

    def flush(self):
        self.barrier()
        nc = self.nc
        streams = self.streams
        self.streams = {e: [] for e in ENGS}
        targets = {e: set() for e in ENGS}
        for e in ENGS:
            for waits, meth, args, kw, kind, x in streams[e]:
                for (src, val) in waits:
                    if isinstance(src, str):
                        targets[src].add(val)
        value_of = {}
        for e in ENGS:
            for waits, meth, args, kw, kind, x in streams[e]:
                if kind == "E" and x in targets[e]:
                    self.eval_[e] += 1
                    value_of[(e, x)] = self.eval_[e]
        for e in ENGS:
            for t in targets[e]:
                assert (e, t) in value_of, ("wait target from an earlier flush", e, t)
        esem = self.esem
        with nc.Block() as block:
            def run(engname):
                def body(eng):
                    for waits, meth, args, kw, kind, x in streams[engname]:
                        for (src, val) in waits:
                            if isinstance(src, str):
                                eng.wait_ge(esem[src], value_of[(src, val)])
                            else:
                                eng.wait_ge(src, val)
                        if meth is not None:
                            ins = getattr(eng, meth)(*args, **kw)
                            if kind == "D":
                                ins.then_inc(x, 16)
                            elif (engname, x) in value_of:
                                ins.then_inc(esem[engname], 1)
                return body
            block.tensor(run("tensor"))
            block.vector(run("vector"))
            block.scalar(run("scalar"))
            block.gpsimd(run("gpsimd"))
            block.sync(run("sync"))


class Ring:
    def __init__(self, alloc, name, shape, dt, n):
        self.bufs = [alloc(name + str(i), shape, dt) for i in range(n)]
        self.keys = [name + str(i) for i in range(n)]
        self.i = 0

    def next(self):
        i = self.i
        self.i = (i + 1) % len(self.bufs)
        return self.bufs[i], self.keys[i]


def interleave(gens):
    gens = list(gens)
    while gens:
        for g in list(gens):
            try:
                next(g)
            except StopIteration:
                gens.remove(g)


def build(debug=False, stop_after=None, n_layers=DEPTH, only=None):
    nc = bass.Bass("TRN2", target_bir_lowering=False)
    dk = "ExternalOutput" if debug else "Internal"

    def din(name, shape, dt=F32):
        return nc.dram_tensor(name, list(shape), dt, kind="ExternalInput").ap()

    def dscr(name, shape, dt=F32):
        return nc.dram_tensor(name, list(shape), dt, kind=dk).ap()

    h0 = din("h0", [LP, D])
    w_in = din("w_in", [DEPTH, D, DIN])
    w_out = din("w_out", [DEPTH, D, D])
    w1 = din("w1", [DEPTH, NE, D, 256])
    w3 = din("w3", [DEPTH, NE, D, 256])
    w2 = din("w2", [DEPTH, NE, 256, D])
    wr = din("wr", [DEPTH, D, 36])
    brep = din("brep", [DEPTH, 128, 36])
    cwT = din("cwT", [DEPTH, 128, 48])
    alog = din("alog", [DEPTH, 128, 68])
    dtb = din("dtb", [DEPTH, 128, 68])
    ngr = din("ngr", [DEPTH, 128, 512])
    ln1g = din("ln1g", [DEPTH, 128, D])
    ln1b = din("ln1b", [DEPTH, 128, D])
    ln2g = din("ln2g", [DEPTH, 128, D])
    ln2b = din("ln2b", [DEPTH, 128, D])
    ropec = din("ropec", [128, LP])
    ropes = din("ropes", [128, LP])
    y = nc.dram_tensor("y", [SEQ, D], F32, kind="ExternalOutput").ap()

    h_d = dscr("h_d", [LP, D])
    qkvT_d = dscr("qkvT_d", [12, 128, LP])
    ropeT_d = dscr("ropeT_d", [13, 128, LP], BF16)
    z_d = dscr("z_d", [LP, 512])
    sv_d = dscr("sv_d", [LP, 512], BF16)
    sm_d = dscr("sm_d", [LP, 16])
    mixT_d = dscr("mixT_d", [8, 128, LP], BF16)

    top = ExitStack()
    P = Prog(nc, top)

    uid = [0]

    def uname(name):
        uid[0] += 1
        return "%s_u%d" % (name, uid[0])

    def stage_allocs(st):
        def sb(name, shape, dt=F32):
            return st.enter_context(nc.sbuf_tensor(uname(name), list(shape), dt))

        def ps(name, shape=(128, 512), dt=F32):
            P.excl.add(name)
            return st.enter_context(nc.psum_tensor(uname(name), list(shape), dt))
        return sb, ps

    csb, _ = stage_allocs(top)
    ident = csb("ident", [128, 128])
    identb = csb("identb", [128, 128], BF16)
    ones = csb("ones", [128, 128])
    negones = csb("negones", [128, 128])
    P.op("gpsimd", "memset", [], ["ident"], ident[:], 1.0)
    P.op("gpsimd", "affine_select", ["ident"], ["ident"], out=ident[:], in_=ident[:], pattern=[[-1, 128]],
         compare_op=ALU.is_equal, fill=0.0, base=0, channel_multiplier=1)
    P.op("gpsimd", "tensor_copy", ["ident"], ["identb"], out=identb[:], in_=ident[:])
    P.op("gpsimd", "memset", [], ["ones"], ones[:], 1.0)
    P.op("gpsimd", "memset", [], ["negones"], negones[:], -1.0)

    def ln_tile(sb_t, tkey, g_rep, b_rep, outt, okey, scr, skey, st2, st2key):
        P.op("scalar", "activation", [tkey], [skey, st2key], out=scr[:], in_=sb_t[:], func=AF.Identity,
             accum_out=st2[:, 0:1])
        yield
        P.op("vector", "tensor_scalar", [st2key], [st2key], out=st2[:, 1:2], in0=st2[:, 0:1], scalar1=-1.0 / D,
             scalar2=None, op0=ALU.mult)
        yield
        P.op("scalar", "activation", [tkey, st2key], [skey, st2key], out=scr[:], in_=sb_t[:], func=AF.Square,
             bias=st2[:, 1:2], scale=1.0, accum_out=st2[:, 2:3])
        yield
        P.op("vector", "tensor_scalar", [st2key], [st2key], out=st2[:, 3:4], in0=st2[:, 2:3], scalar1=1.0 / D,
             scalar2=1e-5, op0=ALU.mult, op1=ALU.add)
        yield
        P.op("scalar", "activation", [st2key], [st2key], out=st2[:, 3:4], in_=st2[:, 3:4], func=AF.Ln)
        P.op("scalar", "activation", [st2key], [st2key], out=st2[:, 3:4], in_=st2[:, 3:4], func=AF.Exp, scale=-0.5)
        yield
        P.op("vector", "tensor_scalar", [tkey, st2key], [okey], out=outt[:], in0=sb_t[:], scalar1=st2[:, 1:2],
             scalar2=st2[:, 3:4], op0=ALU.add, op1=ALU.mult)
        yield
        P.op("vector", "tensor_tensor", [okey, "lng"], [okey], out=outt[:], in0=outt[:], in1=g_rep[:], op=ALU.mult)
        yield
        P.op("vector", "tensor_tensor", [okey, "lnb"], [okey], out=outt[:], in0=outt[:], in1=b_rep[:], op=ALU.add)
        yield

    def stage_hT(src, hT, l_unused=None):
        with ExitStack() as st:
            sb, ps = stage_allocs(st)
            ht = Ring(sb, "ht_in", [128, D], F32, 2)
            pT = [ps("hT_ps%d" % i, [128, 1024]) for i in range(2)]
            for t in range(NT):
                a, ak = ht.next()
                P.dma("sync", [("h", t)], [ak], out=a[:], in_=src[t * 128:(t + 1) * 128, :])
                pt, pk = pT[t % 2], "hT_ps%d" % (t % 2)
                for c in range(8):
                    P.op("tensor", "transpose", [ak, "ident"], [pk], out=pt[:, c * 128:(c + 1) * 128],
                         in_=a[:, c * 128:(c + 1) * 128], identity=ident[:])
                eng = "vector" if t % 2 == 0 else "scalar"
                if eng == "vector":
                    P.op("vector", "tensor_copy", [pk], [("hT", t)], out=hT[:, :, t * 128:(t + 1) * 128],
                         in_=pt[:].rearrange("p (c t) -> p c t", c=8))
                else:
                    P.op("scalar", "copy", [pk], [("hT", t)], out=hT[:, :, t * 128:(t + 1) * 128],
                         in_=pt[:].rearrange("p (c t) -> p c t", c=8))
            P.flush()

    def stage_proj(l, hT):
        with ExitStack() as st:
            sb, ps = stage_allocs(st)
            NC_FM = 38 * 128
            W = sb("Wp", [128, 8, NC_FM + 1040], BF16)
            cosT = sb("cosT", [128, LP])
            sinT = sb("sinT", [128, LP])
            P.dma("sync", [], ["cosT"], out=cosT[:], in_=ropec[:, :])
            P.dma("sync", [], ["sinT"], out=sinT[:], in_=ropes[:, :])
            wl = w_in[l].rearrange("(c p) n -> p c n", p=128)

            wstg = Ring(sb, "pj_wst", [128, 8, 256], F32, 3)

            def wk(c0, n):
                return [("Wc", j) for j in range(c0 // 128, (c0 + n + 127) // 128)]

            def ld(dst0, src0, n, key):
                for o in range(0, n, 256):
                    nn_ = min(256, n - o)
                    sg, sgk = wstg.next()
                    P.dma("sync", [], [sgk], out=sg[:, :, 0:nn_], in_=wl[:, :, src0 + o:src0 + o + nn_])
                    P.op("gpsimd", "tensor_copy", [sgk], wk(dst0 + o, nn_), out=W[:, :, dst0 + o:dst0 + o + nn_], in_=sg[:, :, 0:nn_])

            def ld_perm(dst0, src0, nheads, key):
                for o in range(0, nheads, 4):
                    nh_ = min(4, nheads - o)
                    nn_ = nh_ * 64
                    sg, sgk = wstg.next()
                    P.dma("sync", [], [sgk], out=sg[:, :, 0:nn_], in_=wl[:, :, src0 + o * 64:src0 + o * 64 + nn_])
                    dv = W[:, :, dst0 + o * 64:dst0 + o * 64 + nn_].rearrange("p c (h two j) -> p c h two j", two=2, j=32)
                    sv = sg[:, :, 0:nn_].rearrange("p c (h two j) -> p c h two j", two=2, j=32)
                    for half in range(2):
                        P.op("gpsimd", "tensor_copy", [sgk], wk(dst0 + o * 64, nn_), out=dv[:, :, :, half, :], in_=sv[:, :, :, 1 - half, :])
            ld(0, 0, 1536, ("W", 0))
            ld(12 * 128, 2056, 512, ("W", 1))
            ld_perm(16 * 128, 2056, 8, ("W", 1))
            ld(20 * 128, 2568, 512, ("W", 2))
            ld_perm(24 * 128, 2568, 8, ("W", 2))
            ld(28 * 128, 3592, 512, ("W", 3))
            ld_perm(32 * 128, 3592, 8, ("W", 3))
            ld(36 * 128, 4104, 64, ("W", 4))
            ld(36 * 128 + 64, 4104, 64, ("W", 4))
            ld_perm(37 * 128, 4104, 1, ("W", 4))
            ld_perm(37 * 128 + 64, 4104, 1, ("W", 4))
            T0 = NC_FM
            ld(T0, 1536, 512, ("W", 5))
            ld(T0 + 512, 3080, 512, ("W", 5))
            ld(T0 + 1024, 2048, 8, ("W", 5))
            ld(T0 + 1032, 4168, 8, ("W", 5))
            wkeys = [("W", i) for i in range(6)]

            pbank = Ring(ps, "pj_ps", [128, 512], F32, 4)
            stg32 = Ring(sb, "pj_s32", [128, LP], F32, 2)
            stg16 = Ring(sb, "pj_s16", [128, LP], BF16, 2)
            tmp = Ring(sb, "pj_tmp", [128, 512], F32, 2)

            def wgrp(m):
                return [("Wc", m)]

            def mm_fm(m, n0, nn, pt, pk):
                for k in range(8):
                    P.op("tensor", "matmul", wgrp(m) + [("hT", i) for i in range(n0 // 128, (n0 + nn) // 128)], [pk],
                         pt[:, 0:nn], lhsT=W[:, k, m * 128:(m + 1) * 128], rhs=hT[:, k, n0:n0 + nn],
                         start=(k == 0), stop=(k == 7))
            for m in range(12):
                s, sk = stg32.next()
                for (n0, nn) in NTILES:
                    pt, pk = pbank.next()
                    mm_fm(m, n0, nn, pt, pk)
                    if (n0 // 512) % 2 == 0:
                        P.op("vector", "tensor_copy", [pk], [sk], out=s[:, n0:n0 + nn], in_=pt[:, 0:nn])
                    else:
                        P.op("scalar", "copy", [pk], [sk], out=s[:, n0:n0 + nn], in_=pt[:, 0:nn])
                P.dma("sync", [sk], [("qkvT", m)], out=qkvT_d[m, :, :], in_=s[:])
            rope_src = [12, 13, 14, 15, 20, 21, 22, 23, 28, 29, 30, 31, 36]
            rope_prm = [16, 17, 18, 19, 24, 25, 26, 27, 32, 33, 34, 35, 37]
            for r in range(13):
                s, sk = stg16.next()
                for (n0, nn) in NTILES:
                    pa, pak = pbank.next()
                    mm_fm(rope_src[r], n0, nn, pa, pak)
                    pb, pbk = pbank.next()
                    mm_fm(rope_prm[r], n0, nn, pb, pbk)
                    t1, t1k = tmp.next()
                    t2, t2k = tmp.next()
                    P.op("vector", "tensor_tensor", [pak, "cosT"], [t1k], out=t1[:, 0:nn], in0=pa[:, 0:nn],
                         in1=cosT[:, n0:n0 + nn], op=ALU.mult)
                    P.op("vector", "tensor_tensor", [pbk, "sinT"], [t2k], out=t2[:, 0:nn], in0=pb[:, 0:nn],
                         in1=sinT[:, n0:n0 + nn], op=ALU.mult)
                    P.op("gpsimd", "tensor_tensor", [t1k, t2k], [sk], out=s[:, n0:n0 + nn], in0=t1[:, 0:nn],
                         in1=t2[:, 0:nn], op=ALU.add)
                P.dma("sync", [sk], [("ropeT", r)], out=ropeT_d[r, :, :], in_=s[:])
            zst = Ring(sb, "pj_z", [128, 512], F32, 2)
            svst = Ring(sb, "pj_sv", [128, 512], BF16, 2)
            smst = Ring(sb, "pj_sm", [128, 16], F32, 2)
            for t in range(NT):
                for (c0, cn, kind) in ((T0, 512, "z"), (T0 + 512, 512, "sv"), (T0 + 1024, 16, "sm")):
                    pt, pk = pbank.next()
                    for k in range(8):
                        P.op("tensor", "matmul", wk(c0, cn) + [("hT", t)], [pk], pt[:, 0:cn],
                             lhsT=hT[:, k, t * 128:(t + 1) * 128], rhs=W[:, k, c0:c0 + cn], start=(k == 0), stop=(k == 7))
                    if kind == "z":
                        s, sk = zst.next()
                        P.op("scalar", "copy", [pk], [sk], out=s[:], in_=pt[:, 0:512])
                        P.dma("sync", [sk], [("z", t)], out=z_d[t * 128:(t + 1) * 128, :], in_=s[:])
                    elif kind == "sv":
                        s, sk = svst.next()
                        P.op("vector", "tensor_copy", [pk], [sk], out=s[:], in_=pt[:, 0:512])
                        P.dma("sync", [sk], [("sv", t)], out=sv_d[t * 128:(t + 1) * 128, :], in_=s[:])
                    else:
                        s, sk = smst.next()
                        P.op("vector", "tensor_copy", [pk], [sk], out=s[:], in_=pt[:, 0:16])
                        P.dma("sync", [sk], [("sm", t)], out=sm_d[t * 128:(t + 1) * 128, :], in_=s[:])
            P.flush()

    def stage_dn(l):
        with ExitStack() as st:
            sb, ps = stage_allocs(st)
            qT = sb("dn_qT", [128, 4, LP], BF16)
            kT = sb("dn_kT", [128, 4, LP], BF16)
            vT = sb("dn_vT", [128, 4, LP], BF16)
            cw = sb("dn_cw", [128, 48])
            P.dma("sync", [], ["cw"], out=cw[:], in_=cwT[l, :, :])
            with ExitStack() as st1:
                sb1, ps1 = stage_allocs(st1)
                xin = Ring(sb1, "dn_xin", [128, LP + 3], F32, 2)
                acc = Ring(sb1, "dn_acc", [128, LP], F32, 2)
                sq = Ring(sb1, "dn_sq", [128, LP], F32, 2)
                rn = Ring(sb1, "dn_rn", [128, 512], F32, 2)
                pss = Ring(ps1, "dn_ps", [128, 512], F32, 2)
                for m in range(12):
                    x, xk = xin.next()
                    P.op("gpsimd", "memset", [], [xk], x[:, 0:3], 0.0)
                    P.dma("sync", [("qkvT", m)], [xk], out=x[:, 3:LP + 3], in_=qkvT_d[m, :, :])
                    a, ak = acc.next()
                    P.op("vector", "tensor_scalar", [xk, "cw"], [ak], out=a[:], in0=x[:, 3:LP + 3],
                         scalar1=cw[:, m * 4 + 3:m * 4 + 4], scalar2=None, op0=ALU.mult)
                    for j in range(3):
                        P.op("vector", "scalar_tensor_tensor", [xk, "cw", ak], [ak], out=a[:],
                             in0=x[:, j:LP + j], scalar=cw[:, m * 4 + j:m * 4 + j + 1], in1=a[:], op0=ALU.mult, op1=ALU.add)
                    if m >= 8:
                        P.op("scalar", "activation", [ak], [("vT", m - 8)], out=vT[:, m - 8, :], in_=a[:], func=AF.Silu)
                        continue
                    P.op("scalar", "activation", [ak], [ak], out=a[:], in_=a[:], func=AF.Silu)
                    s, sk = sq.next()
                    P.op("gpsimd", "tensor_tensor", [ak], [sk], out=s[:], in0=a[:], in1=a[:], op=ALU.mult)
                    dst = qT if m < 4 else kT
                    dkey = ("qT", m) if m < 4 else ("kT", m - 4)
                    for (n0, nn) in NTILES:
                        pt, pk = pss.next()
                        P.op("tensor", "matmul", [sk, "ones"], [pk], pt[:, 0:nn], lhsT=ones[:], rhs=s[:, n0:n0 + nn],
                             start=True, stop=True)
                        r, rk = rn.next()
                        P.op("scalar", "activation", [pk], [rk], out=r[:, 0:nn], in_=pt[:, 0:nn], func=AF.Ln,
                             bias=1e-6, scale=1.0)
                        P.op("scalar", "activation", [rk], [rk], out=r[:, 0:nn], in_=r[:, 0:nn], func=AF.Exp, scale=-0.5)
                        P.op("vector", "scalar_tensor_tensor", [ak, rk], [dkey], out=dst[:, m % 4, n0:n0 + nn],
                             in0=a[:, n0:n0 + nn], scalar=(128.0 ** -0.5 if m < 4 else 1.0), in1=r[:, 0:nn],
                             op0=ALU.mult, op1=ALU.mult)
                P.flush()
            if DN_CUT == 1:
                return
            QK = [("qT", i) for i in range(4)]
            KK = [("kT", i) for i in range(4)]
            VK = [("vT", i) for i in range(4)]
            sm = sb("dn_sm", [128, NT, 16])
            P.dma("sync", [("sm", t) for t in range(NT)], ["smt"], out=sm[:],
                  in_=sm_d.rearrange("(t p) c -> p t c", p=128))
            alr = sb("dn_alr", [128, 68])
            dtr = sb("dn_dtr", [128, 68])
            P.dma("sync", [], ["alr"], out=alr[:], in_=alog[l, :, :])
            P.dma("sync", [], ["dtr"], out=dtr[:], in_=dtb[l, :, :])
            beta = sb("dn_beta", [128, NT, 4])
            g = sb("dn_g", [128, NT, 4])
            tmpg = sb("dn_tmpg", [128, NT, 4])
            v3 = lambda a: a[:].rearrange("p (t h) -> p t h", h=4)
            P.op("scalar", "activation", ["smt"], ["beta"], out=beta[:], in_=sm[:, :, 0:4], func=AF.Sigmoid)
            P.op("vector", "tensor_tensor", ["smt", "dtr"], ["tmpg"], out=tmpg[:], in0=sm[:, :, 4:8], in1=v3(dtr), op=ALU.add)
            P.op("scalar", "activation", ["tmpg"], ["tmpg"], out=tmpg[:], in_=tmpg[:], func=AF.Exp)
            P.op("scalar", "activation", ["tmpg"], ["tmpg"], out=tmpg[:], in_=tmpg[:], func=AF.Ln, bias=1.0, scale=1.0)
            P.op("scalar", "activation", ["alr"], ["alr"], out=alr[:], in_=alr[:], func=AF.Exp)
            P.op("vector", "scalar_tensor_tensor", ["tmpg", "alr"], ["g"], out=g[:], in0=tmpg[:], scalar=-1.0,
                 in1=v3(alr), op0=ALU.mult, op1=ALU.mult)
            U = sb("dn_U", [128, 128])
            Mst = sb("dn_Mst", [128, 4, 128])
            Mup = sb("dn_Mup", [128, 4, 128])
            P.op("gpsimd", "memset", [], ["U"], U[:], 1.0)
            P.op("gpsimd", "affine_select", ["U"], ["U"], out=U[:], in_=U[:], pattern=[[1, 128]],
                 compare_op=ALU.is_ge, fill=0.0, base=0, channel_multiplier=-1)
            P.op("gpsimd", "memset", [], ["Mst"], Mst[:], 0.0)
            P.op("gpsimd", "affine_select", ["Mst"], ["Mst"], out=Mst[:], in_=Mst[:], pattern=[[0, 4], [-1, 128]],
                 compare_op=ALU.is_ge, fill=NEG, base=-1, channel_multiplier=1)
            P.op("gpsimd", "memset", [], ["Mup"], Mup[:], 0.0)
            P.op("gpsimd", "affine_select", ["Mup"], ["Mup"], out=Mup[:], in_=Mup[:], pattern=[[0, 4], [1, 128]],
                 compare_op=ALU.is_ge, fill=NEG, base=0, channel_multiplier=-1)
            gc = sb("dn_gc", [128, 68])
            ngc = sb("dn_ngc", [128, 68])
            gl = sb("dn_gl", [128, 68])
            egc = sb("dn_egc", [128, 68])
            ekd = sb("dn_ekd", [128, 68])
            egl = sb("dn_egl", [128, 68])
            bgc = sb("dn_bgc", [128, 68])
            st_g = ExitStack()
            psg = st_g.enter_context(nc.psum_tensor(uname("dn_psg"), [128, 512], F32))
            g2 = g[:].rearrange("p t h -> p (t h)")
            P.op("tensor", "matmul", ["g", "U"], ["psg"], psg[:, 0:68], lhsT=U[:], rhs=g2, start=True, stop=True)
            P.op("tensor", "matmul", ["g", "ones"], ["psg"], psg[:, 128:196], lhsT=ones[:], rhs=g2, start=True, stop=True)
            P.op("vector", "tensor_copy", ["psg"], ["gc"], out=gc[:], in_=psg[:, 0:68])
            P.op("vector", "tensor_copy", ["psg"], ["gl"], out=gl[:], in_=psg[:, 128:196])
            P.op("vector", "tensor_scalar", ["gc"], ["ngc"], out=ngc[:], in0=gc[:], scalar1=-1.0, scalar2=None, op0=ALU.mult)
            P.op("scalar", "activation", ["gc"], ["egc"], out=egc[:], in_=gc[:], func=AF.Exp)
            P.op("scalar", "activation", ["gl"], ["egl"], out=egl[:], in_=gl[:], func=AF.Exp)
            P.op("vector", "tensor_tensor", ["gl", "gc"], ["ekd"], out=ekd[:], in0=gl[:], in1=gc[:], op=ALU.subtract)
            P.op("scalar", "activation", ["ekd"], ["ekd"], out=ekd[:], in_=ekd[:], func=AF.Exp)
            P.op("vector", "tensor_tensor", ["egc", "beta"], ["bgc"], out=bgc[:], in0=egc[:],
                 in1=beta[:].rearrange("p t h -> p (t h)"), op=ALU.mult)
            P.flush()
            st_g.close()
            if DN_CUT == 2:
                return

            NB = 4
            pA = [ps("dn_pA%d" % i) for i in range(2)]
            pB = [ps("dn_pB%d" % i) for i in range(2)]
            pS = [ps("dn_pS%d" % i) for i in range(2)]
            pTk = ps("dn_pTk")
            pTv = ps("dn_pTv")
            Dg_ = [Ring(sb, "dn_Dg%d_" % p_, [128, 4, 128], F32, 1) for p_ in range(2)]
            dec_ = [Ring(sb, "dn_dec%d_" % p_, [128, 4, 128], F32, 1) for p_ in range(2)]
            decT = Ring(sb, "dn_decT", [128, 4, 128], F32, NB)
            Abuf_ = [Ring(sb, "dn_A%d_" % p_, [128, 4, 128], F32, 2) for p_ in range(2)]
            Bbuf_ = [Ring(sb, "dn_B%d_" % p_, [128, 4, 128], F32, 2) for p_ in range(2)]
            Xbuf_ = [Ring(sb, "dn_X%d_" % p_, [128, 4, 128], F32, 2) for p_ in range(2)]
            bvb_ = [Ring(sb, "dn_bv%d_" % p_, [128, 4, 128], F32, 1) for p_ in range(2)]
            kbgb_ = [Ring(sb, "dn_kbg%d_" % p_, [128, 4, 128], F32, 1) for p_ in range(2)]
            u4b = Ring(sb, "dn_u4", [128, 4, 128], F32, NB)
            wT4b = Ring(sb, "dn_wT4", [128, 4, 128], BF16, NB)
            aqk4b = Ring(sb, "dn_aqk", [128, 4, 128], BF16, NB)
            kd4b = Ring(sb, "dn_kd4", [128, 4, 128], BF16, NB)

            def slot(ring, t):
                i_ = t % len(ring.bufs)
                return ring.bufs[i_], ring.keys[i_]
            S4 = sb("dn_S4", [128, 4, 128])
            S4b = sb("dn_S4b", [128, 4, 128], BF16)
            P.op("vector", "memset", [], ["S4"], S4[:], 0.0)
            P.op("gpsimd", "memset", [], ["S4b"], S4b[:], 0.0)
            vn4b = Ring(sb, "dn_vn4", [128, 4, 128], BF16, 2)
            qs4b = Ring(sb, "dn_qs4", [128, 4, 128], F32, 2)
            o4b = Ring(sb, "dn_o4", [128, 4, 128], F32, 2)
            ztb = Ring(sb, "dn_zt", [128, 512], F32, 2)
            ogb = Ring(sb, "dn_og", [128, 512], BF16, 2)
            ssb = Ring(sb, "dn_ss", [128, 8], F32, 2)
            junk = sb("dn_junk", [128, 128])
            odT = sb("dn_odT", [128, 4, LP], BF16)
            ng = sb("dn_ng", [128, 512])
            P.dma("sync", [], ["ng"], out=ng[:], in_=ngr[l, :, :])
            prep_out = {}

            def F(ap):
                return ap[:].rearrange("p h c -> p (h c)")

            def prep(t):
                par = t % 2
                Dg, dec, Abuf, Bbuf, Xbuf, bvb, kbgb = Dg_[par], dec_[par], Abuf_[par], Bbuf_[par], Xbuf_[par], bvb_[par], kbgb_[par]
                cs = slice(t * 128, (t + 1) * 128)
                col = lambda a, h: a[:, t * 4 + h:t * 4 + h + 1]
                dg, dgk = Dg.next()
                for h in range(4):
                    P.op("gpsimd", "tensor_scalar", ["ident", "gc"], [dgk], out=dg[:, h, :], in0=ident[:],
                         scalar1=col(gc, h), scalar2=None, op0=ALU.mult)
                a_ps, ak_ps = pA[par], "dn_pA%d" % par
                b_ps, bk_ps = pB[par], "dn_pB%d" % par
                x_ps, xk_ps = b_ps, bk_ps
                P.op("tensor", "matmul", [dgk, "negones"], [ak_ps], a_ps[:], lhsT=negones[:], rhs=F(dg), start=True, stop=False)
                P.op("tensor", "matmul", ["Mst", "ident"], [ak_ps], a_ps[:], lhsT=ident[:], rhs=F(Mst), start=False, stop=True)
                P.op("tensor", "matmul", [dgk, "ones"], [bk_ps], b_ps[:], lhsT=ones[:], rhs=F(dg), start=True, stop=False)
                P.op("tensor", "matmul", ["Mup", "ident"], [bk_ps], b_ps[:], lhsT=ident[:], rhs=F(Mup), start=False, stop=True)
                de, dek = dec.next()
                deT, deTk = slot(decT, t)
                for h in range(4):
                    P.op("scalar", "activation", [ak_ps, "gc"], [dek], out=de[:, h, :], in_=a_ps[:, h * 128:(h + 1) * 128],
                         func=AF.Exp, bias=col(gc, h), scale=1.0)
                    P.op("scalar", "activation", [bk_ps, "ngc"], [deTk], out=deT[:, h, :], in_=b_ps[:, h * 128:(h + 1) * 128],
                         func=AF.Exp, bias=col(ngc, h), scale=1.0)
                yield
                if PREP_CUT == 1:
                    return
                for h in range(4):
                    P.op("tensor", "matmul", KK, [xk_ps], x_ps[:, h * 128:(h + 1) * 128], lhsT=kT[:, h, cs], rhs=kT[:, h, cs],
                         start=True, stop=True)
                A, Ak = Abuf.next()
                for h in range(4):
                    P.op("vector", "scalar_tensor_tensor", [xk_ps, "beta", dek], [Ak], out=A[:, h, :],
                         in0=x_ps[:, h * 128:(h + 1) * 128], scalar=beta[:, t, h:h + 1], in1=de[:, h, :],
                         op0=ALU.mult, op1=ALU.mult)
                for h in range(4):
                    P.op("tensor", "transpose", [Ak, "ident"], [ak_ps], out=a_ps[:, h * 128:(h + 1) * 128], in_=A[:, h, :],
                         identity=ident[:])
                Bm, Bk = Bbuf.next()
                P.op("scalar", "copy", [ak_ps], [Bk], out=F(Bm), in_=a_ps[:])
                X, Xk = Xbuf.next()
                for h in range(4):
                    P.op("gpsimd", "tensor_tensor", ["ident", Bk], [Xk], out=X[:, h, :], in0=ident[:], in1=Bm[:, h, :],
                         op=ALU.subtract)
                if PREP_CUT == 2:
                    return
                for h in range(4):
                    P.op("tensor", "matmul", KK + ["identb"], ["dn_pTk"], pTk[:, h * 128:(h + 1) * 128], lhsT=kT[:, h, cs],
                         rhs=identb[:], start=True, stop=True)
                    P.op("tensor", "matmul", VK + ["identb"], ["dn_pTv"], pTv[:, h * 128:(h + 1) * 128], lhsT=vT[:, h, cs],
                         rhs=identb[:], start=True, stop=True)
                kbg, kbgk = kbgb.next()
                kd4, kd4k = slot(kd4b, t)
                bv, bvk = bvb.next()
                if PREP_CUT == 31:
                    return
                for h in range(4):
                    P.op("vector", "tensor_scalar", ["dn_pTk", "bgc"], [kbgk], out=kbg[:, h, :], in0=pTk[:, h * 128:(h + 1) * 128],
                         scalar1=col(bgc, h), scalar2=None, op0=ALU.mult)
                    if PREP_CUT == 32:
                        continue
                    P.op("scalar", "activation", ["dn_pTk", "ekd"], [kd4k], out=kd4[:, h, :], in_=pTk[:, h * 128:(h + 1) * 128],
                         func=AF.Copy, scale=col(ekd, h))
                    if PREP_CUT == 33:
                        continue
                    P.op("vector", "tensor_scalar", ["dn_pTv", "beta"], [bvk], out=bv[:, h, :],
                         in0=pTv[:, h * 128:(h + 1) * 128], scalar1=beta[:, t, h:h + 1], scalar2=None, op0=ALU.mult)
                if PREP_CUT in (32, 33):
                    return
                yield
                if PREP_CUT == 3:
                    return
                for n in range(1, 7):
                    A2, A2k = Abuf.next()
                    for h in range(4):
                        P.op("tensor", "matmul", [Ak, Bk], [ak_ps], a_ps[:, h * 128:(h + 1) * 128], lhsT=Bm[:, h, :], rhs=A[:, h, :],
                             start=True, stop=True)
                    if n < 6:
                        B2, B2k = Bbuf.next()
                        for h in range(4):
                            P.op("tensor", "matmul", [Ak, Bk], [bk_ps], b_ps[:, h * 128:(h + 1) * 128], lhsT=A[:, h, :], rhs=Bm[:, h, :],
                                 start=True, stop=True)
                    P.op("scalar", "copy", [ak_ps], [A2k], out=F(A2), in_=a_ps[:])
                    if n < 6:
                        P.op("vector", "tensor_copy", [bk_ps], [B2k], out=F(B2), in_=b_ps[:])
                    for h in range(4):
                        P.op("tensor", "matmul", [A2k, Xk], [ak_ps], a_ps[:, h * 128:(h + 1) * 128], lhsT=A2[:, h, :], rhs=X[:, h, :],
                             start=True, stop=True)
                    X2, X2k = Xbuf.next()
                    P.op("vector", "tensor_tensor", [ak_ps, Xk], [X2k], out=F(X2), in0=a_ps[:], in1=F(X), op=ALU.add)
                    A, Ak = A2, A2k
                    if n < 6:
                        Bm, Bk = B2, B2k
                    X, Xk = X2, X2k
                    yield
                if PREP_CUT == 4:
                    return
                u4, u4k = slot(u4b, t)
                wT4, wT4k = slot(wT4b, t)
                aqk, aqkk = slot(aqk4b, t)
                for h in range(4):
                    P.op("tensor", "matmul", [Xk, bvk], [ak_ps], a_ps[:, h * 128:(h + 1) * 128], lhsT=X[:, h, :], rhs=bv[:, h, :],
                         start=True, stop=True)
                    P.op("tensor", "matmul", [Xk, kbgk], [bk_ps], b_ps[:, h * 128:(h + 1) * 128], lhsT=kbg[:, h, :], rhs=X[:, h, :],
                         start=True, stop=True)
                P.op("scalar", "copy", [ak_ps], [u4k], out=F(u4), in_=a_ps[:])
                P.op("scalar", "copy", [bk_ps], [wT4k], out=F(wT4), in_=b_ps[:])
                for h in range(4):
                    P.op("tensor", "matmul", KK + QK, [ak_ps], a_ps[:, h * 128:(h + 1) * 128], lhsT=kT[:, h, cs], rhs=qT[:, h, cs],
                         start=True, stop=True)
                P.op("vector", "tensor_tensor", [ak_ps, deTk], [aqkk], out=F(aqk), in0=a_ps[:], in1=F(deT), op=ALU.mult)
                prep_out[t] = (u4, u4k, wT4, wT4k, aqk, aqkk, kd4, kd4k)
                yield

            def scan(ts):
                for t in ts:
                    u4, u4k, wT4, wT4k, aqk, aqkk, kd4, kd4k = prep_out.pop(t)
                    cs = slice(t * 128, (t + 1) * 128)
                    col = lambda a, h: a[:, t * 4 + h:t * 4 + h + 1]
                    p1, p1k = pS[0], "dn_pS0"
                    p2, p2k = pS[1], "dn_pS1"
                    for h in range(4):
                        P.op("tensor", "matmul", [wT4k, "S4b"], [p1k], p1[:, h * 128:(h + 1) * 128], lhsT=wT4[:, h, :], rhs=S4b[:, h, :],
                             start=True, stop=True)
                    for h in range(4):
                        P.op("tensor", "matmul", QK + ["S4b"], [p2k], p2[:, h * 128:(h + 1) * 128], lhsT=qT[:, h, cs], rhs=S4b[:, h, :],
                             start=True, stop=True)
                    vn, vnk = vn4b.next()
                    P.op("vector", "tensor_tensor", [u4k, p1k], [vnk], out=F(vn), in0=F(u4), in1=p1[:], op=ALU.subtract)
                    qs, qsk = qs4b.next()
                    for h in range(4):
                        P.op("scalar", "activation", [p2k, "egc"], [qsk], out=qs[:, h, :], in_=p2[:, h * 128:(h + 1) * 128],
                             func=AF.Copy, scale=col(egc, h))
                    yield
                    for h in range(4):
                        P.op("tensor", "matmul", [aqkk, vnk], [p1k], p1[:, h * 128:(h + 1) * 128], lhsT=aqk[:, h, :], rhs=vn[:, h, :],
                             start=True, stop=True)
                    for h in range(4):
                        P.op("tensor", "matmul", [kd4k, vnk], [p2k], p2[:, h * 128:(h + 1) * 128], lhsT=kd4[:, h, :], rhs=vn[:, h, :],
                             start=True, stop=True)
                    o4, o4k = o4b.next()
                    P.op("vector", "tensor_tensor", [p1k, qsk], [o4k], out=F(o4), in0=p1[:], in1=F(qs), op=ALU.add)
                    for h in range(4):
                        P.op("vector", "scalar_tensor_tensor", ["S4", "egl", p2k], ["S4"], out=S4[:, h, :], in0=S4[:, h, :],
                             scalar=col(egl, h), in1=p2[:, h * 128:(h + 1) * 128], op0=ALU.mult, op1=ALU.add)
                    P.op("gpsimd", "tensor_copy", ["S4"], ["S4b"], out=F(S4b), in_=F(S4))
                    yield
                    zt, ztk = ztb.next()
                    P.dma("sync", [("z", t)], [ztk], out=zt[:], in_=z_d[t * 128:(t + 1) * 128, :])
                    ss, ssk = ssb.next()
                    for h in range(4):
                        P.op("scalar", "activation", [o4k], ["dn_junk", ssk], out=junk[:], in_=o4[:, h, :], func=AF.Square,
                             accum_out=ss[:, h:h + 1])
                    P.op("vector", "tensor_scalar", [ssk], [ssk], out=ss[:, 4:8], in0=ss[:, 0:4], scalar1=1.0 / 128, scalar2=1e-6,
                         op0=ALU.mult, op1=ALU.add)
                    P.op("scalar", "activation", [ssk], [ssk], out=ss[:, 4:8], in_=ss[:, 4:8], func=AF.Sqrt)
                    P.op("vector", "reciprocal", [ssk], [ssk], out=ss[:, 4:8], in_=ss[:, 4:8])
                    P.op("scalar", "activation", [ztk], [ztk], out=zt[:], in_=zt[:], func=AF.Silu)
                    P.op("gpsimd", "tensor_tensor", [ztk, "ng"], [ztk], out=zt[:], in0=zt[:], in1=ng[:], op=ALU.mult)
                    og, ogk = ogb.next()
                    for h in range(4):
                        P.op("gpsimd", "tensor_scalar", [o4k, ssk], [o4k], out=o4[:, h, :], in0=o4[:, h, :],
                             scalar1=ss[:, 4 + h:5 + h], scalar2=None, op0=ALU.mult)
                        P.op("gpsimd", "tensor_tensor", [o4k, ztk], [ogk], out=og[:, h * 128:(h + 1) * 128], in0=o4[:, h, :],
                             in1=zt[:, h * 128:(h + 1) * 128], op=ALU.mult)
                    for h in range(4):
                        P.op("tensor", "matmul", [ogk, "identb"], ["dn_pTk"], pTk[:, h * 128:(h + 1) * 128],
                             lhsT=og[:, h * 128:(h + 1) * 128], rhs=identb[:], start=True, stop=True)
                    P.op("scalar", "copy", ["dn_pTk"], [("odT", t)], out=odT[:, :, cs],
                         in_=pTk[:, 0:512].rearrange("p (h c) -> p h c", h=4))
                    yield

            for s_ in range(0, NT + 2, 2):
                if DN_CUT == 3 and s_ >= 2:
                    break
                gens = [prep(t) for t in (s_, s_ + 1) if t < NT]
                prev = [t for t in (s_ - 2, s_ - 1) if 0 <= t < NT]
                if prev:
                    gens.append(scan(prev))
                interleave(gens)
            for h in range(4):
                P.dma("sync", [("odT", t) for t in range(NT)], [("mixT", h)], out=mixT_d[h, :, :], in_=odT[:, h, :])
            P.flush()

    def stage_dsa(l):
        with ExitStack() as st:
            sb, ps = stage_allocs(st)
            saq = sb("sa_q", [128, 4, LP], BF16)
            sak = sb("sa_k", [128, 4, LP], BF16)
            iq = sb("sa_iq", [128, 4, LP], BF16)
            ik = sb("sa_ik", [128, LP], BF16)
            for c in range(4):
                P.dma("sync", [("ropeT", c)], ["saq"], out=saq[:, c, :], in_=ropeT_d[c, :, :])
                P.dma("sync", [("ropeT", 4 + c)], ["sak"], out=sak[:, c, :], in_=ropeT_d[4 + c, :, :])
                P.dma("sync", [("ropeT", 8 + c)], ["iq"], out=iq[:, c, :], in_=ropeT_d[8 + c, :, :])
            P.dma("sync", [("ropeT", 12)], ["ik"], out=ik[:], in_=ropeT_d[12, :, :])
            va = sb("sa_va", [128, NT, 8, 65], BF16)
            P.op("gpsimd", "memset", [], ["va"], va[:], 1.0)
            for t in range(NT):
                P.dma("sync", [("sv", t)], ["va"], out=va[:, t, :, 0:64],
                      in_=sv_d[t * 128:(t + 1) * 128, :].rearrange("p (h d) -> p h d", d=64))
            sm = sb("sa_sm", [128, NT, 16])
            P.dma("sync", [("sm", t) for t in range(NT)], ["sasm"], out=sm[:], in_=sm_d.rearrange("(t p) c -> p t c", p=128))
            cmask = sb("sa_cmask", [128, 128], BF16)
            P.op("gpsimd", "memset", [], ["cmask"], cmask[:], 0.0)
            P.op("gpsimd", "affine_select", ["cmask"], ["cmask"], out=cmask[:], in_=cmask[:], pattern=[[-1, 128]],
                 compare_op=ALU.is_ge, fill=NEG, base=0, channel_multiplier=1)
            osT = sb("sa_osT", [128, 4, LP], BF16)
            sc_ps = Ring(ps, "sa_scps", [128, 512], F32, 2)
            s_ps = Ring(ps, "sa_sps", [128, 512], F32, 3)
            o_ps = [ps("sa_ops%d" % i, [128, 512]) for i in range(2)]
            pTb = ps("sa_pTb")
            relu_b = Ring(sb, "sa_relu", [128, 512], F32, 3)
            accb = Ring(sb, "sa_acc", [128, LP], F32, 2)
            mbb = Ring(sb, "sa_mb", [128, LP], BF16, 4)
            junkb = Ring(sb, "sa_junk", [128, LP], BF16, 2)
            ptb = Ring(sb, "sa_pt", [128, 512], BF16, 3)
            stt = Ring(sb, "sa_st", [128, 8], F32, 2)
            wtb = Ring(sb, "sa_wt", [128, NIT + 2], F32, 2)
            ftab = sb("sa_ftab", [128, NIT + 2])
            for n_ in range(NIT + 2):
                P.op("gpsimd", "memset", [], ["ftab"], ftab[:, n_:n_ + 1], 2.0 ** (-n_))
            osb = Ring(sb, "sa_os", [128, 512], BF16, 2)
            rdb = Ring(sb, "sa_rd", [128, 8], F32, 2)

            def hrows(h):
                return slice(0, 64) if h % 2 == 0 else slice(64, 128)

            mb_of = {}

            def scores(i):
                nk = 128 * (i + 1)
                qs_ = slice(i * 128, (i + 1) * 128)
                acc, acck = accb.next()
                for h in range(8):
                    for k0 in range(0, nk, 512):
                        kn = min(512, nk - k0)
                        pt, pk = sc_ps.next()
                        P.op("tensor", "matmul", ["iq", "ik"], [pk], pt[:, 0:kn], lhsT=iq[hrows(h), h // 2, qs_],
                             rhs=ik[hrows(h), k0:k0 + kn], start=True, stop=True)
                        if h == 0:
                            r, rk = relu_b.next()
                            P.op("scalar", "activation", [pk], [rk], out=r[:, 0:kn], in_=pt[:, 0:kn], func=AF.Relu)
                            P.op("vector", "tensor_scalar", [rk, "sasm"], [acck], out=acc[:, k0:k0 + kn], in0=r[:, 0:kn],
                                 scalar1=sm[:, i, 8:9], scalar2=None, op0=ALU.mult)
                        else:
                            r, rk = relu_b.next()
                            P.op("scalar", "activation", [pk], [rk], out=r[:, 0:kn], in_=pt[:, 0:kn], func=AF.Relu)
                            P.op("vector", "scalar_tensor_tensor", [rk, "sasm", acck], [acck], out=acc[:, k0:k0 + kn], in0=r[:, 0:kn],
                                 scalar=sm[:, i, 8 + h:9 + h], in1=acc[:, k0:k0 + kn], op0=ALU.mult, op1=ALU.add)
                    yield
                P.op("gpsimd", "affine_select", [acck], [acck], out=acc[:, i * 128:nk], in_=acc[:, i * 128:nk], pattern=[[-1, 128]],
                     compare_op=ALU.is_ge, fill=-1e30, base=0, channel_multiplier=1)
                mb, mbk = mbb.next()
                s8, s8k = stt.next()
                if nk <= KTOP:
                    P.op("vector", "memset", [], [s8k], s8[:, 0:1], -1e29)
                else:
                    jk, jkk = junkb.next()
                    P.op("vector", "tensor_reduce", [acck], [s8k], out=s8[:, 5:6], in_=acc[:, 0:nk], axis=mybir.AxisListType.X, op=ALU.max)
                    P.op("vector", "tensor_reduce", [acck], [s8k], out=s8[:, 0:1], in_=acc[:, 0:i * 128], axis=mybir.AxisListType.X, op=ALU.min)
                    P.op("vector", "tensor_tensor", [s8k], [s8k], out=s8[:, 1:2], in0=s8[:, 5:6], in1=s8[:, 0:1], op=ALU.subtract)
                    P.op("vector", "tensor_scalar", [s8k], [s8k], out=s8[:, 1:2], in0=s8[:, 1:2], scalar1=1.0001, scalar2=1e-6,
                         op0=ALU.mult, op1=ALU.add)
                    wt, wtk = wtb.next()
                    P.op("vector", "tensor_scalar", ["ftab", s8k], [wtk], out=wt[:], in0=ftab[:], scalar1=s8[:, 1:2], scalar2=None,
                         op0=ALU.mult)
                    P.op("vector", "tensor_tensor", [s8k, wtk], [s8k], out=s8[:, 2:3], in0=s8[:, 0:1], in1=wt[:, 1:2], op=ALU.add)
                    for it in range(1, NIT + 1):
                        P.op("vector", "tensor_scalar", [acck, s8k], [jkk, s8k], out=jk[:, 0:nk], in0=acc[:, 0:nk], scalar1=s8[:, 2:3],
                             scalar2=None, op0=ALU.is_ge, op1=ALU.add, accum_out=s8[:, 3:4])
                        P.op("vector", "tensor_scalar", [s8k], [s8k], out=s8[:, 4:5], in0=s8[:, 3:4], scalar1=KTOP - 0.5, scalar2=0.5,
                             op0=ALU.is_ge, op1=ALU.subtract)
                        P.op("vector", "scalar_tensor_tensor", [s8k, wtk], [s8k], out=s8[:, 2:3], in0=s8[:, 4:5], scalar=wt[:, it:it + 1],
                             in1=s8[:, 2:3], op0=ALU.mult, op1=ALU.add)
                        yield
                    P.op("vector", "tensor_tensor", [s8k, wtk], [s8k], out=s8[:, 0:1], in0=s8[:, 2:3], in1=wt[:, NIT + 1:NIT + 2],
                         op=ALU.subtract)
                P.op("vector", "tensor_scalar", [acck, s8k], [mbk], out=mb[:, 0:nk], in0=acc[:, 0:nk], scalar1=s8[:, 0:1], scalar2=NEG,
                     op0=ALU.is_lt, op1=ALU.mult)
                mb_of[i] = (mb, mbk)
                yield

            def attention(i):
                mb, mbk = mb_of.pop(i)
                qs_ = slice(i * 128, (i + 1) * 128)
                groups = [(kt, hg) for kt in range(i + 1) for hg in range(2)]
                pend = None
                for g in groups + [None]:
                    cur = None
                    if g is not None:
                        kt, hg = g
                        ks_ = slice(kt * 128, (kt + 1) * 128)
                        sp, spk = s_ps.next()
                        for pair in ((0, 2), (1, 3)):
                            for hh in pair:
                                h = hg * 4 + hh
                                P.op("tensor", "matmul", ["sak", "saq"], [spk], sp[:, hh * 128:(hh + 1) * 128],
                                     lhsT=sak[hrows(h), h // 2, ks_], rhs=saq[hrows(h), h // 2, qs_], start=(hh == 0), stop=False,
                                     skip_group_check=True)
                            for hh in pair:
                                P.op("tensor", "matmul", [mbk, "identb"], [spk], sp[:, hh * 128:(hh + 1) * 128],
                                     lhsT=mb[:, ks_], rhs=identb[:], start=False, stop=True, skip_group_check=True)
                        pt, ptk = ptb.next()
                        P.op("scalar", "activation", [spk], [ptk], out=pt[:], in_=sp[:], func=AF.Exp, scale=0.125)
                        cur = (kt, hg, pt, ptk)
                    if pend is not None:
                        kt, hg, pt, ptk = pend
                        for hh in range(4):
                            h = hg * 4 + hh
                            P.op("tensor", "matmul", [ptk, "va"], ["sa_ops%d" % hg], o_ps[hg][:, hh * 65:(hh + 1) * 65],
                                 lhsT=pt[:, hh * 128:(hh + 1) * 128], rhs=va[:, kt, h, :], start=(kt == 0 and hh == 0), stop=(kt == i),
                                 skip_group_check=True)
                    pend = cur
                    yield
                rd, rdk = rdb.next()
                for hg in range(2):
                    P.op("vector", "reciprocal", ["sa_ops%d" % hg], [rdk], out=rd[:, hg * 4:(hg + 1) * 4],
                         in_=o_ps[hg][:, 0:260].rearrange("p (h d) -> p h d", d=65)[:, :, 64])
                os_, osk = osb.next()
                for h in range(8):
                    hg, hh = h // 4, h % 4
                    P.op("vector", "tensor_scalar", ["sa_ops%d" % hg, rdk], [osk], out=os_[:, h * 64:(h + 1) * 64],
                         in0=o_ps[hg][:, hh * 65:hh * 65 + 64], scalar1=rd[:, h:h + 1], scalar2=None, op0=ALU.mult)
                for c in range(4):
                    P.op("tensor", "matmul", [osk, "identb"], ["sa_pTb"], pTb[:, c * 128:(c + 1) * 128],
                         lhsT=os_[:, c * 128:(c + 1) * 128], rhs=identb[:], start=True, stop=True)
                P.op("scalar", "copy", ["sa_pTb"], [("osT", i)], out=osT[:, :, qs_],
                     in_=pTb[:, 0:512].rearrange("p (h c) -> p h c", h=4))
                yield

            def att_chain(ts):
                for t_ in ts:
                    yield from attention(t_)

            for i in range(0, NT + 2, 2):
                gens = [scores(t_) for t_ in (i, i + 1) if t_ < NT]
                prev = [t_ for t_ in (i - 2, i - 1) if 0 <= t_ < NT]
                if prev:
                    gens.append(att_chain(prev))
                interleave(gens)
            for c in range(4):
                P.dma("sync", [("osT", t) for t in range(NT)], [("mixT", 4 + c)], out=mixT_d[4 + c, :, :], in_=osT[:, c, :])
            P.flush()

    def stage_mix_ln1(l, src, hT, comb):
        with ExitStack() as st:
            sb, ps = stage_allocs(st)
            mixT = sb("mx_mixT", [128, 8, LP], BF16)
            for c in range(8):
                P.dma("sync", [("mixT", c)], ["mixT"], out=mixT[:, c, :], in_=mixT_d[c, :, :])
            wo = sb("mx_wo", [128, 8, D], BF16)
            wol = w_out[l].rearrange("(c p) n -> p c n", p=128)
            for c in range(8):
                P.dma("gpsimd", [], ["wo"], out=wo[:, c, :], in_=wol[:, c, :])
            g_rep = sb("mx_g", [128, D])
            b_rep = sb("mx_b", [128, D])
            P.dma("sync", [], ["lng"], out=g_rep[:], in_=ln1g[l, :, :])
            P.dma("sync", [], ["lnb"], out=b_rep[:], in_=ln1b[l, :, :])
            wrs = sb("mx_wr", [128, 8, 36])
            P.dma("sync", [], ["wrs"], out=wrs[:], in_=wr[l].rearrange("(c p) n -> p c n", p=128))
            br = sb("mx_br", [128, 36])
            P.dma("sync", [], ["br"], out=br[:], in_=brep[l, :, :])
            hin = Ring(sb, "mx_hin", [128, D], F32, 2)
            tb = Ring(sb, "mx_t", [128, D], F32, 2)
            ob = Ring(sb, "mx_o", [128, D], F32, 2)
            scr = sb("mx_scr", [128, D])
            stb = Ring(sb, "mx_st", [128, 4], F32, 2)
            hTf = Ring(sb, "mx_hTf", [128, 8, 128], F32, 2)
            pM = [ps("mx_pM%d" % i, [128, 1024]) for i in range(2)]
            pT = [ps("mx_pT%d" % i, [128, 1024]) for i in range(1)]
            pLs = [ps("mx_pL%d" % i, [128, 512]) for i in range(2)]
            rt = Ring(sb, "mx_rt", [128, 160], F32, 2)
            def mtile(t):
                cs = slice(t * 128, (t + 1) * 128)
                pL, pLk = pLs[t % 2], "mx_pL%d" % (t % 2)
                pm, pmk = pM[t % 2], "mx_pM%d" % (t % 2)
                for n in range(2):
                    for c in range(8):
                        P.op("tensor", "matmul", ["mixT", "wo"], [pmk], pm[:, n * 512:(n + 1) * 512], lhsT=mixT[:, c, cs],
                             rhs=wo[:, c, n * 512:(n + 1) * 512], start=(c == 0), stop=(c == 7))
                hi_, hik = hin.next()
                P.dma("sync", [("h", t)], [hik], out=hi_[:], in_=src[t * 128:(t + 1) * 128, :])
                tt, ttk = tb.next()
                P.op("vector", "scalar_tensor_tensor", [hik, pmk], [ttk], out=tt[:], in0=hi_[:], scalar=ALPHA, in1=pm[:],
                     op0=ALU.mult, op1=ALU.add)
                o, ok = ob.next()
                s4, s4k = stb.next()
                yield
                yield from ln_tile(tt, ttk, g_rep, b_rep, o, ok, scr, "mx_scr", s4, s4k)
                P.dma("sync", [ok], [("h", t)], out=h_d[t * 128:(t + 1) * 128, :], in_=o[:])
                yield
                pt, ptk = pT[0], "mx_pT0"
                for c in range(8):
                    P.op("tensor", "transpose", [ok, "ident"], [ptk], out=pt[:, c * 128:(c + 1) * 128], in_=o[:, c * 128:(c + 1) * 128],
                         identity=ident[:])
                hf, hfk = hTf.next()
                P.op("scalar", "copy", [ptk], [hfk], out=hf[:].rearrange("p c t -> p (c t)"), in_=pt[:])
                P.op("gpsimd", "tensor_copy", [hfk], [("hT", t)], out=hT[:, :, cs], in_=hf[:])
                yield
                for c in range(8):
                    P.op("tensor", "matmul", [hfk, "wrs"], [pLk], pL[:, 0:36], lhsT=hf[:, c, :], rhs=wrs[:, c, :],
                         start=(c == 0), stop=(c == 7))
                yield
                r, rk = rt.next()
                P.op("vector", "tensor_tensor", [pLk, "br"], [rk], out=r[:, 0:36], in0=pL[:, 0:36], in1=br[:], op=ALU.add)
                P.op("vector", "tensor_reduce", [rk], [rk], out=r[:, 148:149], in_=r[:, 0:4], axis=mybir.AxisListType.X, op=ALU.max)
                P.op("vector", "tensor_scalar", [rk], [rk], out=r[:, 149:150], in0=r[:, 148:149], scalar1=-1.0, scalar2=None, op0=ALU.mult)
                P.op("scalar", "activation", [rk], [rk], out=r[:, 36:40], in_=r[:, 0:4], func=AF.Exp, bias=r[:, 149:150], scale=1.0,
                     accum_out=r[:, 150:151])
                P.op("vector", "reciprocal", [rk], [rk], out=r[:, 151:152], in_=r[:, 150:151])
                P.op("vector", "tensor_scalar", [rk], [rk], out=r[:, 44:48], in0=r[:, 0:4], scalar1=r[:, 148:149], scalar2=None,
                     op0=ALU.is_ge)
                P.op("vector", "tensor_scalar", [rk], [rk], out=r[:, 48:52], in0=r[:, 44:48], scalar1=-1.0, scalar2=1e30,
                     op0=ALU.add, op1=ALU.mult)
                for gq in range(4):
                    P.op("vector", "tensor_scalar", [rk], [rk], out=r[:, 52 + gq * 8:60 + gq * 8], in0=r[:, 4 + gq * 8:12 + gq * 8],
                         scalar1=r[:, 48 + gq:49 + gq], scalar2=None, op0=ALU.add)
                yield
                P.op("vector", "max", [rk], [rk], out=r[:, 36:44], in_=r[:, 52:84])
                P.op("vector", "tensor_scalar", [rk], [rk], out=r[:, 84:116], in0=r[:, 52:84], scalar1=r[:, 36:37], scalar2=None,
                     op0=ALU.is_equal)
                P.op("vector", "tensor_scalar", [rk], [rk], out=r[:, 116:148], in0=r[:, 52:84], scalar1=r[:, 37:38], scalar2=None,
                     op0=ALU.is_equal)
                P.op("vector", "tensor_tensor", [rk], [rk], out=r[:, 152:153], in0=r[:, 37:38], in1=r[:, 36:37], op=ALU.subtract)
                yield
                P.op("scalar", "activation", [rk], [rk], out=r[:, 153:154], in_=r[:, 152:153], func=AF.Exp)
                P.op("vector", "tensor_scalar", [rk], [rk], out=r[:, 154:155], in0=r[:, 153:154], scalar1=1.0, scalar2=None, op0=ALU.add)
                P.op("vector", "reciprocal", [rk], [rk], out=r[:, 154:155], in_=r[:, 154:155])
                P.op("vector", "tensor_tensor", [rk], [rk], out=r[:, 155:156], in0=r[:, 154:155], in1=r[:, 151:152], op=ALU.mult)
                P.op("vector", "tensor_tensor", [rk], [rk], out=r[:, 156:157], in0=r[:, 155:156], in1=r[:, 153:154], op=ALU.mult)
                P.op("vector", "tensor_scalar", [rk], [("comb", t)], out=comb[:, t, :], in0=r[:, 84:116], scalar1=r[:, 155:156],
                     scalar2=None, op0=ALU.mult)
                P.op("vector", "scalar_tensor_tensor", [rk, ("comb", t)], [("comb", t)], out=comb[:, t, :], in0=r[:, 116:148],
                     scalar=r[:, 156:157], in1=comb[:, t, :], op0=ALU.mult, op1=ALU.add)
            for t0 in range(0, NT, 2):
                interleave([mtile(t) for t in (t0, t0 + 1) if t < NT])
            P.flush()

    def stage_moe(l, hT, comb, yacc):
        with ExitStack() as st:
            sb, ps = stage_allocs(st)
            w1b = Ring(sb, "mo_w1", [128, 8, 256], BF16, 3)
            w3b = Ring(sb, "mo_w3", [128, 8, 256], BF16, 3)
            w2b = Ring(sb, "mo_w2", [128, 2, D], BF16, 3)
            hidb = Ring(sb, "mo_hid", [128, 2, LP], BF16, 2)
            silb = Ring(sb, "mo_sil", [128, 512], F32, 3)
            stg = Ring(sb, "mo_stg", [128, 2048], F32, 3)
            p1 = Ring(ps, "mo_p1", [128, 512], F32, 2)
            p3 = Ring(ps, "mo_p3", [128, 512], F32, 2)
            pY = [ps("mo_pY%d" % i, [128, 1024]) for i in range(2)]
            HT = [("hT", t) for t in range(NT)]
            yi = [0]
            wts = {}

            def hphase(e):
                a1, a1k = w1b.next()
                a3, a3k = w3b.next()
                a2, a2k = w2b.next()
                for (dst_, dkey_, src_, c_) in ((a1, a1k, w1[l, e].rearrange("(c p) f -> p c f", p=128), 8),
                                                (a3, a3k, w3[l, e].rearrange("(c p) f -> p c f", p=128), 8),
                                                (a2, a2k, w2[l, e].rearrange("(c p) n -> p c n", p=128), 2)):
                    sg, sgk = stg.next()
                    sv_ = sg[:].rearrange("p (c f) -> p c f", c=c_)
                    P.dma("sync", [], [sgk], out=sv_, in_=src_)
                    P.op("gpsimd", "tensor_copy", [sgk], [dkey_], out=dst_[:], in_=sv_)
                hid, hidk = hidb.next()
                for fc in range(2):
                    for (n0, nn) in NTILES:
                        q1, q1k = p1.next()
                        q3, q3k = p3.next()
                        for k in range(8):
                            P.op("tensor", "matmul", [a1k] + HT, [q1k], q1[:, 0:nn], lhsT=a1[:, k, fc * 128:(fc + 1) * 128],
                                 rhs=hT[:, k, n0:n0 + nn], start=(k == 0), stop=(k == 7))
                        for k in range(8):
                            P.op("tensor", "matmul", [a3k] + HT, [q3k], q3[:, 0:nn], lhsT=a3[:, k, fc * 128:(fc + 1) * 128],
                                 rhs=hT[:, k, n0:n0 + nn], start=(k == 0), stop=(k == 7))
                        s, sk = silb.next()
                        P.op("scalar", "activation", [q1k], [sk], out=s[:, 0:nn], in_=q1[:, 0:nn], func=AF.Silu)
                        P.op("vector", "tensor_tensor", [sk, q3k], [hidk], out=hid[:, fc, n0:n0 + nn], in0=s[:, 0:nn], in1=q3[:, 0:nn],
                             op=ALU.mult)
                        yield
                wts[e] = (hid, hidk, a2, a2k)

            def yphase(e):
                hid, hidk, a2, a2k = wts.pop(e)
                for t in range(NT):
                    cs = slice(t * 128, (t + 1) * 128)
                    py, pyk = pY[yi[0] % 2], "mo_pY%d" % (yi[0] % 2)
                    yi[0] += 1
                    for n in range(2):
                        for fc in range(2):
                            P.op("tensor", "matmul", [hidk, a2k], [pyk], py[:, n * 512:(n + 1) * 512], lhsT=hid[:, fc, cs],
                                 rhs=a2[:, fc, n * 512:(n + 1) * 512], start=(fc == 0), stop=(fc == 1))
                    if e == 0:
                        P.op("vector", "tensor_scalar", [pyk, ("comb", t)], [("yacc", t)], out=yacc[:, t, :], in0=py[:],
                             scalar1=comb[:, t, e:e + 1], scalar2=None, op0=ALU.mult)
                    else:
                        P.op("vector", "scalar_tensor_tensor", [pyk, ("comb", t), ("yacc", t)], [("yacc", t)], out=yacc[:, t, :],
                             in0=py[:], scalar=comb[:, t, e:e + 1], in1=yacc[:, t, :], op0=ALU.mult, op1=ALU.add)
                    yield

            interleave([hphase(0)])
            for e in range(NE):
                gens = [yphase(e)]
                if e + 1 < NE:
                    gens.append(hphase(e + 1))
                interleave(gens)
            P.flush()

    def stage_ln2(l, yacc, last):
        with ExitStack() as st:
            sb, ps = stage_allocs(st)
            g_rep = sb("l2_g", [128, D])
            b_rep = sb("l2_b", [128, D])
            P.dma("sync", [], ["lng"], out=g_rep[:], in_=ln2g[l, :, :])
            P.dma("sync", [], ["lnb"], out=b_rep[:], in_=ln2b[l, :, :])
            hin = Ring(sb, "l2_hin", [128, D], F32, 2)
            tb = Ring(sb, "l2_t", [128, D], F32, 2)
            ob = Ring(sb, "l2_o", [128, D], F32, 2)
            scr = sb("l2_scr", [128, D])
            stb = Ring(sb, "l2_st", [128, 4], F32, 2)
            def ltile(t):
                hi_, hik = hin.next()
                P.dma("sync", [("h", t)], [hik], out=hi_[:], in_=h_d[t * 128:(t + 1) * 128, :])
                tt, ttk = tb.next()
                P.op("vector", "scalar_tensor_tensor", [hik, ("yacc", t)], [ttk], out=tt[:], in0=hi_[:], scalar=ALPHA,
                     in1=yacc[:, t, :], op0=ALU.mult, op1=ALU.add)
                o, ok = ob.next()
                s4, s4k = stb.next()
                yield
                yield from ln_tile(tt, ttk, g_rep, b_rep, o, ok, scr, "l2_scr", s4, s4k)
                if not last:
                    P.dma("sync", [ok], [("h", t)], out=h_d[t * 128:(t + 1) * 128, :], in_=o[:])
                else:
                    if t == 0:
                        P.dma("sync", [ok], [("y", t)], out=y[0:112, :], in_=o[16:128, :])
                    elif t < 16:
                        P.dma("sync", [ok], [("y", t)], out=y[t * 128 - 16:t * 128 + 112, :], in_=o[:])
                    else:
                        P.dma("sync", [ok], [("y", t)], out=y[2032:2048, :], in_=o[0:16, :])
            for t0 in range(0, NT, 2):
                interleave([ltile(t) for t in (t0, t0 + 1) if t < NT])
            P.flush()

    stages = []
    res = ExitStack()
    rsb, _ = stage_allocs(res)
    hT = rsb("hT", [128, 8, LP], BF16)
    done = False

    def want(name, l):
        nonlocal done
        if done:
            return False
        if only is not None and (name, l) not in only:
            return False
        if stop_after is not None and stop_after == (name, l):
            done = True
        return True

    for l in range(n_layers):
        src = h0 if l == 0 else h_d
        if want("hT", l):
            stage_hT(src, hT)
        if want("proj", l):
            stage_proj(l, hT)
        if want("dn", l):
            stage_dn(l)
        if want("dsa", l):
            stage_dsa(l)
        moe_st = ExitStack()
        msb, _ = stage_allocs(moe_st)
        comb = msb("comb", [128, NT, NE])
        if want("mix", l):
            stage_mix_ln1(l, src, hT, comb)
        yacc = msb("yacc", [128, NT, D])
        if want("moe", l):
            stage_moe(l, hT, comb, yacc)
        if want("ln2", l):
            stage_ln2(l, yacc, last=(l == n_layers - 1))
        moe_st.close()
    P.finish([("y", t) for t in range(NT)])
    res.close()
    top.close()
    return nc


def _rope_tables():
    inv = 1.0 / (10000.0 ** (np.arange(0, 64, 2, dtype=np.float32) / np.float32(64)))
    pos = np.arange(LP, dtype=np.float32)
    ang = pos[:, None] * inv[None, :].astype(np.float32)
    ang = np.concatenate([ang, ang], -1)
    cos = np.cos(ang).astype(np.float32)
    sin = np.sin(ang).astype(np.float32)
    sgn = np.concatenate([-np.ones(32, np.float32), np.ones(32, np.float32)])
    sins = sin * sgn[None, :]
    cosT = np.ascontiguousarray(np.concatenate([cos.T, cos.T], 0))
    sinT = np.ascontiguousarray(np.concatenate([sins.T, sins.T], 0))
    return cosT, sinT


def make_shared(inp):
    f = lambda a: np.ascontiguousarray(np.asarray(a, dtype=np.float32))
    rep = lambda a: f(np.broadcast_to(np.asarray(a, np.float32)[:, None, :], (DEPTH, 128, np.asarray(a).shape[-1])))
    cosT, sinT = _rope_tables()
    cw = np.asarray(inp["conv_w"], np.float32)
    cwT = f(cw.reshape(DEPTH, 4, 12, 128).transpose(0, 3, 2, 1).reshape(DEPTH, 128, 48))
    sh = {
        "w_in": f(inp["w_in"]), "w_out": f(inp["w_out"]),
        "w1": f(inp["w1"]), "w3": f(inp["w3"]), "w2": f(inp["w2"]),
        "wr": f(np.concatenate([np.asarray(inp["w_grp"], np.float32), np.asarray(inp["w_rtr"], np.float32)], -1)),
        "brep": rep(np.concatenate([np.asarray(inp["b_grp"], np.float32), np.asarray(inp["b_rtr"], np.float32)], -1)),
        "cwT": cwT,
        "alog": rep(np.tile(np.asarray(inp["a_log"], np.float32), (1, NT))),
        "dtb": rep(np.tile(np.asarray(inp["dt_bias"], np.float32), (1, NT))),
        "ngr": rep(np.tile(np.asarray(inp["dn_norm_g"], np.float32), (1, 4))),
        "ln1g": rep(inp["ln1_g"]), "ln1b": rep(inp["ln1_b"]), "ln2g": rep(inp["ln2_g"]), "ln2b": rep(inp["ln2_b"]),
        "ropec": cosT, "ropes": sinT,
    }
    return sh


def make_h0(x_b, meta):
    h0 = np.zeros((LP, D), np.float32)
    h0[:NMETA] = meta
    h0[NMETA:L] = x_b
    return h0


_NC_CACHE = {}


def kernel(**inputs):
    x = np.asarray(inputs["x"], np.float32)
    meta = np.asarray(inputs["meta_tokens"], np.float32)
    sh = make_shared(inputs)
    if "nc" not in _NC_CACHE:
        _NC_CACHE["nc"] = build()
    nc = _NC_CACHE["nc"]
    in_maps = []
    for b in range(8):
        m = dict(sh)
        m["h0"] = make_h0(x[b], meta)
        in_maps.append(m)
    res = run_bass_kernel_spmd(nc, in_maps, core_ids=list(range(8)))
    return np.stack([np.asarray(r["y"], np.float32) for r in res.results], 0)
```

```python
import numpy as np
from contextlib import ExitStack
import concourse.bass as bass
import concourse.mybir as mybir
from concourse.bass_utils import run_bass_kernel_spmd

F32 = mybir.dt.float32
BF16 = mybir.dt.bfloat16
AF = mybir.ActivationFunctionType
ALU = mybir.AluOpType

ENGS = ("tensor", "vector", "scalar", "gpsimd", "sync")
N_DMA_SEMS = 12

D = 1024
SEQ = 2048
NMETA = 16
L = SEQ + NMETA
NT = 17
LP = NT * 128
DEPTH = 2
DIN = 4176
ALPHA = (2.0 * DEPTH) ** 0.25
NEG = -30000.0
KTOP = 256
NIT = 20
NE = 32
DN_CUT = 0
PREP_CUT = 0
NTILES = [(0, 512), (512, 512), (1024, 512), (1536, 512), (2048, 128)]


class Prog:
    def __init__(self, nc, stack):
        self.nc = nc
        self.streams = {e: [] for e in ENGS}
        self.esem = {e: stack.enter_context(nc.semaphore("s_" + e)) for e in ENGS}
        self.eseq = {e: 0 for e in ENGS}
        self.eval_ = {e: 0 for e in ENGS}
        self.dsem = {e: [stack.enter_context(nc.semaphore("d_%s%d" % (e, i)))
                         for i in range(N_DMA_SEMS)] for e in ("sync", "gpsimd", "scalar")}
        self.dcnt = {e: [0] * N_DMA_SEMS for e in self.dsem}
        self.drr = {e: 0 for e in self.dsem}
        self.waited = {e: {} for e in ENGS}
        self.last_w = {}
        self.readers = {}
        self.n_ops = 0
        self.excl = set()

    @staticmethod
    def _sk(src):
        return src if isinstance(src, str) else id(src)

    def _need(self, eng, ev, out):
        if ev is None:
            return
        src, val = ev
        if eng == "tensor" and src == "tensor":
            return
        k = self._sk(src)
        if self.waited[eng].get(k, 0) >= val:
            return
        self.waited[eng][k] = val
        out.append((src, val))

    def _deps(self, eng, reads, writes):
        waits = []
        for k in reads:
            self._need(eng, self.last_w.get(k), waits)
        for k in writes:
            self._need(eng, self.last_w.get(k), waits)
            for ev in self.readers.get(k, {}).values():
                self._need(eng, ev, waits)
        return waits

    def _commit(self, ev, reads, writes):
        for k in reads:
            self.readers.setdefault(k, {})[self._sk(ev[0])] = ev
        for k in writes:
            self.last_w[k] = ev
            self.readers[k] = {}

    def op(self, eng, meth, reads, writes, *args, **kw):
        if eng != "tensor":
            ex = [k for k in reads if isinstance(k, str) and k in self.excl and k not in writes]
            if ex:
                writes = list(writes) + ex
        waits = self._deps(eng, reads, writes)
        self.eseq[eng] += 1
        ev = (eng, self.eseq[eng])
        self.streams[eng].append((waits, meth, args, kw, "E", ev[1]))
        self._commit(ev, reads, writes)
        self.n_ops += 1

    def dma(self, q, reads, writes, out, in_, **kw):
        i = self.drr[q]
        self.drr[q] = (i + 1) % N_DMA_SEMS
        sem = self.dsem[q][i]
        waits = self._deps(q, reads, writes)
        if self.dcnt[q][i] > 0:
            self._need(q, (sem, 16 * self.dcnt[q][i]), waits)
        self.dcnt[q][i] += 1
        ev = (sem, 16 * self.dcnt[q][i])
        kw = dict(kw)
        kw["out"] = out
        kw["in_"] = in_
        self.streams[q].append((waits, "dma_start", (), kw, "D", sem))
        self._commit(ev, reads, writes)
        self.n_ops += 1

    def finish(self, final_keys):
        waits = []
        for k in final_keys:
            self._need("sync", self.last_w.get(k), waits)
        self.streams["sync"].append((waits, None, (), {}, None, None))
        self.flush()

    def barrier(self):
        evs = [(e, self.eseq[e]) for e in ENGS if self.eseq[e] > 0]
        for q in self.dsem:
            for i in range(N_DMA_SEMS):
                if self.dcnt[q][i] > 0:
                    evs.append((self.dsem[q][i], 16 * self.dcnt[q][i]))
        for e in ENGS:
            waits = []
            for ev in evs:
                self._need(e, ev, waits)
            if waits:
                self.streams[e].append((waits, None, (), {}, None, None))

    def flush(self):
        self.barrier()
        nc = self.nc
        streams = self.streams
        self.streams = {e: [] for e in ENGS}
        targets = {e: set() for e in ENGS}
        for e in ENGS:
            for waits, meth, args, kw, kind, x in streams[e]:
                for (src, val) in waits:
                    if isinstance(src, str):
                        targets[src].add(val)
        value_of = {}
        for e in ENGS:
            for waits, meth, args, kw, kind, x in streams[e]:
                if kind == "E" and x in targets[e]:
                    self.eval_[e] += 1
                    value_of[(e, x)] = self.eval_[e]
        for e in ENGS:
            for t in targets[e]:
                assert (e, t) in value_of, ("wait target from an earlier flush", e, t)
        esem = self.esem
        with nc.Block() as block:
            def run(engname):
                def body(eng):
                    for waits, meth, args, kw, kind, x in streams[engname]:
                        for (src, val) in waits:
                            if isinstance(src, str):
                                eng.wait_ge(esem[src], value_of[(src, val)])
                            else:
                                eng.wait_ge(src, val)
                        if meth is not None:
                            ins = getattr(eng, meth)(*args, **kw)
                            if kind == "D":
                                ins.then_inc(x, 16)
                            elif (engname, x) in value_of:
                                ins.then_inc(esem[engname], 1)
                return body
            block.tensor(run("tensor"))
            block.vector(run("vector"))
            block.scalar(run("scalar"))
            block.gpsimd(run("gpsimd"))
            block.sync(run("sync"))


class Ring:
    def __init__(self, alloc, name, shape, dt, n):
        self.bufs = [alloc(name + str(i), shape, dt) for i in range(n)]
        self.keys = [name + str(i) for i in range(n)]
        self.i = 0

    def next(self):
        i = self.i
        self.i = (i + 1) % len(self.bufs)
        return self.bufs[i], self.keys[i]


def interleave(gens):
    gens = list(gens)
    while gens:
        for g in list(gens):
            try:
                next(g)
            except StopIteration:
                gens.remove(g)


def build(debug=False, stop_after=None, n_layers=DEPTH, only=None):
    nc = bass.Bass("TRN2", target_bir_lowering=False)
    dk = "ExternalOutput" if debug else "Internal"

    def din(name, shape, dt=F32):
        return nc.dram_tensor(name, list(shape), dt, kind="ExternalInput").ap()

    def dscr(name, shape, dt=F32):
        return nc.dram_tensor(name, list(shape), dt, kind=dk).ap()

    h0 = din("h0", [LP, D])
    w_in = din("w_in", [DEPTH, D, DIN])
    w_out = din("w_out", [DEPTH, D, D])
    w1 = din("w1", [DEPTH, NE, D, 256])
    w3 = din("w3", [DEPTH, NE, D, 256])
    w2 = din("w2", [DEPTH, NE, 256, D])
    wr = din("wr", [DEPTH, D, 36])
    brep = din("brep", [DEPTH, 128, 36])
    cwT = din("cwT", [DEPTH, 128, 48])
    alog = din("alog", [DEPTH, 128, 68])
    dtb = din("dtb", [DEPTH, 128, 68])
    ngr = din("ngr", [DEPTH, 128, 512])
    ln1g = din("ln1g", [DEPTH, 128, D])
    ln1b = din("ln1b", [DEPTH, 128, D])
    ln2g = din("ln2g", [DEPTH, 128, D])
    ln2b = din("ln2b", [DEPTH, 128, D])
    ropec = din("ropec", [128, LP])
    ropes = din("ropes", [128, LP])
    y = nc.dram_tensor("y", [SEQ, D], F32, kind="ExternalOutput").ap()

    h_d = dscr("h_d", [LP, D])
    qkvT_d = dscr("qkvT_d", [12, 128, LP])
    ropeT_d = dscr("ropeT_d", [13, 128, LP], BF16)
    z_d = dscr("z_d", [LP, 512])
    sv_d = dscr("sv_d", [LP, 512], BF16)
    sm_d = dscr("sm_d", [LP, 16])
    mixT_d = dscr("mixT_d", [8, 128, LP], BF16)

    top = ExitStack()
    P = Prog(nc, top)

    uid = [0]

    def uname(name):
        uid[0] += 1
        return "%s_u%d" % (name, uid[0])

    def stage_allocs(st):
        def sb(name, shape, dt=F32):
            return st.enter_context(nc.sbuf_tensor(uname(name), list(shape), dt))

        def ps(name, shape=(128, 512), dt=F32):
            P.excl.add(name)
            return st.enter_context(nc.psum_tensor(uname(name), list(shape), dt))
        return sb, ps

    csb, _ = stage_allocs(top)
    ident = csb("ident", [128, 128])
    identb = csb("identb", [128, 128], BF16)
    ones = csb("ones", [128, 128])
    negones = csb("negones", [128, 128])
    P.op("gpsimd", "memset", [], ["ident"], ident[:], 1.0)
    P.op("gpsimd", "affine_select", ["ident"], ["ident"], out=ident[:], in_=ident[:], pattern=[[-1, 128]],
         compare_op=ALU.is_equal, fill=0.0, base=0, channel_multiplier=1)
    P.op("gpsimd", "tensor_copy", ["ident"], ["identb"], out=identb[:], in_=ident[:])
    P.op("gpsimd", "memset", [], ["ones"], ones[:], 1.0)
    P.op("gpsimd", "memset", [], ["negones"], negones[:], -1.0)

    def ln_tile(sb_t, tkey, g_rep, b_rep, outt, okey, scr, skey, st2, st2key):
        P.op("scalar", "activation", [tkey], [skey, st2key], out=scr[:], in_=sb_t[:], func=AF.Identity,
             accum_out=st2[:, 0:1])
        yield
        P.op("vector", "tensor_scalar", [st2key], [st2key], out=st2[:, 1:2], in0=st2[:, 0:1], scalar1=-1.0 / D,
             scalar2=None, op0=ALU.mult)
        yield
        P.op("scalar", "activation", [tkey, st2key], [skey, st2key], out=scr[:], in_=sb_t[:], func=AF.Square,
             bias=st2[:, 1:2], scale=1.0, accum_out=st2[:, 2:3])
        yield
        P.op("vector", "tensor_scalar", [st2key], [st2key], out=st2[:, 3:4], in0=st2[:, 2:3], scalar1=1.0 / D,
             scalar2=1e-5, op0=ALU.mult, op1=ALU.add)
        yield
        P.op("scalar", "activation", [st2key], [st2key], out=st2[:, 3:4], in_=st2[:, 3:4], func=AF.Ln)
        P.op("scalar", "activation", [st2key], [st2key], out=st2[:, 3:4], in_=st2[:, 3:4], func=AF.Exp, scale=-0.5)
        yield
        P.op("vector", "tensor_scalar", [tkey, st2key], [okey], out=outt[:], in0=sb_t[:], scalar1=st2[:, 1:2],
             scalar2=st2[:, 3:4], op0=ALU.add, op1=ALU.mult)
        yield
        P.op("vector", "tensor_tensor", [okey, "lng"], [okey], out=outt[:], in0=outt[:], in1=g_rep[:], op=ALU.mult)
        yield
        P.op("vector", "tensor_tensor", [okey, "lnb"], [okey], out=outt[:], in0=outt[:], in1=b_rep[:], op=ALU.add)
        yield

    def stage_hT(src, hT, l_unused=None):
        with ExitStack() as st:
            sb, ps = stage_allocs(st)
            ht = Ring(sb, "ht_in", [128, D], F32, 2)
            pT = [ps("hT_ps%d" % i, [128, 1024]) for i in range(2)]
            for t in range(NT):
                a, ak = ht.next()
                P.dma("sync", [("h", t)], [ak], out=a[:], in_=src[t * 128:(t + 1) * 128, :])
                pt, pk = pT[t % 2], "hT_ps%d" % (t % 2)
                for c in range(8):
                    P.op("tensor", "transpose", [ak, "ident"], [pk], out=pt[:, c * 128:(c + 1) * 128],
                         in_=a[:, c * 128:(c + 1) * 128], identity=ident[:])
                eng = "vector" if t % 2 == 0 else "scalar"
                if eng == "vector":
                    P.op("vector", "tensor_copy", [pk], [("hT", t)], out=hT[:, :, t * 128:(t + 1) * 128],
                         in_=pt[:].rearrange("p (c t) -> p c t", c=8))
                else:
                    P.op("scalar", "copy", [pk], [("hT", t)], out=hT[:, :, t * 128:(t + 1) * 128],
                         in_=pt[:].rearrange("p (c t) -> p c t", c=8))
            P.flush()

    def stage_proj(l, hT):
        with ExitStack() as st:
            sb, ps = stage_allocs(st)
            NC_FM = 38 * 128
            W = sb("Wp", [128, 8, NC_FM + 1040], BF16)
            cosT = sb("cosT", [128, LP])
            sinT = sb("sinT", [128, LP])
            P.dma("sync", [], ["cosT"], out=cosT[:], in_=ropec[:, :])
            P.dma("sync", [], ["sinT"], out=sinT[:], in_=ropes[:, :])
            wl = w_in[l].rearrange("(c p) n -> p c n", p=128)

            wstg = Ring(sb, "pj_wst", [128, 8, 256], F32, 3)

            def wk(c0, n):
                return [("Wc", j) for j in range(c0 // 128, (c0 + n + 127) // 128)]

            def ld(dst0, src0, n, key):
                for o in range(0, n, 256):
                    nn_ = min(256, n - o)
                    sg, sgk = wstg.next()
                    P.dma("sync", [], [sgk], out=sg[:, :, 0:nn_], in_=wl[:, :, src0 + o:src0 + o + nn_])
                    P.op("gpsimd", "tensor_copy", [sgk], wk(dst0 + o, nn_), out=W[:, :, dst0 + o:dst0 + o + nn_], in_=sg[:, :, 0:nn_])

            def ld_perm(dst0, src0, nheads, key):
                for o in range(0, nheads, 4):
                    nh_ = min(4, nheads - o)
                    nn_ = nh_ * 64
                    sg, sgk = wstg.next()
                    P.dma("sync", [], [sgk], out=sg[:, :, 0:nn_], in_=wl[:, :, src0 + o * 64:src0 + o * 64 + nn_])
                    dv = W[:, :, dst0 + o * 64:dst0 + o * 64 + nn_].rearrange("p c (h two j) -> p c h two j", two=2, j=32)
                    sv = sg[:, :, 0:nn_].rearrange("p c (h two j) -> p c h two j", two=2, j=32)
                    for half in range(2):
                        P.op("gpsimd", "tensor_copy", [sgk], wk(dst0 + o * 64, nn_), out=dv[:, :, :, half, :], in_=sv[:, :, :, 1 - half, :])
            ld(0, 0, 1536, ("W", 0))
            ld(12 * 128, 2056, 512, ("W", 1))
            ld_perm(16 * 128, 2056, 8, ("W", 1))
            ld(20 * 128, 2568, 512, ("W", 2))
            ld_perm(24 * 128, 2568, 8, ("W", 2))
            ld(28 * 128, 3592, 512, ("W", 3))
            ld_perm(32 * 128, 3592, 8, ("W", 3))
            ld(36 * 128, 4104, 64, ("W", 4))
            ld(36 * 128 + 64, 4104, 64, ("W", 4))
            ld_perm(37 * 128, 4104, 1, ("W", 4))
            ld_perm(37 * 128 + 64, 4104, 1, ("W", 4))
            T0 = NC_FM
            ld(T0, 1536, 512, ("W", 5))
            ld(T0 + 512, 3080, 512, ("W", 5))
            ld(T0 + 1024, 2048, 8, ("W", 5))
            ld(T0 + 1032, 4168, 8, ("W", 5))
            wkeys = [("W", i) for i in range(6)]

            pbank = Ring(ps, "pj_ps", [128, 512], F32, 4)
            stg32 = Ring(sb, "pj_s32", [128, LP], F32, 2)
            stg16 = Ring(sb, "pj_s16", [128, LP], BF16, 2)
            tmp = Ring(sb, "pj_tmp", [128, 512], F32, 2)

            def wgrp(m):
                return [("Wc", m)]

            def mm_fm(m, n0, nn, pt, pk):
                for k in range(8):
                    P.op("tensor", "matmul", wgrp(m) + [("hT", i) for i in range(n0 // 128, (n0 + nn) // 128)], [pk],
                         pt[:, 0:nn], lhsT=W[:, k, m * 128:(m + 1) * 128], rhs=hT[:, k, n0:n0 + nn],
                         start=(k == 0), stop=(k == 7))
            for m in range(12):
                s, sk = stg32.next()
                for (n0, nn) in NTILES:
                    pt, pk = pbank.next()
                    mm_fm(m, n0, nn, pt, pk)
                    if (n0 // 512) % 2 == 0:
                        P.op("vector", "tensor_copy", [pk], [sk], out=s[:, n0:n0 + nn], in_=pt[:, 0:nn])
                    else:
                        P.op("scalar", "copy", [pk], [sk], out=s[:, n0:n0 + nn], in_=pt[:, 0:nn])
                P.dma("sync", [sk], [("qkvT", m)], out=qkvT_d[m, :, :], in_=s[:])
            rope_src = [12, 13, 14, 15, 20, 21, 22, 23, 28, 29, 30, 31, 36]
            rope_prm = [16, 17, 18, 19, 24, 25, 26, 27, 32, 33, 34, 35, 37]
            for r in range(13):
                s, sk = stg16.next()
                for (n0, nn) in NTILES:
                    pa, pak = pbank.next()
                    mm_fm(rope_src[r], n0, nn, pa, pak)
                    pb, pbk = pbank.next()
                    mm_fm(rope_prm[r], n0, nn, pb, pbk)
                    t1, t1k = tmp.next()
                    t2, t2k = tmp.next()
                    P.op("vector", "tensor_tensor", [pak, "cosT"], [t1k], out=t1[:, 0:nn], in0=pa[:, 0:nn],
                         in1=cosT[:, n0:n0 + nn], op=ALU.mult)
                    P.op("vector", "tensor_tensor", [pbk, "sinT"], [t2k], out=t2[:, 0:nn], in0=pb[:, 0:nn],
                         in1=sinT[:, n0:n0 + nn], op=ALU.mult)
                    P.op("gpsimd", "tensor_tensor", [t1k, t2k], [sk], out=s[:, n0:n0 + nn], in0=t1[:, 0:nn],
                         in1=t2[:, 0:nn], op=ALU.add)
                P.dma("sync", [sk], [("ropeT", r)], out=ropeT_d[r, :, :], in_=s[:])
            zst = Ring(sb, "pj_z", [128, 512], F32, 2)
            svst = Ring(sb, "pj_sv", [128, 512], BF16, 2)
            smst = Ring(sb, "pj_sm", [128, 16], F32, 2)
            for t in range(NT):
                for (c0, cn, kind) in ((T0, 512, "z"), (T0 + 512, 512, "sv"), (T0 + 1024, 16, "sm")):
                    pt, pk = pbank.next()
                    for k in range(8):
                        P.op("tensor", "matmul", wk(c0, cn) + [("hT", t)], [pk], pt[:, 0:cn],
                             lhsT=hT[:, k, t * 128:(t + 1) * 128], rhs=W[:, k, c0:c0 + cn], start=(k == 0), stop=(k == 7))
                    if kind == "z":
                        s, sk = zst.next()
                        P.op("scalar", "copy", [pk], [sk], out=s[:], in_=pt[:, 0:512])
                        P.dma("sync", [sk], [("z", t)], out=z_d[t * 128:(t + 1) * 128, :], in_=s[:])
                    elif kind == "sv":
                        s, sk = svst.next()
                        P.op("vector", "tensor_copy", [pk], [sk], out=s[:], in_=pt[:, 0:512])
                        P.dma("sync", [sk], [("sv", t)], out=sv_d[t * 128:(t + 1) * 128, :], in_=s[:])
                    else:
                        s, sk = smst.next()
                        P.op("vector", "tensor_copy", [pk], [sk], out=s[:], in_=pt[:, 0:16])
                        P.dma("sync", [sk], [("sm", t)], out=sm_d[t * 128:(t + 1) * 128, :], in_=s[:])
            P.flush()

    def stage_dn(l):
        with ExitStack() as st:
            sb, ps = stage_allocs(st)
            qT = sb("dn_qT", [128, 4, LP], BF16)
            kT = sb("dn_kT", [128, 4, LP], BF16)
            vT = sb("dn_vT", [128, 4, LP], BF16)
            cw = sb("dn_cw", [128, 48])
            P.dma("sync", [], ["cw"], out=cw[:], in_=cwT[l, :, :])
            with ExitStack() as st1:
                sb1, ps1 = stage_allocs(st1)
                xin = Ring(sb1, "dn_xin", [128, LP + 3], F32, 2)
                acc = Ring(sb1, "dn_acc", [128, LP], F32, 2)
                sq = Ring(sb1, "dn_sq", [128, LP], F32, 2)
                rn = Ring(sb1, "dn_rn", [128, 512], F32, 2)
                pss = Ring(ps1, "dn_ps", [128, 512], F32, 2)
                for m in range(12):
                    x, xk = xin.next()
                    P.op("gpsimd", "memset", [], [xk], x[:, 0:3], 0.0)
                    P.dma("sync", [("qkvT", m)], [xk], out=x[:, 3:LP + 3], in_=qkvT_d[m, :, :])
                    a, ak = acc.next()
                    P.op("vector", "tensor_scalar", [xk, "cw"], [ak], out=a[:], in0=x[:, 3:LP + 3],
                         scalar1=cw[:, m * 4 + 3:m * 4 + 4], scalar2=None, op0=ALU.mult)
                    for j in range(3):
                        P.op("vector", "scalar_tensor_tensor", [xk, "cw", ak], [ak], out=a[:],
                             in0=x[:, j:LP + j], scalar=cw[:, m * 4 + j:m * 4 + j + 1], in1=a[:], op0=ALU.mult, op1=ALU.add)
                    if m >= 8:
                        P.op("scalar", "activation", [ak], [("vT", m - 8)], out=vT[:, m - 8, :], in_=a[:], func=AF.Silu)
                        continue
                    P.op("scalar", "activation", [ak], [ak], out=a[:], in_=a[:], func=AF.Silu)
                    s, sk = sq.next()
                    P.op("gpsimd", "tensor_tensor", [ak], [sk], out=s[:], in0=a[:], in1=a[:], op=ALU.mult)
                    dst = qT if m < 4 else kT
                    dkey = ("qT", m) if m < 4 else ("kT", m - 4)
                    for (n0, nn) in NTILES:
                        pt, pk = pss.next()
                        P.op("tensor", "matmul", [sk, "ones"], [pk], pt[:, 0:nn], lhsT=ones[:], rhs=s[:, n0:n0 + nn],
                             start=True, stop=True)
                        r, rk = rn.next()
                        P.op("scalar", "activation", [pk], [rk], out=r[:, 0:nn], in_=pt[:, 0:nn], func=AF.Ln,
                             bias=1e-6, scale=1.0)
                        P.op("scalar", "activation", [rk], [rk], out=r[:, 0:nn], in_=r[:, 0:nn], func=AF.Exp, scale=-0.5)
                        P.op("vector", "scalar_tensor_tensor", [ak, rk], [dkey], out=dst[:, m % 4, n0:n0 + nn],
                             in0=a[:, n0:n0 + nn], scalar=(128.0 ** -0.5 if m < 4 else 1.0), in1=r[:, 0:nn],
                             op0=ALU.mult, op1=ALU.mult)
                P.flush()
            if DN_CUT == 1:
                return
            QK = [("qT", i) for i in range(4)]
            KK = [("kT", i) for i in range(4)]
            VK = [("vT", i) for i in range(4)]
            sm = sb("dn_sm", [128, NT, 16])
            P.dma("sync", [("sm", t) for t in range(NT)], ["smt"], out=sm[:],
                  in_=sm_d.rearrange("(t p) c -> p t c", p=128))
            alr = sb("dn_alr", [128, 68])
            dtr = sb("dn_dtr", [128, 68])
            P.dma("sync", [], ["alr"], out=alr[:], in_=alog[l, :, :])
            P.dma("sync", [], ["dtr"], out=dtr[:], in_=dtb[l, :, :])
            beta = sb("dn_beta", [128, NT, 4])
            g = sb("dn_g", [128, NT, 4])
            tmpg = sb("dn_tmpg", [128, NT, 4])
            v3 = lambda a: a[:].rearrange("p (t h) -> p t h", h=4)
            P.op("scalar", "activation", ["smt"], ["beta"], out=beta[:], in_=sm[:, :, 0:4], func=AF.Sigmoid)
            P.op("vector", "tensor_tensor", ["smt", "dtr"], ["tmpg"], out=tmpg[:], in0=sm[:, :, 4:8], in1=v3(dtr), op=ALU.add)
            P.op("scalar", "activation", ["tmpg"], ["tmpg"], out=tmpg[:], in_=tmpg[:], func=AF.Exp)
            P.op("scalar", "activation", ["tmpg"], ["tmpg"], out=tmpg[:], in_=tmpg[:], func=AF.Ln, bias=1.0, scale=1.0)
            P.op("scalar", "activation", ["alr"], ["alr"], out=alr[:], in_=alr[:], func=AF.Exp)
            P.op("vector", "scalar_tensor_tensor", ["tmpg", "alr"], ["g"], out=g[:], in0=tmpg[:], scalar=-1.0,
                 in1=v3(alr), op0=ALU.mult, op1=ALU.mult)
            U = sb("dn_U", [128, 128])
            Mst = sb("dn_Mst", [128, 4, 128])
            Mup = sb("dn_Mup", [128, 4, 128])
            P.op("gpsimd", "memset", [], ["U"], U[:], 1.0)
            P.op("gpsimd", "affine_select", ["U"], ["U"], out=U[:], in_=U[:], pattern=[[1, 128]],
                 compare_op=ALU.is_ge, fill=0.0, base=0, channel_multiplier=-1)
            P.op("gpsimd", "memset", [], ["Mst"], Mst[:], 0.0)
            P.op("gpsimd", "affine_select", ["Mst"], ["Mst"], out=Mst[:], in_=Mst[:], pattern=[[0, 4], [-1, 128]],
                 compare_op=ALU.is_ge, fill=NEG, base=-1, channel_multiplier=1)
            P.op("gpsimd", "memset", [], ["Mup"], Mup[:], 0.0)
            P.op("gpsimd", "affine_select", ["Mup"], ["Mup"], out=Mup[:], in_=Mup[:], pattern=[[0, 4], [1, 128]],
                 compare_op=ALU.is_ge, fill=NEG, base=0, channel_multiplier=-1)
            gc = sb("dn_gc", [128, 68])
            ngc = sb("dn_ngc", [128, 68])
            gl = sb("dn_gl", [128, 68])
            egc = sb("dn_egc", [128, 68])
            ekd = sb("dn_ekd", [128, 68])
            egl = sb("dn_egl", [128, 68])
            bgc = sb("dn_bgc", [128, 68])
            st_g = ExitStack()
            psg = st_g.enter_context(nc.psum_tensor(uname("dn_psg"), [128, 512], F32))
            g2 = g[:].rearrange("p t h -> p (t h)")
            P.op("tensor", "matmul", ["g", "U"], ["psg"], psg[:, 0:68], lhsT=U[:], rhs=g2, start=True, stop=True)
            P.op("tensor", "matmul", ["g", "ones"], ["psg"], psg[:, 128:196], lhsT=ones[:], rhs=g2, start=True, stop=True)
            P.op("vector", "tensor_copy", ["psg"], ["gc"], out=gc[:], in_=psg[:, 0:68])
            P.op("vector", "tensor_copy", ["psg"], ["gl"], out=gl[:], in_=psg[:, 128:196])
            P.op("vector", "tensor_scalar", ["gc"], ["ngc"], out=ngc[:], in0=gc[:], scalar1=-1.0, scalar2=None, op0=ALU.mult)
            P.op("scalar", "activation", ["gc"], ["egc"], out=egc[:], in_=gc[:], func=AF.Exp)
            P.op("scalar", "activation", ["gl"], ["egl"], out=egl[:], in_=gl[:], func=AF.Exp)
            P.op("vector", "tensor_tensor", ["gl", "gc"], ["ekd"], out=ekd[:], in0=gl[:], in1=gc[:], op=ALU.subtract)
            P.op("scalar", "activation", ["ekd"], ["ekd"], out=ekd[:], in_=ekd[:], func=AF.Exp)
            P.op("vector", "tensor_tensor", ["egc", "beta"], ["bgc"], out=bgc[:], in0=egc[:],
                 in1=beta[:].rearrange("p t h -> p (t h)"), op=ALU.mult)
            P.flush()
            st_g.close()
            if DN_CUT == 2:
                return

            NB = 4
            pA = [ps("dn_pA%d" % i) for i in range(2)]
            pB = [ps("dn_pB%d" % i) for i in range(2)]
            pS = [ps("dn_pS%d" % i) for i in range(2)]
            pTk = ps("dn_pTk")
            pTv = ps("dn_pTv")
            Dg_ = [Ring(sb, "dn_Dg%d_" % p_, [128, 4, 128], F32, 1) for p_ in range(2)]
            dec_ = [Ring(sb, "dn_dec%d_" % p_, [128, 4, 128], F32, 1) for p_ in range(2)]
            decT = Ring(sb, "dn_decT", [128, 4, 128], F32, NB)
            Abuf_ = [Ring(sb, "dn_A%d_" % p_, [128, 4, 128], F32, 2) for p_ in range(2)]
            Bbuf_ = [Ring(sb, "dn_B%d_" % p_, [128, 4, 128], F32, 2) for p_ in range(2)]
            Xbuf_ = [Ring(sb, "dn_X%d_" % p_, [128, 4, 128], F32, 2) for p_ in range(2)]
            bvb_ = [Ring(sb, "dn_bv%d_" % p_, [128, 4, 128], F32, 1) for p_ in range(2)]
            kbgb_ = [Ring(sb, "dn_kbg%d_" % p_, [128, 4, 128], F32, 1) for p_ in range(2)]
            u4b = Ring(sb, "dn_u4", [128, 4, 128], F32, NB)
            wT4b = Ring(sb, "dn_wT4", [128, 4, 128], BF16, NB)
            aqk4b = Ring(sb, "dn_aqk", [128, 4, 128], BF16, NB)
            kd4b = Ring(sb, "dn_kd4", [128, 4, 128], BF16, NB)

            def slot(ring, t):
                i_ = t % len(ring.bufs)
                return ring.bufs[i_], ring.keys[i_]
            S4 = sb("dn_S4", [128, 4, 128])
            S4b = sb("dn_S4b", [128, 4, 128], BF16)
            P.op("vector", "memset", [], ["S4"], S4[:], 0.0)
            P.op("gpsimd", "memset", [], ["S4b"], S4b[:], 0.0)
            vn4b = Ring(sb, "dn_vn4", [128, 4, 128], BF16, 2)
            qs4b = Ring(sb, "dn_qs4", [128, 4, 128], F32, 2)
            o4b = Ring(sb, "dn_o4", [128, 4, 128], F32, 2)
            ztb = Ring(sb, "dn_zt", [128, 512], F32, 2)
            ogb = Ring(sb, "dn_og", [128, 512], BF16, 2)
            ssb = Ring(sb, "dn_ss", [128, 8], F32, 2)
            junk = sb("dn_junk", [128, 128])
            odT = sb("dn_odT", [128, 4, LP], BF16)
            ng = sb("dn_ng", [128, 512])
            P.dma("sync", [], ["ng"], out=ng[:], in_=ngr[l, :, :])
            prep_out = {}

            def F(ap):
                return ap[:].rearrange("p h c -> p (h c)")

            def prep(t):
                par = t % 2
                Dg, dec, Abuf, Bbuf, Xbuf, bvb, kbgb = Dg_[par], dec_[par], Abuf_[par], Bbuf_[par], Xbuf_[par], bvb_[par], kbgb_[par]
                cs = slice(t * 128, (t + 1) * 128)
                col = lambda a, h: a[:, t * 4 + h:t * 4 + h + 1]
                dg, dgk = Dg.next()
                for h in range(4):
                    P.op("gpsimd", "tensor_scalar", ["ident", "gc"], [dgk], out=dg[:, h, :], in0=ident[:],
                         scalar1=col(gc, h), scalar2=None, op0=ALU.mult)
                a_ps, ak_ps = pA[par], "dn_pA%d" % par
                b_ps, bk_ps = pB[par], "dn_pB%d" % par
                x_ps, xk_ps = b_ps, bk_ps
                P.op("tensor", "matmul", [dgk, "negones"], [ak_ps], a_ps[:], lhsT=negones[:], rhs=F(dg), start=True, stop=False)
                P.op("tensor", "matmul", ["Mst", "ident"], [ak_ps], a_ps[:], lhsT=ident[:], rhs=F(Mst), start=False, stop=True)
                P.op("tensor", "matmul", [dgk, "ones"], [bk_ps], b_ps[:], lhsT=ones[:], rhs=F(dg), start=True, stop=False)
                P.op("tensor", "matmul", ["Mup", "ident"], [bk_ps], b_ps[:], lhsT=ident[:], rhs=F(Mup), start=False, stop=True)
                de, dek = dec.next()
                deT, deTk = slot(decT, t)
                for h in range(4):
                    P.op("scalar", "activation", [ak_ps, "gc"], [dek], out=de[:, h, :], in_=a_ps[:, h * 128:(h + 1) * 128],
                         func=AF.Exp, bias=col(gc, h), scale=1.0)
                    P.op("scalar", "activation", [bk_ps, "ngc"], [deTk], out=deT[:, h, :], in_=b_ps[:, h * 128:(h + 1) * 128],
                         func=AF.Exp, bias=col(ngc, h), scale=1.0)
                yield
                if PREP_CUT == 1:
                    return
                for h in range(4):
                    P.op("tensor", "matmul", KK, [xk_ps], x_ps[:, h * 128:(h + 1) * 128], lhsT=kT[:, h, cs], rhs=kT[:, h, cs],
                         start=True, stop=True)
                A, Ak = Abuf.next()
                for h in range(4):
                    P.op("vector", "scalar_tensor_tensor", [xk_ps, "beta", dek], [Ak], out=A[:, h, :],
                         in0=x_ps[:, h * 128:(h + 1) * 128], scalar=beta[:, t, h:h + 1], in1=de[:, h, :],
                         op0=ALU.mult, op1=ALU.mult)
                for h in range(4):
                    P.op("tensor", "transpose", [Ak, "ident"], [ak_ps], out=a_ps[:, h * 128:(h + 1) * 128], in_=A[:, h, :],
                         identity=ident[:])
                Bm, Bk = Bbuf.next()
                P.op("scalar", "copy", [ak_ps], [Bk], out=F(Bm), in_=a_ps[:])
                X, Xk = Xbuf.next()
                for h in range(4):
                    P.op("gpsimd", "tensor_tensor", ["ident", Bk], [Xk], out=X[:, h, :], in0=ident[:], in1=Bm[:, h, :],
                         op=ALU.subtract)
                if PREP_CUT == 2:
                    return
                for h in range(4):
                    P.op("tensor", "matmul", KK + ["identb"], ["dn_pTk"], pTk[:, h * 128:(h + 1) * 128], lhsT=kT[:, h, cs],
                         rhs=identb[:], start=True, stop=True)
                    P.op("tensor", "matmul", VK + ["identb"], ["dn_pTv"], pTv[:, h * 128:(h + 1) * 128], lhsT=vT[:, h, cs],
                         rhs=identb[:], start=True, stop=True)
                kbg, kbgk = kbgb.next()
                kd4, kd4k = slot(kd4b, t)
                bv, bvk = bvb.next()
                if PREP_CUT == 31:
                    return
                for h in range(4):
                    P.op("vector", "tensor_scalar", ["dn_pTk", "bgc"], [kbgk], out=kbg[:, h, :], in0=pTk[:, h * 128:(h + 1) * 128],
                         scalar1=col(bgc, h), scalar2=None, op0=ALU.mult)
                    if PREP_CUT == 32:
                        continue
                    P.op("scalar", "activation", ["dn_pTk", "ekd"], [kd4k], out=kd4[:, h, :], in_=pTk[:, h * 128:(h + 1) * 128],
                         func=AF.Copy, scale=col(ekd, h))
                    if PREP_CUT == 33:
                        continue
                    P.op("vector", "tensor_scalar", ["dn_pTv", "beta"], [bvk], out=bv[:, h, :],
                         in0=pTv[:, h * 128:(h + 1) * 128], scalar1=beta[:, t, h:h + 1], scalar2=None, op0=ALU.mult)
                if PREP_CUT in (32, 33):
                    return
                yield
                if PREP_CUT == 3:
                    return
                for n in range(1, 7):
                    A2, A2k = Abuf.next()
                    for h in range(4):
                        P.op("tensor", "matmul", [Ak, Bk], [ak_ps], a_ps[:, h * 128:(h + 1) * 128], lhsT=Bm[:, h, :], rhs=A[:, h, :],
                             start=True, stop=True)
                    if n < 6:
                        B2, B2k = Bbuf.next()
                        for h in range(4):
                            P.op("tensor", "matmul", [Ak, Bk], [bk_ps], b_ps[:, h * 128:(h + 1) * 128], lhsT=A[:, h, :], rhs=Bm[:, h, :],
                                 start=True, stop=True)
                    P.op("scalar", "copy", [ak_ps], [A2k], out=F(A2), in_=a_ps[:])
                    if n < 6:
                        P.op("vector", "tensor_copy", [bk_ps], [B2k], out=F(B2), in_=b_ps[:])
                    for h in range(4):
                        P.op("tensor", "matmul", [A2k, Xk], [ak_ps], a_ps[:, h * 128:(h + 1) * 128], lhsT=A2[:, h, :], rhs=X[:, h, :],
                             start=True, stop=True)
                    X2, X2k = Xbuf.next()
                    P.op("vector", "tensor_tensor", [ak_ps, Xk], [X2k], out=F(X2), in0=a_ps[:], in1=F(X), op=ALU.add)
                    A, Ak = A2, A2k
                    if n < 6:
                        Bm, Bk = B2, B2k
                    X, Xk = X2, X2k
                    yield
                if PREP_CUT == 4:
                    return
                u4, u4k = slot(u4b, t)
                wT4, wT4k = slot(wT4b, t)
                aqk, aqkk = slot(aqk4b, t)
                for h in range(4):
                    P.op("tensor", "matmul", [Xk, bvk], [ak_ps], a_ps[:, h * 128:(h + 1) * 128], lhsT=X[:, h, :], rhs=bv[:, h, :],
                         start=True, stop=True)
                    P.op("tensor", "matmul", [Xk, kbgk], [bk_ps], b_ps[:, h * 128:(h + 1) * 128], lhsT=kbg[:, h, :], rhs=X[:, h, :],
                         start=True, stop=True)
                P.op("scalar", "copy", [ak_ps], [u4k], out=F(u4), in_=a_ps[:])
                P.op("scalar", "copy", [bk_ps], [wT4k], out=F(wT4), in_=b_ps[:])
                for h in range(4):
                    P.op("tensor", "matmul", KK + QK, [ak_ps], a_ps[:, h * 128:(h + 1) * 128], lhsT=kT[:, h, cs], rhs=qT[:, h, cs],
                         start=True, stop=True)
                P.op("vector", "tensor_tensor", [ak_ps, deTk], [aqkk], out=F(aqk), in0=a_ps[:], in1=F(deT), op=ALU.mult)
                prep_out[t] = (u4, u4k, wT4, wT4k, aqk, aqkk, kd4, kd4k)
                yield

            def scan(ts):
                for t in ts:
                    u4, u4k, wT4, wT4k, aqk, aqkk, kd4, kd4k = prep_out.pop(t)
                    cs = slice(t * 128, (t + 1) * 128)
                    col = lambda a, h: a[:, t * 4 + h:t * 4 + h + 1]
                    p1, p1k = pS[0], "dn_pS0"
                    p2, p2k = pS[1], "dn_pS1"
                    for h in range(4):
                        P.op("tensor", "matmul", [wT4k, "S4b"], [p1k], p1[:, h * 128:(h + 1) * 128], lhsT=wT4[:, h, :], rhs=S4b[:, h, :],
                             start=True, stop=True)
                    for h in range(4):
                        P.op("tensor", "matmul", QK + ["S4b"], [p2k], p2[:, h * 128:(h + 1) * 128], lhsT=qT[:, h, cs], rhs=S4b[:, h, :],
                             start=True, stop=True)
                    vn, vnk = vn4b.next()
                    P.op("vector", "tensor_tensor", [u4k, p1k], [vnk], out=F(vn), in0=F(u4), in1=p1[:], op=ALU.subtract)
                    qs, qsk = qs4b.next()
                    for h in range(4):
                        P.op("scalar", "activation", [p2k, "egc"], [qsk], out=qs[:, h, :], in_=p2[:, h * 128:(h + 1) * 128],
                             func=AF.Copy, scale=col(egc, h))
                    yield
                    for h in range(4):
                        P.op("tensor", "matmul", [aqkk, vnk], [p1k], p1[:, h * 128:(h + 1) * 128], lhsT=aqk[:, h, :], rhs=vn[:, h, :],
                             start=True, stop=True)
                    for h in range(4):
                        P.op("tensor", "matmul", [kd4k, vnk], [p2k], p2[:, h * 128:(h + 1) * 128], lhsT=kd4[:, h, :], rhs=vn[:, h, :],
                             start=True, stop=True)
                    o4, o4k = o4b.next()
                    P.op("vector", "tensor_tensor", [p1k, qsk], [o4k], out=F(o4), in0=p1[:], in1=F(qs), op=ALU.add)
                    for h in range(4):
                        P.op("vector", "scalar_tensor_tensor", ["S4", "egl", p2k], ["S4"], out=S4[:, h, :], in0=S4[:, h, :],
                             scalar=col(egl, h), in1=p2[:, h * 128:(h + 1) * 128], op0=ALU.mult, op1=ALU.add)
                    P.op("gpsimd", "tensor_copy", ["S4"], ["S4b"], out=F(S4b), in_=F(S4))
                    yield
                    zt, ztk = ztb.next()
                    P.dma("sync", [("z", t)], [ztk], out=zt[:], in_=z_d[t * 128:(t + 1) * 128, :])
                    ss, ssk = ssb.next()
                    for h in range(4):
                        P.op("scalar", "activation", [o4k], ["dn_junk", ssk], out=junk[:], in_=o4[:, h, :], func=AF.Square,
                             accum_out=ss[:, h:h + 1])
                    P.op("vector", "tensor_scalar", [ssk], [ssk], out=ss[:, 4:8], in0=ss[:, 0:4], scalar1=1.0 / 128, scalar2=1e-6,
                         op0=ALU.mult, op1=ALU.add)
                    P.op("scalar", "activation", [ssk], [ssk], out=ss[:, 4:8], in_=ss[:, 4:8], func=AF.Sqrt)
                    P.op("vector", "reciprocal", [ssk], [ssk], out=ss[:, 4:8], in_=ss[:, 4:8])
                    P.op("scalar", "activation", [ztk], [ztk], out=zt[:], in_=zt[:], func=AF.Silu)
                    P.op("gpsimd", "tensor_tensor", [ztk, "ng"], [ztk], out=zt[:], in0=zt[:], in1=ng[:], op=ALU.mult)
                    og, ogk = ogb.next()
                    for h in range(4):
                        P.op("gpsimd", "tensor_scalar", [o4k, ssk], [o4k], out=o4[:, h, :], in0=o4[:, h, :],
                             scalar1=ss[:, 4 + h:5 + h], scalar2=None, op0=ALU.mult)
                        P.op("gpsimd", "tensor_tensor", [o4k, ztk], [ogk], out=og[:, h * 128:(h + 1) * 128], in0=o4[:, h, :],
                             in1=zt[:, h * 128:(h + 1) * 128], op=ALU.mult)
                    for h in range(4):
                        P.op("tensor", "matmul", [ogk, "identb"], ["dn_pTk"], pTk[:, h * 128:(h + 1) * 128],
                             lhsT=og[:, h * 128:(h + 1) * 128], rhs=identb[:], start=True, stop=True)
                    P.op("scalar", "copy", ["dn_pTk"], [("odT", t)], out=odT[:, :, cs],
                         in_=pTk[:, 0:512].rearrange("p (h c) -> p h c", h=4))
                    yield

            for s_ in range(0, NT + 2, 2):
                if DN_CUT == 3 and s_ >= 2:
                    break
                gens = [prep(t) for t in (s_, s_ + 1) if t < NT]
                prev = [t for t in (s_ - 2, s_ - 1) if 0 <= t < NT]
                if prev:
                    gens.append(scan(prev))
                interleave(gens)
            for h in range(4):
                P.dma("sync", [("odT", t) for t in range(NT)], [("mixT", h)], out=mixT_d[h, :, :], in_=odT[:, h, :])
            P.flush()

    def stage_dsa(l):
        with ExitStack() as st:
            sb, ps = stage_allocs(st)
            saq = sb("sa_q", [128, 4, LP], BF16)
            sak = sb("sa_k", [128, 4, LP], BF16)
            iq = sb("sa_iq", [128, 4, LP], BF16)
            ik = sb("sa_ik", [128, LP], BF16)
            for c in range(4):
                P.dma("sync", [("ropeT", c)], ["saq"], out=saq[:, c, :], in_=ropeT_d[c, :, :])
                P.dma("sync", [("ropeT", 4 + c)], ["sak"], out=sak[:, c, :], in_=ropeT_d[4 + c, :, :])
                P.dma("sync", [("ropeT", 8 + c)], ["iq"], out=iq[:, c, :], in_=ropeT_d[8 + c, :, :])
            P.dma("sync", [("ropeT", 12)], ["ik"], out=ik[:], in_=ropeT_d[12, :, :])
            va = sb("sa_va", [128, NT, 8, 65], BF16)
            P.op("gpsimd", "memset", [], ["va"], va[:], 1.0)
            for t in range(NT):
                P.dma("sync", [("sv", t)], ["va"], out=va[:, t, :, 0:64],
                      in_=sv_d[t * 128:(t + 1) * 128, :].rearrange("p (h d) -> p h d", d=64))
            sm = sb("sa_sm", [128, NT, 16])
            P.dma("sync", [("sm", t) for t in range(NT)], ["sasm"], out=sm[:], in_=sm_d.rearrange("(t p) c -> p t c", p=128))
            cmask = sb("sa_cmask", [128, 128], BF16)
            P.op("gpsimd", "memset", [], ["cmask"], cmask[:], 0.0)
            P.op("gpsimd", "affine_select", ["cmask"], ["cmask"], out=cmask[:], in_=cmask[:], pattern=[[-1, 128]],
                 compare_op=ALU.is_ge, fill=NEG, base=0, channel_multiplier=1)
            osT = sb("sa_osT", [128, 4, LP], BF16)
            sc_ps = Ring(ps, "sa_scps", [128, 512], F32, 2)
            s_ps = Ring(ps, "sa_sps", [128, 512], F32, 3)
            o_ps = [ps("sa_ops%d" % i, [128, 512]) for i in range(2)]
            pTb = ps("sa_pTb")
            relu_b = Ring(sb, "sa_relu", [128, 512], F32, 3)
            accb = Ring(sb, "sa_acc", [128, LP], F32, 2)
            mbb = Ring(sb, "sa_mb", [128, LP], BF16, 4)
            junkb = Ring(sb, "sa_junk", [128, LP], BF16, 2)
            ptb = Ring(sb, "sa_pt", [128, 512], BF16, 3)
            stt = Ring(sb, "sa_st", [128, 8], F32, 2)
            wtb = Ring(sb, "sa_wt", [128, NIT + 2], F32, 2)
            ftab = sb("sa_ftab", [128, NIT + 2])
            for n_ in range(NIT + 2):
                P.op("gpsimd", "memset", [], ["ftab"], ftab[:, n_:n_ + 1], 2.0 ** (-n_))
            osb = Ring(sb, "sa_os", [128, 512], BF16, 2)
            rdb = Ring(sb, "sa_rd", [128, 8], F32, 2)

            def hrows(h):
                return slice(0, 64) if h % 2 == 0 else slice(64, 128)

            mb_of = {}

            def scores(i):
                nk = 128 * (i + 1)
                qs_ = slice(i * 128, (i + 1) * 128)
                acc, acck = accb.next()
                for h in range(8):
                    for k0 in range(0, nk, 512):
                        kn = min(512, nk - k0)
                        pt, pk = sc_ps.next()
                        P.op("tensor", "matmul", ["iq", "ik"], [pk], pt[:, 0:kn], lhsT=iq[hrows(h), h // 2, qs_],
                             rhs=ik[hrows(h), k0:k0 + kn], start=True, stop=True)
                        if h == 0:
                            r, rk = relu_b.next()
                            P.op("scalar", "activation", [pk], [rk], out=r[:, 0:kn], in_=pt[:, 0:kn], func=AF.Relu)
                            P.op("vector", "tensor_scalar", [rk, "sasm"], [acck], out=acc[:, k0:k0 + kn], in0=r[:, 0:kn],
                                 scalar1=sm[:, i, 8:9], scalar2=None, op0=ALU.mult)
                        else:
                            r, rk = relu_b.next()
                            P.op("scalar", "activation", [pk], [rk], out=r[:, 0:kn], in_=pt[:, 0:kn], func=AF.Relu)
                            P.op("vector", "scalar_tensor_tensor", [rk, "sasm", acck], [acck], out=acc[:, k0:k0 + kn], in0=r[:, 0:kn],
                                 scalar=sm[:, i, 8 + h:9 + h], in1=acc[:, k0:k0 + kn], op0=ALU.mult, op1=ALU.add)
                    yield
                P.op("gpsimd", "affine_select", [acck], [acck], out=acc[:, i * 128:nk], in_=acc[:, i * 128:nk], pattern=[[-1, 128]],
                     compare_op=ALU.is_ge, fill=-1e30, base=0, channel_multiplier=1)
                mb, mbk = mbb.next()
                s8, s8k = stt.next()
                if nk <= KTOP:
                    P.op("vector", "memset", [], [s8k], s8[:, 0:1], -1e29)
                else:
                    jk, jkk = junkb.next()
                    P.op("vector", "tensor_reduce", [acck], [s8k], out=s8[:, 5:6], in_=acc[:, 0:nk], axis=mybir.AxisListType.X, op=ALU.max)
                    P.op("vector", "tensor_reduce", [acck], [s8k], out=s8[:, 0:1], in_=acc[:, 0:i * 128], axis=mybir.AxisListType.X, op=ALU.min)
                    P.op("vector", "tensor_tensor", [s8k], [s8k], out=s8[:, 1:2], in0=s8[:, 5:6], in1=s8[:, 0:1], op=ALU.subtract)
                    P.op("vector", "tensor_scalar", [s8k], [s8k], out=s8[:, 1:2], in0=s8[:, 1:2], scalar1=1.0001, scalar2=1e-6,
                         op0=ALU.mult, op1=ALU.add)
                    wt, wtk = wtb.next()
                    P.op("vector", "tensor_scalar", ["ftab", s8k], [wtk], out=wt[:], in0=ftab[:], scalar1=s8[:, 1:2], scalar2=None,
                         op0=ALU.mult)
                    P.op("vector", "tensor_tensor", [s8k, wtk], [s8k], out=s8[:, 2:3], in0=s8[:, 0:1], in1=wt[:, 1:2], op=ALU.add)
                    for it in range(1, NIT + 1):
                        P.op("vector", "tensor_scalar", [acck, s8k], [jkk, s8k], out=jk[:, 0:nk], in0=acc[:, 0:nk], scalar1=s8[:, 2:3],
                             scalar2=None, op0=ALU.is_ge, op1=ALU.add, accum_out=s8[:, 3:4])
                        P.op("vector", "tensor_scalar", [s8k], [s8k], out=s8[:, 4:5], in0=s8[:, 3:4], scalar1=KTOP - 0.5, scalar2=0.5,
                             op0=ALU.is_ge, op1=ALU.subtract)
                        P.op("vector", "scalar_tensor_tensor", [s8k, wtk], [s8k], out=s8[:, 2:3], in0=s8[:, 4:5], scalar=wt[:, it:it + 1],
                             in1=s8[:, 2:3], op0=ALU.mult, op1=ALU.add)
                        yield
                    P.op("vector", "tensor_tensor", [s8k, wtk], [s8k], out=s8[:, 0:1], in0=s8[:, 2:3], in1=wt[:, NIT + 1:NIT + 2],
                         op=ALU.subtract)
                P.op("vector", "tensor_scalar", [acck, s8k], [mbk], out=mb[:, 0:nk], in0=acc[:, 0:nk], scalar1=s8[:, 0:1], scalar2=NEG,
                     op0=ALU.is_lt, op1=ALU.mult)
                mb_of[i] = (mb, mbk)
                yield

            def attention(i):
                mb, mbk = mb_of.pop(i)
                qs_ = slice(i * 128, (i + 1) * 128)
                groups = [(kt, hg) for kt in range(i + 1) for hg in range(2)]
                pend = None
                for g in groups + [None]:
                    cur = None
                    if g is not None:
                        kt, hg = g
                        ks_ = slice(kt * 128, (kt + 1) * 128)
                        sp, spk = s_ps.next()
                        for pair in ((0, 2), (1, 3)):
                            for hh in pair:
                                h = hg * 4 + hh
                                P.op("tensor", "matmul", ["sak", "saq"], [spk], sp[:, hh * 128:(hh + 1) * 128],
                                     lhsT=sak[hrows(h), h // 2, ks_], rhs=saq[hrows(h), h // 2, qs_], start=(hh == 0), stop=False,
                                     skip_group_check=True)
                            for hh in pair:
                                P.op("tensor", "matmul", [mbk, "identb"], [spk], sp[:, hh * 128:(hh + 1) * 128],
                                     lhsT=mb[:, ks_], rhs=identb[:], start=False, stop=True, skip_group_check=True)
                        pt, ptk = ptb.next()
                        P.op("scalar", "activation", [spk], [ptk], out=pt[:], in_=sp[:], func=AF.Exp, scale=0.125)
                        cur = (kt, hg, pt, ptk)
                    if pend is not None:
                        kt, hg, pt, ptk = pend
                        for hh in range(4):
                            h = hg * 4 + hh
                            P.op("tensor", "matmul", [ptk, "va"], ["sa_ops%d" % hg], o_ps[hg][:, hh * 65:(hh + 1) * 65],
                                 lhsT=pt[:, hh * 128:(hh + 1) * 128], rhs=va[:, kt, h, :], start=(kt == 0 and hh == 0), stop=(kt == i),
                                 skip_group_check=True)
                    pend = cur
                    yield
                rd, rdk = rdb.next()
                for hg in range(2):
                    P.op("vector", "reciprocal", ["sa_ops%d" % hg], [rdk], out=rd[:, hg * 4:(hg + 1) * 4],
                         in_=o_ps[hg][:, 0:260].rearrange("p (h d) -> p h d", d=65)[:, :, 64])
                os_, osk = osb.next()
                for h in range(8):
                    hg, hh = h // 4, h % 4
                    P.op("vector", "tensor_scalar", ["sa_ops%d" % hg, rdk], [osk], out=os_[:, h * 64:(h + 1) * 64],
                         in0=o_ps[hg][:, hh * 65:hh * 65 + 64], scalar1=rd[:, h:h + 1], scalar2=None, op0=ALU.mult)
                for c in range(4):
                    P.op("tensor", "matmul", [osk, "identb"], ["sa_pTb"], pTb[:, c * 128:(c + 1) * 128],
                         lhsT=os_[:, c * 128:(c + 1) * 128], rhs=identb[:], start=True, stop=True)
                P.op("scalar", "copy", ["sa_pTb"], [("osT", i)], out=osT[:, :, qs_],
                     in_=pTb[:, 0:512].rearrange("p (h c) -> p h c", h=4))
                yield

            def att_chain(ts):
                for t_ in ts:
                    yield from attention(t_)

            for i in range(0, NT + 2, 2):
                gens = [scores(t_) for t_ in (i, i + 1) if t_ < NT]
                prev = [t_ for t_ in (i - 2, i - 1) if 0 <= t_ < NT]
                if prev:
                    gens.append(att_chain(prev))
                interleave(gens)
            for c in range(4):
                P.dma("sync", [("osT", t) for t in range(NT)], [("mixT", 4 + c)], out=mixT_d[4 + c, :, :], in_=osT[:, c, :])
            P.flush()

    def stage_mix_ln1(l, src, hT, comb):
        with ExitStack() as st:
            sb, ps = stage_allocs(st)
            mixT = sb("mx_mixT", [128, 8, LP], BF16)
            for c in range(8):
                P.dma("sync", [("mixT", c)], ["mixT"], out=mixT[:, c, :], in_=mixT_d[c, :, :])
            wo = sb("mx_wo", [128, 8, D], BF16)
            wol = w_out[l].rearrange("(c p) n -> p c n", p=128)
            for c in range(8):
                P.dma("gpsimd", [], ["wo"], out=wo[:, c, :], in_=wol[:, c, :])
            g_rep = sb("mx_g", [128, D])
            b_rep = sb("mx_b", [128, D])
            P.dma("sync", [], ["lng"], out=g_rep[:], in_=ln1g[l, :, :])
            P.dma("sync", [], ["lnb"], out=b_rep[:], in_=ln1b[l, :, :])
            wrs = sb("mx_wr", [128, 8, 36])
            P.dma("sync", [], ["wrs"], out=wrs[:], in_=wr[l].rearrange("(c p) n -> p c n", p=128))
            br = sb("mx_br", [128, 36])
            P.dma("sync", [], ["br"], out=br[:], in_=brep[l, :, :])
            hin = Ring(sb, "mx_hin", [128, D], F32, 2)
            tb = Ring(sb, "mx_t", [128, D], F32, 2)
            ob = Ring(sb, "mx_o", [128, D], F32, 2)
            scr = sb("mx_scr", [128, D])
            stb = Ring(sb, "mx_st", [128, 4], F32, 2)
            hTf = Ring(sb, "mx_hTf", [128, 8, 128], F32, 2)
            pM = [ps("mx_pM%d" % i, [128, 1024]) for i in range(2)]
            pT = [ps("mx_pT%d" % i, [128, 1024]) for i in range(1)]
            pLs = [ps("mx_pL%d" % i, [128, 512]) for i in range(2)]
            rt = Ring(sb, "mx_rt", [128, 160], F32, 2)
            def mtile(t):
                cs = slice(t * 128, (t + 1) * 128)
                pL, pLk = pLs[t % 2], "mx_pL%d" % (t % 2)
                pm, pmk = pM[t % 2], "mx_pM%d" % (t % 2)
                for n in range(2):
                    for c in range(8):
                        P.op("tensor", "matmul", ["mixT", "wo"], [pmk], pm[:, n * 512:(n + 1) * 512], lhsT=mixT[:, c, cs],
                             rhs=wo[:, c, n * 512:(n + 1) * 512], start=(c == 0), stop=(c == 7))
                hi_, hik = hin.next()
                P.dma("sync", [("h", t)], [hik], out=hi_[:], in_=src[t * 128:(t + 1) * 128, :])
                tt, ttk = tb.next()
                P.op("vector", "scalar_tensor_tensor", [hik, pmk], [ttk], out=tt[:], in0=hi_[:], scalar=ALPHA, in1=pm[:],
                     op0=ALU.mult, op1=ALU.add)
                o, ok = ob.next()
                s4, s4k = stb.next()
                yield
                yield from ln_tile(tt, ttk, g_rep, b_rep, o, ok, scr, "mx_scr", s4, s4k)
                P.dma("sync", [ok], [("h", t)], out=h_d[t * 128:(t + 1) * 128, :], in_=o[:])
                yield
                pt, ptk = pT[0], "mx_pT0"
                for c in range(8):
                    P.op("tensor", "transpose", [ok, "ident"], [ptk], out=pt[:, c * 128:(c + 1) * 128], in_=o[:, c * 128:(c + 1) * 128],
                         identity=ident[:])
                hf, hfk = hTf.next()
                P.op("scalar", "copy", [ptk], [hfk], out=hf[:].rearrange("p c t -> p (c t)"), in_=pt[:])
                P.op("gpsimd", "tensor_copy", [hfk], [("hT", t)], out=hT[:, :, cs], in_=hf[:])
                yield
                for c in range(8):
                    P.op("tensor", "matmul", [hfk, "wrs"], [pLk], pL[:, 0:36], lhsT=hf[:, c, :], rhs=wrs[:, c, :],
                         start=(c == 0), stop=(c == 7))
                yield
                r, rk = rt.next()
                P.op("vector", "tensor_tensor", [pLk, "br"], [rk], out=r[:, 0:36], in0=pL[:, 0:36], in1=br[:], op=ALU.add)
                P.op("vector", "tensor_reduce", [rk], [rk], out=r[:, 148:149], in_=r[:, 0:4], axis=mybir.AxisListType.X, op=ALU.max)
                P.op("vector", "tensor_scalar", [rk], [rk], out=r[:, 149:150], in0=r[:, 148:149], scalar1=-1.0, scalar2=None, op0=ALU.mult)
                P.op("scalar", "activation", [rk], [rk], out=r[:, 36:40], in_=r[:, 0:4], func=AF.Exp, bias=r[:, 149:150], scale=1.0,
                     accum_out=r[:, 150:151])
                P.op("vector", "reciprocal", [rk], [rk], out=r[:, 151:152], in_=r[:, 150:151])
                P.op("vector", "tensor_scalar", [rk], [rk], out=r[:, 44:48], in0=r[:, 0:4], scalar1=r[:, 148:149], scalar2=None,
                     op0=ALU.is_ge)
                P.op("vector", "tensor_scalar", [rk], [rk], out=r[:, 48:52], in0=r[:, 44:48], scalar1=-1.0, scalar2=1e30,
                     op0=ALU.add, op1=ALU.mult)
                for gq in range(4):
                    P.op("vector", "tensor_scalar", [rk], [rk], out=r[:, 52 + gq * 8:60 + gq * 8], in0=r[:, 4 + gq * 8:12 + gq * 8],
                         scalar1=r[:, 48 + gq:49 + gq], scalar2=None, op0=ALU.add)
                yield
                P.op("vector", "max", [rk], [rk], out=r[:, 36:44], in_=r[:, 52:84])
                P.op("vector", "tensor_scalar", [rk], [rk], out=r[:, 84:116], in0=r[:, 52:84], scalar1=r[:, 36:37], scalar2=None,
                     op0=ALU.is_equal)
                P.op("vector", "tensor_scalar", [rk], [rk], out=r[:, 116:148], in0=r[:, 52:84], scalar1=r[:, 37:38], scalar2=None,
                     op0=ALU.is_equal)
                P.op("vector", "tensor_tensor", [rk], [rk], out=r[:, 152:153], in0=r[:, 37:38], in1=r[:, 36:37], op=ALU.subtract)
                yield
                P.op("scalar", "activation", [rk], [rk], out=r[:, 153:154], in_=r[:, 152:153], func=AF.Exp)
                P.op("vector", "tensor_scalar", [rk], [rk], out=r[:, 154:155], in0=r[:, 153:154], scalar1=1.0, scalar2=None, op0=ALU.add)
                P.op("vector", "reciprocal", [rk], [rk], out=r[:, 154:155], in_=r[:, 154:155])
                P.op("vector", "tensor_tensor", [rk], [rk], out=r[:, 155:156], in0=r[:, 154:155], in1=r[:, 151:152], op=ALU.mult)
                P.op("vector", "tensor_tensor", [rk], [rk], out=r[:, 156:157], in0=r[:, 155:156], in1=r[:, 153:154], op=ALU.mult)
                P.op("vector", "tensor_scalar", [rk], [("comb", t)], out=comb[:, t, :], in0=r[:, 84:116], scalar1=r[:, 155:156],
                     scalar2=None, op0=ALU.mult)
                P.op("vector", "scalar_tensor_tensor", [rk, ("comb", t)], [("comb", t)], out=comb[:, t, :], in0=r[:, 116:148],
                     scalar=r[:, 156:157], in1=comb[:, t, :], op0=ALU.mult, op1=ALU.add)
            for t0 in range(0, NT, 2):
                interleave([mtile(t) for t in (t0, t0 + 1) if t < NT])
            P.flush()

    def stage_moe(l, hT, comb, yacc):
        with ExitStack() as st:
            sb, ps = stage_allocs(st)
            w1b = Ring(sb, "mo_w1", [128, 8, 256], BF16, 3)
            w3b = Ring(sb, "mo_w3", [128, 8, 256], BF16, 3)
            w2b = Ring(sb, "mo_w2", [128, 2, D], BF16, 3)
            hidb = Ring(sb, "mo_hid", [128, 2, LP], BF16, 2)
            silb = Ring(sb, "mo_sil", [128, 512], F32, 3)
            stg = Ring(sb, "mo_stg", [128, 2048], F32, 3)
            p1 = Ring(ps, "mo_p1", [128, 512], F32, 2)
            p3 = Ring(ps, "mo_p3", [128, 512], F32, 2)
            pY = [ps("mo_pY%d" % i, [128, 1024]) for i in range(2)]
            HT = [("hT", t) for t in range(NT)]
            yi = [0]
            wts = {}

            def hphase(e):
                a1, a1k = w1b.next()
                a3, a3k = w3b.next()
                a2, a2k = w2b.next()
                for (dst_, dkey_, src_, c_) in ((a1, a1k, w1[l, e].rearrange("(c p) f -> p c f", p=128), 8),
                                                (a3, a3k, w3[l, e].rearrange("(c p) f -> p c f", p=128), 8),
                                                (a2, a2k, w2[l, e].rearrange("(c p) n -> p c n", p=128), 2)):
                    sg, sgk = stg.next()
                    sv_ = sg[:].rearrange("p (c f) -> p c f", c=c_)
                    P.dma("sync", [], [sgk], out=sv_, in_=src_)
                    P.op("gpsimd", "tensor_copy", [sgk], [dkey_], out=dst_[:], in_=sv_)
                hid, hidk = hidb.next()
                for fc in range(2):
                    for (n0, nn) in NTILES:
                        q1, q1k = p1.next()
                        q3, q3k = p3.next()
                        for k in range(8):
                            P.op("tensor", "matmul", [a1k] + HT, [q1k], q1[:, 0:nn], lhsT=a1[:, k, fc * 128:(fc + 1) * 128],
                                 rhs=hT[:, k, n0:n0 + nn], start=(k == 0), stop=(k == 7))
                        for k in range(8):
                            P.op("tensor", "matmul", [a3k] + HT, [q3k], q3[:, 0:nn], lhsT=a3[:, k, fc * 128:(fc + 1) * 128],
                                 rhs=hT[:, k, n0:n0 + nn], start=(k == 0), stop=(k == 7))
                        s, sk = silb.next()
                        P.op("scalar", "activation", [q1k], [sk], out=s[:, 0:nn], in_=q1[:, 0:nn], func=AF.Silu)
                        P.op("vector", "tensor_tensor", [sk, q3k], [hidk], out=hid[:, fc, n0:n0 + nn], in0=s[:, 0:nn], in1=q3[:, 0:nn],
                             op=ALU.mult)
                        yield
                wts[e] = (hid, hidk, a2, a2k)

            def yphase(e):
                hid, hidk, a2, a2k = wts.pop(e)
                for t in range(NT):
                    cs = slice(t * 128, (t + 1) * 128)
                    py, pyk = pY[yi[0] % 2], "mo_pY%d" % (yi[0] % 2)
                    yi[0] += 1
                    for n in range(2):
                        for fc in range(2):
                            P.op("tensor", "matmul", [hidk, a2k], [pyk], py[:, n * 512:(n + 1) * 512], lhsT=hid[:, fc, cs],
                                 rhs=a2[:, fc, n * 512:(n + 1) * 512], start=(fc == 0), stop=(fc == 1))
                    if e == 0:
                        P.op("vector", "tensor_scalar", [pyk, ("comb", t)], [("yacc", t)], out=yacc[:, t, :], in0=py[:],
                             scalar1=comb[:, t, e:e + 1], scalar2=None, op0=ALU.mult)
                    else:
                        P.op("vector", "scalar_tensor_tensor", [pyk, ("comb", t), ("yacc", t)], [("yacc", t)], out=yacc[:, t, :],
                             in0=py[:], scalar=comb[:, t, e:e + 1], in1=yacc[:, t, :], op0=ALU.mult, op1=ALU.add)
                    yield

            interleave([hphase(0)])
            for e in range(NE):
                gens = [yphase(e)]
                if e + 1 < NE:
                    gens.append(hphase(e + 1))
                interleave(gens)
            P.flush()

    def stage_ln2(l, yacc, last):
        with ExitStack() as st:
            sb, ps = stage_allocs(st)
            g_rep = sb("l2_g", [128, D])
            b_rep = sb("l2_b", [128, D])
            P.dma("sync", [], ["lng"], out=g_rep[:], in_=ln2g[l, :, :])
            P.dma("sync", [], ["lnb"], out=b_rep[:], in_=ln2b[l, :, :])
            hin = Ring(sb, "l2_hin", [128, D], F32, 4)
            tb = Ring(sb, "l2_t", [128, D], F32, 4)
            ob = Ring(sb, "l2_o", [128, D], F32, 4)
            scrb = Ring(sb, "l2_scr", [128, D], BF16, 4)
            stb = Ring(sb, "l2_st", [128, 4], F32, 4)
            def ltile(t):
                hi_, hik = hin.next()
                P.dma("sync", [("h", t)], [hik], out=hi_[:], in_=h_d[t * 128:(t + 1) * 128, :])
                tt, ttk = tb.next()
                P.op("vector", "scalar_tensor_tensor", [hik, ("yacc", t)], [ttk], out=tt[:], in0=hi_[:], scalar=ALPHA,
                     in1=yacc[:, t, :], op0=ALU.mult, op1=ALU.add)
                o, ok = ob.next()
                s4, s4k = stb.next()
                scr, scrk = scrb.next()
                yield
                yield from ln_tile(tt, ttk, g_rep, b_rep, o, ok, scr, scrk, s4, s4k)
                if not last:
                    P.dma("sync", [ok], [("h", t)], out=h_d[t * 128:(t + 1) * 128, :], in_=o[:])
                else:
                    if t == 0:
                        P.dma("sync", [ok], [("y", t)], out=y[0:112, :], in_=o[16:128, :])
                    elif t < 16:
                        P.dma("sync", [ok], [("y", t)], out=y[t * 128 - 16:t * 128 + 112, :], in_=o[:])
                    else:
                        P.dma("sync", [ok], [("y", t)], out=y[2032:2048, :], in_=o[0:16, :])
            for t0 in range(0, NT, 4):
                interleave([ltile(t) for t in range(t0, t0 + 4) if t < NT])
            P.flush()

    stages = []
    res = ExitStack()
    rsb, _ = stage_allocs(res)
    hT = rsb("hT", [128, 8, LP], BF16)
    done = False

    def want(name, l):
        nonlocal done
        if done:
            return False
        if only is not None and (name, l) not in only:
            return False
        if stop_after is not None and stop_after == (name, l):
            done = True
        return True

    for l in range(n_layers):
        src = h0 if l == 0 else h_d
        if want("hT", l):
            stage_hT(src, hT)
        if want("proj", l):
            stage_proj(l, hT)
        if want("dn", l):
            stage_dn(l)
        if want("dsa", l):
            stage_dsa(l)
        moe_st = ExitStack()
        msb, _ = stage_allocs(moe_st)
        comb = msb("comb", [128, NT, NE])
        if want("mix", l):
            stage_mix_ln1(l, src, hT, comb)
        yacc = msb("yacc", [128, NT, D])
        if want("moe", l):
            stage_moe(l, hT, comb, yacc)
        if want("ln2", l):
            stage_ln2(l, yacc, last=(l == n_layers - 1))
        moe_st.close()
    P.finish([("y", t) for t in range(NT)])
    res.close()
    top.close()
    return nc


def _rope_tables():
    inv = 1.0 / (10000.0 ** (np.arange(0, 64, 2, dtype=np.float32) / np.float32(64)))
    pos = np.arange(LP, dtype=np.float32)
    ang = pos[:, None] * inv[None, :].astype(np.float32)
    ang = np.concatenate([ang, ang], -1)
    cos = np.cos(ang).astype(np.float32)
    sin = np.sin(ang).astype(np.float32)
    sgn = np.concatenate([-np.ones(32, np.float32), np.ones(32, np.float32)])
    sins = sin * sgn[None, :]
    cosT = np.ascontiguousarray(np.concatenate([cos.T, cos.T], 0))
    sinT = np.ascontiguousarray(np.concatenate([sins.T, sins.T], 0))
    return cosT, sinT


def make_shared(inp):
    f = lambda a: np.ascontiguousarray(np.asarray(a, dtype=np.float32))
    rep = lambda a: f(np.broadcast_to(np.asarray(a, np.float32)[:, None, :], (DEPTH, 128, np.asarray(a).shape[-1])))
    cosT, sinT = _rope_tables()
    cw = np.asarray(inp["conv_w"], np.float32)
    cwT = f(cw.reshape(DEPTH, 4, 12, 128).transpose(0, 3, 2, 1).reshape(DEPTH, 128, 48))
    sh = {
        "w_in": f(inp["w_in"]), "w_out": f(inp["w_out"]),
        "w1": f(inp["w1"]), "w3": f(inp["w3"]), "w2": f(inp["w2"]),
        "wr": f(np.concatenate([np.asarray(inp["w_grp"], np.float32), np.asarray(inp["w_rtr"], np.float32)], -1)),
        "brep": rep(np.concatenate([np.asarray(inp["b_grp"], np.float32), np.asarray(inp["b_rtr"], np.float32)], -1)),
        "cwT": cwT,
        "alog": rep(np.tile(np.asarray(inp["a_log"], np.float32), (1, NT))),
        "dtb": rep(np.tile(np.asarray(inp["dt_bias"], np.float32), (1, NT))),
        "ngr": rep(np.tile(np.asarray(inp["dn_norm_g"], np.float32), (1, 4))),
        "ln1g": rep(inp["ln1_g"]), "ln1b": rep(inp["ln1_b"]), "ln2g": rep(inp["ln2_g"]), "ln2b": rep(inp["ln2_b"]),
        "ropec": cosT, "ropes": sinT,
    }
    return sh


def make_h0(x_b, meta):
    h0 = np.zeros((LP, D), np.float32)
    h0[:NMETA] = meta
    h0[NMETA:L] = x_b
    return h0


_NC_CACHE = {}


def kernel(**inputs):
    x = np.asarray(inputs["x"], np.float32)
    meta = np.asarray(inputs["meta_tokens"], np.float32)
    sh = make_shared(inputs)
    if "nc" not in _NC_CACHE:
        _NC_CACHE["nc"] = build()
    nc = _NC_CACHE["nc"]
    in_maps = []
    for b in range(8):
        m = dict(sh)
        m["h0"] = make_h0(x[b], meta)
        in_maps.append(m)
    res = run_bass_kernel_spmd(nc, in_maps, core_ids=list(range(8)))
    return np.stack([np.asarray(r["y"], np.float32) for r in res.results], 0)
```

```python
import numpy as np
from contextlib import ExitStack
import concourse.bass as bass
import concourse.mybir as mybir
from concourse.bass_utils import run_bass_kernel_spmd

F32 = mybir.dt.float32
BF16 = mybir.dt.bfloat16
AF = mybir.ActivationFunctionType
ALU = mybir.AluOpType

ENGS = ("tensor", "vector", "scalar", "gpsimd", "sync")
N_DMA_SEMS = 12

D = 1024
SEQ = 2048
NMETA = 16
L = SEQ + NMETA
NT = 17
LP = NT * 128
DEPTH = 2
DIN = 4176
ALPHA = (2.0 * DEPTH) ** 0.25
NEG = -30000.0
KTOP = 256
NIT = 20
NE = 32
DN_CUT = 0
PREP_CUT = 0
NTILES = [(0, 512), (512, 512), (1024, 512), (1536, 512), (2048, 128)]


class Prog:
    def __init__(self, nc, stack):
        self.nc = nc
        self.streams = {e: [] for e in ENGS}
        self.esem = {e: stack.enter_context(nc.semaphore("s_" + e)) for e in ENGS}
        self.eseq = {e: 0 for e in ENGS}
        self.eval_ = {e: 0 for e in ENGS}
        self.dsem = {e: [stack.enter_context(nc.semaphore("d_%s%d" % (e, i)))
                         for i in range(N_DMA_SEMS)] for e in ("sync", "gpsimd", "scalar")}
        self.dcnt = {e: [0] * N_DMA_SEMS for e in self.dsem}
        self.drr = {e: 0 for e in self.dsem}
        self.waited = {e: {} for e in ENGS}
        self.last_w = {}
        self.readers = {}
        self.n_ops = 0
        self.excl = set()

    @staticmethod
    def _sk(src):
        return src if isinstance(src, str) else id(src)

    def _need(self, eng, ev, out):
        if ev is None:
            return
        src, val = ev
        if eng == "tensor" and src == "tensor":
            return
        k = self._sk(src)
        if self.waited[eng].get(k, 0) >= val:
            return
        self.waited[eng][k] = val
        out.append((src, val))

    def _deps(self, eng, reads, writes):
        waits = []
        for k in reads:
            self._need(eng, self.last_w.get(k), waits)
        for k in writes:
            self._need(eng, self.last_w.get(k), waits)
            for ev in self.readers.get(k, {}).values():
                self._need(eng, ev, waits)
        return waits

    def _commit(self, ev, reads, writes):
        for k in reads:
            self.readers.setdefault(k, {})[self._sk(ev[0])] = ev
        for k in writes:
            self.last_w[k] = ev
            self.readers[k] = {}

    def op(self, eng, meth, reads, writes, *args, **kw):
        if eng != "tensor":
            ex = [k for k in reads if isinstance(k, str) and k in self.excl and k not in writes]
            if ex:
                writes = list(writes) + ex
        waits = self._deps(eng, reads, writes)
        self.eseq[eng] += 1
        ev = (eng, self.eseq[eng])
        self.streams[eng].append((waits, meth, args, kw, "E", ev[1]))
        self._commit(ev, reads, writes)
        self.n_ops += 1

    def dma(self, q, reads, writes, out, in_, **kw):
        i = self.drr[q]
        self.drr[q] = (i + 1) % N_DMA_SEMS
        sem = self.dsem[q][i]
        waits = self._deps(q, reads, writes)
        if self.dcnt[q][i] > 0:
            self._need(q, (sem, 16 * self.dcnt[q][i]), waits)
        self.dcnt[q][i] += 1
        ev = (sem, 16 * self.dcnt[q][i])
        kw = dict(kw)
        kw["out"] = out
        kw["in_"] = in_
        self.streams[q].append((waits, "dma_start", (), kw, "D", sem))
        self._commit(ev, reads, writes)
        self.n_ops += 1

    def finish(self, final_keys):
        waits = []
        for k in final_keys:
            self._need("sync", self.last_w.get(k), waits)
        self.streams["sync"].append((waits, None, (), {}, None, None))
        self.flush()

    def barrier(self):
        evs = [(e, self.eseq[e]) for e in ENGS if self.eseq[e] > 0]
        for q in self.dsem:
            for i in range(N_DMA_SEMS):
                if self.dcnt[q][i] > 0:
                    evs.append((self.dsem[q][i], 16 * self.dcnt[q][i]))
        for e in ENGS:
            waits = []
            for ev in evs:
                self._need(e, ev, waits)
            if waits:
                self.streams[e].append((waits, None, (), {}, None, None))

    def flush(self):
        self.barrier()
        nc = self.nc
        streams = self.streams
        self.streams = {e: [] for e in ENGS}
        targets = {e: set() for e in ENGS}
        for e in ENGS:
            for waits, meth, args, kw, kind, x in streams[e]:
                for (src, val) in waits:
                    if isinstance(src, str):
                        targets[src].add(val)
        value_of = {}
        for e in ENGS:
            for waits, meth, args, kw, kind, x in streams[e]:
                if kind == "E" and x in targets[e]:
                    self.eval_[e] += 1
                    value_of[(e, x)] = self.eval_[e]
        for e in ENGS:
            for t in targets[e]:
                assert (e, t) in value_of, ("wait target from an earlier flush", e, t)
        esem = self.esem
        with nc.Block() as block:
            def run(engname):
                def body(eng):
                    for waits, meth, args, kw, kind, x in streams[engname]:
                        for (src, val) in waits:
                            if isinstance(src, str):
                                eng.wait_ge(esem[src], value_of[(src, val)])
                            else:
                                eng.wait_ge(src, val)
                        if meth is not None:
                            ins = getattr(eng, meth)(*args, **kw)
                            if kind == "D":
                                ins.then_inc(x, 16)
                            elif (engname, x) in value_of:
                                ins.then_inc(esem[engname], 1)
                return body
            block.tensor(run("tensor"))
            block.vector(run("vector"))
            block.scalar(run("scalar"))
            block.gpsimd(run("gpsimd"))
            block.sync(run("sync"))


class Ring:
    def __init__(self, alloc, name, shape, dt, n):
        self.bufs = [alloc(name + str(i), shape, dt) for i in range(n)]
        self.keys = [name + str(i) for i in range(n)]
        self.i = 0

    def next(self):
        i = self.i
        self.i = (i + 1) % len(self.bufs)
        return self.bufs[i], self.keys[i]


def interleave(gens):
    gens = list(gens)
    while gens:
        for g in list(gens):
            try:
                next(g)
            except StopIteration:
                gens.remove(g)


def build(debug=False, stop_after=None, n_layers=DEPTH, only=None):
    nc = bass.Bass("TRN2", target_bir_lowering=False)
    dk = "ExternalOutput" if debug else "Internal"

    def din(name, shape, dt=F32):
        return nc.dram_tensor(name, list(shape), dt, kind="ExternalInput").ap()

    def dscr(name, shape, dt=F32):
        return nc.dram_tensor(name, list(shape), dt, kind=dk).ap()

    h0 = din("h0", [LP, D])
    w_in = din("w_in", [DEPTH, D, DIN])
    w_out = din("w_out", [DEPTH, D, D])
    w1 = din("w1", [DEPTH, NE, D, 256])
    w3 = din("w3", [DEPTH, NE, D, 256])
    w2 = din("w2", [DEPTH, NE, 256, D])
    wr = din("wr", [DEPTH, D, 36])
    brep = din("brep", [DEPTH, 128, 36])
    cwT = din("cwT", [DEPTH, 128, 48])
    alog = din("alog", [DEPTH, 128, 68])
    dtb = din("dtb", [DEPTH, 128, 68])
    ngr = din("ngr", [DEPTH, 128, 512])
    ln1g = din("ln1g", [DEPTH, 128, D])
    ln1b = din("ln1b", [DEPTH, 128, D])
    ln2g = din("ln2g", [DEPTH, 128, D])
    ln2b = din("ln2b", [DEPTH, 128, D])
    ropec = din("ropec", [128, LP])
    ropes = din("ropes", [128, LP])
    y = nc.dram_tensor("y", [SEQ, D], F32, kind="ExternalOutput").ap()

    h_d = dscr("h_d", [LP, D])
    qkvT_d = dscr("qkvT_d", [12, 128, LP])
    ropeT_d = dscr("ropeT_d", [13, 128, LP], BF16)
    z_d = dscr("z_d", [LP, 512])
    sv_d = dscr("sv_d", [LP, 512], BF16)
    sm_d = dscr("sm_d", [LP, 16])
    mixT_d = dscr("mixT_d", [8, 128, LP], BF16)

    top = ExitStack()
    P = Prog(nc, top)

    uid = [0]

    def uname(name):
        uid[0] += 1
        return "%s_u%d" % (name, uid[0])

    def stage_allocs(st):
        def sb(name, shape, dt=F32):
            return st.enter_context(nc.sbuf_tensor(uname(name), list(shape), dt))

        def ps(name, shape=(128, 512), dt=F32):
            P.excl.add(name)
            return st.enter_context(nc.psum_tensor(uname(name), list(shape), dt))
        return sb, ps

    csb, _ = stage_allocs(top)
    ident = csb("ident", [128, 128])
    identb = csb("identb", [128, 128], BF16)
    ones = csb("ones", [128, 128])
    negones = csb("negones", [128, 128])
    P.op("gpsimd", "memset", [], ["ident"], ident[:], 1.0)
    P.op("gpsimd", "affine_select", ["ident"], ["ident"], out=ident[:], in_=ident[:], pattern=[[-1, 128]],
         compare_op=ALU.is_equal, fill=0.0, base=0, channel_multiplier=1)
    P.op("gpsimd", "tensor_copy", ["ident"], ["identb"], out=identb[:], in_=ident[:])
    P.op("gpsimd", "memset", [], ["ones"], ones[:], 1.0)
    P.op("gpsimd", "memset", [], ["negones"], negones[:], -1.0)

    def ln_tile(sb_t, tkey, g_rep, b_rep, outt, okey, scr, skey, st2, st2key):
        P.op("scalar", "activation", [tkey], [skey, st2key], out=scr[:], in_=sb_t[:], func=AF.Identity,
             accum_out=st2[:, 0:1])
        yield
        P.op("vector", "tensor_scalar", [st2key], [st2key], out=st2[:, 1:2], in0=st2[:, 0:1], scalar1=-1.0 / D,
             scalar2=None, op0=ALU.mult)
        yield
        P.op("scalar", "activation", [tkey, st2key], [skey, st2key], out=scr[:], in_=sb_t[:], func=AF.Square,
             bias=st2[:, 1:2], scale=1.0, accum_out=st2[:, 2:3])
        yield
        P.op("vector", "tensor_scalar", [st2key], [st2key], out=st2[:, 3:4], in0=st2[:, 2:3], scalar1=1.0 / D,
             scalar2=1e-5, op0=ALU.mult, op1=ALU.add)
        yield
        P.op("scalar", "activation", [st2key], [st2key], out=st2[:, 3:4], in_=st2[:, 3:4], func=AF.Ln)
        P.op("scalar", "activation", [st2key], [st2key], out=st2[:, 3:4], in_=st2[:, 3:4], func=AF.Exp, scale=-0.5)
        yield
        P.op("vector", "tensor_scalar", [tkey, st2key], [okey], out=outt[:], in0=sb_t[:], scalar1=st2[:, 1:2],
             scalar2=st2[:, 3:4], op0=ALU.add, op1=ALU.mult)
        yield
        P.op("vector", "tensor_tensor", [okey, "lng"], [okey], out=outt[:], in0=outt[:], in1=g_rep[:], op=ALU.mult)
        yield
        P.op("vector", "tensor_tensor", [okey, "lnb"], [okey], out=outt[:], in0=outt[:], in1=b_rep[:], op=ALU.add)
        yield

    def stage_hT(src, hT, l_unused=None):
        with ExitStack() as st:
            sb, ps = stage_allocs(st)
            ht = Ring(sb, "ht_in", [128, D], F32, 2)
            pT = [ps("hT_ps%d" % i, [128, 1024]) for i in range(2)]
            for t in range(NT):
                a, ak = ht.next()
                P.dma("sync", [("h", t)], [ak], out=a[:], in_=src[t * 128:(t + 1) * 128, :])
                pt, pk = pT[t % 2], "hT_ps%d" % (t % 2)
                for c in range(8):
                    P.op("tensor", "transpose", [ak, "ident"], [pk], out=pt[:, c * 128:(c + 1) * 128],
                         in_=a[:, c * 128:(c + 1) * 128], identity=ident[:])
                eng = "vector" if t % 2 == 0 else "scalar"
                if eng == "vector":
                    P.op("vector", "tensor_copy", [pk], [("hT", t)], out=hT[:, :, t * 128:(t + 1) * 128],
                         in_=pt[:].rearrange("p (c t) -> p c t", c=8))
                else:
                    P.op("scalar", "copy", [pk], [("hT", t)], out=hT[:, :, t * 128:(t + 1) * 128],
                         in_=pt[:].rearrange("p (c t) -> p c t", c=8))
            P.flush()

    def stage_proj(l, hT):
        with ExitStack() as st:
            sb, ps = stage_allocs(st)
            NC_FM = 38 * 128
            W = sb("Wp", [128, 8, NC_FM + 1040], BF16)
            cosT = sb("cosT", [128, LP])
            sinT = sb("sinT", [128, LP])
            P.dma("sync", [], ["cosT"], out=cosT[:], in_=ropec[:, :])
            P.dma("sync", [], ["sinT"], out=sinT[:], in_=ropes[:, :])
            wl = w_in[l].rearrange("(c p) n -> p c n", p=128)

            wstg = Ring(sb, "pj_wst", [128, 8, 256], F32, 3)

            def wk(c0, n):
                return [("Wc", j) for j in range(c0 // 128, (c0 + n + 127) // 128)]

            def ld(dst0, src0, n, key):
                for o in range(0, n, 256):
                    nn_ = min(256, n - o)
                    sg, sgk = wstg.next()
                    P.dma("sync", [], [sgk], out=sg[:, :, 0:nn_], in_=wl[:, :, src0 + o:src0 + o + nn_])
                    P.op("gpsimd", "tensor_copy", [sgk], wk(dst0 + o, nn_), out=W[:, :, dst0 + o:dst0 + o + nn_], in_=sg[:, :, 0:nn_])

            def ld_perm(dst0, src0, nheads, key):
                for o in range(0, nheads, 4):
                    nh_ = min(4, nheads - o)
                    nn_ = nh_ * 64
                    sg, sgk = wstg.next()
                    P.dma("sync", [], [sgk], out=sg[:, :, 0:nn_], in_=wl[:, :, src0 + o * 64:src0 + o * 64 + nn_])
                    dv = W[:, :, dst0 + o * 64:dst0 + o * 64 + nn_].rearrange("p c (h two j) -> p c h two j", two=2, j=32)
                    sv = sg[:, :, 0:nn_].rearrange("p c (h two j) -> p c h two j", two=2, j=32)
                    for half in range(2):
                        P.op("gpsimd", "tensor_copy", [sgk], wk(dst0 + o * 64, nn_), out=dv[:, :, :, half, :], in_=sv[:, :, :, 1 - half, :])
            ld(0, 0, 1536, ("W", 0))
            ld(12 * 128, 2056, 512, ("W", 1))
            ld_perm(16 * 128, 2056, 8, ("W", 1))
            ld(20 * 128, 2568, 512, ("W", 2))
            ld_perm(24 * 128, 2568, 8, ("W", 2))
            ld(28 * 128, 3592, 512, ("W", 3))
            ld_perm(32 * 128, 3592, 8, ("W", 3))
            ld(36 * 128, 4104, 64, ("W", 4))
            ld(36 * 128 + 64, 4104, 64, ("W", 4))
            ld_perm(37 * 128, 4104, 1, ("W", 4))
            ld_perm(37 * 128 + 64, 4104, 1, ("W", 4))
            T0 = NC_FM
            ld(T0, 1536, 512, ("W", 5))
            ld(T0 + 512, 3080, 512, ("W", 5))
            ld(T0 + 1024, 2048, 8, ("W", 5))
            ld(T0 + 1032, 4168, 8, ("W", 5))
            wkeys = [("W", i) for i in range(6)]

            pbank = Ring(ps, "pj_ps", [128, 512], F32, 4)
            stg32 = Ring(sb, "pj_s32", [128, LP], F32, 2)
            stg16 = Ring(sb, "pj_s16", [128, LP], BF16, 2)
            tmp = Ring(sb, "pj_tmp", [128, 512], F32, 2)

            def wgrp(m):
                return [("Wc", m)]

            def mm_fm(m, n0, nn, pt, pk):
                for k in range(8):
                    P.op("tensor", "matmul", wgrp(m) + [("hT", i) for i in range(n0 // 128, (n0 + nn) // 128)], [pk],
                         pt[:, 0:nn], lhsT=W[:, k, m * 128:(m + 1) * 128], rhs=hT[:, k, n0:n0 + nn],
                         start=(k == 0), stop=(k == 7))
            for m in range(12):
                s, sk = stg32.next()
                for (n0, nn) in NTILES:
                    pt, pk = pbank.next()
                    mm_fm(m, n0, nn, pt, pk)
                    if (n0 // 512) % 2 == 0:
                        P.op("vector", "tensor_copy", [pk], [sk], out=s[:, n0:n0 + nn], in_=pt[:, 0:nn])
                    else:
                        P.op("scalar", "copy", [pk], [sk], out=s[:, n0:n0 + nn], in_=pt[:, 0:nn])
                P.dma("sync", [sk], [("qkvT", m)], out=qkvT_d[m, :, :], in_=s[:])
            rope_src = [12, 13, 14, 15, 20, 21, 22, 23, 28, 29, 30, 31, 36]
            rope_prm = [16, 17, 18, 19, 24, 25, 26, 27, 32, 33, 34, 35, 37]
            for r in range(13):
                s, sk = stg16.next()
                for (n0, nn) in NTILES:
                    pa, pak = pbank.next()
                    mm_fm(rope_src[r], n0, nn, pa, pak)
                    pb, pbk = pbank.next()
                    mm_fm(rope_prm[r], n0, nn, pb, pbk)
                    t1, t1k = tmp.next()
                    t2, t2k = tmp.next()
                    P.op("vector", "tensor_tensor", [pak, "cosT"], [t1k], out=t1[:, 0:nn], in0=pa[:, 0:nn],
                         in1=cosT[:, n0:n0 + nn], op=ALU.mult)
                    P.op("vector", "tensor_tensor", [pbk, "sinT"], [t2k], out=t2[:, 0:nn], in0=pb[:, 0:nn],
                         in1=sinT[:, n0:n0 + nn], op=ALU.mult)
                    P.op("gpsimd", "tensor_tensor", [t1k, t2k], [sk], out=s[:, n0:n0 + nn], in0=t1[:, 0:nn],
                         in1=t2[:, 0:nn], op=ALU.add)
                P.dma("sync", [sk], [("ropeT", r)], out=ropeT_d[r, :, :], in_=s[:])
            zst = Ring(sb, "pj_z", [128, 512], F32, 2)
            svst = Ring(sb, "pj_sv", [128, 512], BF16, 2)
            smst = Ring(sb, "pj_sm", [128, 16], F32, 2)
            for t in range(NT):
                for (c0, cn, kind) in ((T0, 512, "z"), (T0 + 512, 512, "sv"), (T0 + 1024, 16, "sm")):
                    pt, pk = pbank.next()
                    for k in range(8):
                        P.op("tensor", "matmul", wk(c0, cn) + [("hT", t)], [pk], pt[:, 0:cn],
                             lhsT=hT[:, k, t * 128:(t + 1) * 128], rhs=W[:, k, c0:c0 + cn], start=(k == 0), stop=(k == 7))
                    if kind == "z":
                        s, sk = zst.next()
                        P.op("scalar", "copy", [pk], [sk], out=s[:], in_=pt[:, 0:512])
                        P.dma("sync", [sk], [("z", t)], out=z_d[t * 128:(t + 1) * 128, :], in_=s[:])
                    elif kind == "sv":
                        s, sk = svst.next()
                        P.op("vector", "tensor_copy", [pk], [sk], out=s[:], in_=pt[:, 0:512])
                        P.dma("sync", [sk], [("sv", t)], out=sv_d[t * 128:(t + 1) * 128, :], in_=s[:])
                    else:
                        s, sk = smst.next()
                        P.op("vector", "tensor_copy", [pk], [sk], out=s[:], in_=pt[:, 0:16])
                        P.dma("sync", [sk], [("sm", t)], out=sm_d[t * 128:(t + 1) * 128, :], in_=s[:])
            P.flush()

    def stage_dn(l):
        with ExitStack() as st:
            sb, ps = stage_allocs(st)
            qT = sb("dn_qT", [128, 4, LP], BF16)
            kT = sb("dn_kT", [128, 4, LP], BF16)
            vT = sb("dn_vT", [128, 4, LP], BF16)
            cw = sb("dn_cw", [128, 48])
            P.dma("sync", [], ["cw"], out=cw[:], in_=cwT[l, :, :])
            with ExitStack() as st1:
                sb1, ps1 = stage_allocs(st1)
                xin = Ring(sb1, "dn_xin", [128, LP + 3], F32, 2)
                acc = Ring(sb1, "dn_acc", [128, LP], F32, 2)
                sq = Ring(sb1, "dn_sq", [128, LP], F32, 2)
                rn = Ring(sb1, "dn_rn", [128, 512], F32, 2)
                pss = Ring(ps1, "dn_ps", [128, 512], F32, 2)
                for m in range(12):
                    x, xk = xin.next()
                    P.op("gpsimd", "memset", [], [xk], x[:, 0:3], 0.0)
                    P.dma("sync", [("qkvT", m)], [xk], out=x[:, 3:LP + 3], in_=qkvT_d[m, :, :])
                    a, ak = acc.next()
                    P.op("vector", "tensor_scalar", [xk, "cw"], [ak], out=a[:], in0=x[:, 3:LP + 3],
                         scalar1=cw[:, m * 4 + 3:m * 4 + 4], scalar2=None, op0=ALU.mult)
                    for j in range(3):
                        P.op("vector", "scalar_tensor_tensor", [xk, "cw", ak], [ak], out=a[:],
                             in0=x[:, j:LP + j], scalar=cw[:, m * 4 + j:m * 4 + j + 1], in1=a[:], op0=ALU.mult, op1=ALU.add)
                    if m >= 8:
                        P.op("scalar", "activation", [ak], [("vT", m - 8)], out=vT[:, m - 8, :], in_=a[:], func=AF.Silu)
                        continue
                    P.op("scalar", "activation", [ak], [ak], out=a[:], in_=a[:], func=AF.Silu)
                    s, sk = sq.next()
                    P.op("gpsimd", "tensor_tensor", [ak], [sk], out=s[:], in0=a[:], in1=a[:], op=ALU.mult)
                    dst = qT if m < 4 else kT
                    dkey = ("qT", m) if m < 4 else ("kT", m - 4)
                    for (n0, nn) in NTILES:
                        pt, pk = pss.next()
                        P.op("tensor", "matmul", [sk, "ones"], [pk], pt[:, 0:nn], lhsT=ones[:], rhs=s[:, n0:n0 + nn],
                             start=True, stop=True)
                        r, rk = rn.next()
                        P.op("scalar", "activation", [pk], [rk], out=r[:, 0:nn], in_=pt[:, 0:nn], func=AF.Ln,
                             bias=1e-6, scale=1.0)
                        P.op("scalar", "activation", [rk], [rk], out=r[:, 0:nn], in_=r[:, 0:nn], func=AF.Exp, scale=-0.5)
                        P.op("vector", "scalar_tensor_tensor", [ak, rk], [dkey], out=dst[:, m % 4, n0:n0 + nn],
                             in0=a[:, n0:n0 + nn], scalar=(128.0 ** -0.5 if m < 4 else 1.0), in1=r[:, 0:nn],
                             op0=ALU.mult, op1=ALU.mult)
                P.flush()
            if DN_CUT == 1:
                return
            QK = [("qT", i) for i in range(4)]
            KK = [("kT", i) for i in range(4)]
            VK = [("vT", i) for i in range(4)]
            sm = sb("dn_sm", [128, NT, 16])
            P.dma("sync", [("sm", t) for t in range(NT)], ["smt"], out=sm[:],
                  in_=sm_d.rearrange("(t p) c -> p t c", p=128))
            alr = sb("dn_alr", [128, 68])
            dtr = sb("dn_dtr", [128, 68])
            P.dma("sync", [], ["alr"], out=alr[:], in_=alog[l, :, :])
            P.dma("sync", [], ["dtr"], out=dtr[:], in_=dtb[l, :, :])
            beta = sb("dn_beta", [128, NT, 4])
            g = sb("dn_g", [128, NT, 4])
            tmpg = sb("dn_tmpg", [128, NT, 4])
            v3 = lambda a: a[:].rearrange("p (t h) -> p t h", h=4)
            P.op("scalar", "activation", ["smt"], ["beta"], out=beta[:], in_=sm[:, :, 0:4], func=AF.Sigmoid)
            P.op("vector", "tensor_tensor", ["smt", "dtr"], ["tmpg"], out=tmpg[:], in0=sm[:, :, 4:8], in1=v3(dtr), op=ALU.add)
            P.op("scalar", "activation", ["tmpg"], ["tmpg"], out=tmpg[:], in_=tmpg[:], func=AF.Exp)
            P.op("scalar", "activation", ["tmpg"], ["tmpg"], out=tmpg[:], in_=tmpg[:], func=AF.Ln, bias=1.0, scale=1.0)
            P.op("scalar", "activation", ["alr"], ["alr"], out=alr[:], in_=alr[:], func=AF.Exp)
            P.op("vector", "scalar_tensor_tensor", ["tmpg", "alr"], ["g"], out=g[:], in0=tmpg[:], scalar=-1.0,
                 in1=v3(alr), op0=ALU.mult, op1=ALU.mult)
            U = sb("dn_U", [128, 128])
            Mst = sb("dn_Mst", [128, 4, 128])
            Mup = sb("dn_Mup", [128, 4, 128])
            P.op("gpsimd", "memset", [], ["U"], U[:], 1.0)
            P.op("gpsimd", "affine_select", ["U"], ["U"], out=U[:], in_=U[:], pattern=[[1, 128]],
                 compare_op=ALU.is_ge, fill=0.0, base=0, channel_multiplier=-1)
            P.op("gpsimd", "memset", [], ["Mst"], Mst[:], 0.0)
            P.op("gpsimd", "affine_select", ["Mst"], ["Mst"], out=Mst[:], in_=Mst[:], pattern=[[0, 4], [-1, 128]],
                 compare_op=ALU.is_ge, fill=NEG, base=-1, channel_multiplier=1)
            P.op("gpsimd", "memset", [], ["Mup"], Mup[:], 0.0)
            P.op("gpsimd", "affine_select", ["Mup"], ["Mup"], out=Mup[:], in_=Mup[:], pattern=[[0, 4], [1, 128]],
                 compare_op=ALU.is_ge, fill=NEG, base=0, channel_multiplier=-1)
            gc = sb("dn_gc", [128, 68])
            ngc = sb("dn_ngc", [128, 68])
            gl = sb("dn_gl", [128, 68])
            egc = sb("dn_egc", [128, 68])
            ekd = sb("dn_ekd", [128, 68])
            egl = sb("dn_egl", [128, 68])
            bgc = sb("dn_bgc", [128, 68])
            st_g = ExitStack()
            psg = st_g.enter_context(nc.psum_tensor(uname("dn_psg"), [128, 512], F32))
            g2 = g[:].rearrange("p t h -> p (t h)")
            P.op("tensor", "matmul", ["g", "U"], ["psg"], psg[:, 0:68], lhsT=U[:], rhs=g2, start=True, stop=True)
            P.op("tensor", "matmul", ["g", "ones"], ["psg"], psg[:, 128:196], lhsT=ones[:], rhs=g2, start=True, stop=True)
            P.op("vector", "tensor_copy", ["psg"], ["gc"], out=gc[:], in_=psg[:, 0:68])
            P.op("vector", "tensor_copy", ["psg"], ["gl"], out=gl[:], in_=psg[:, 128:196])
            P.op("vector", "tensor_scalar", ["gc"], ["ngc"], out=ngc[:], in0=gc[:], scalar1=-1.0, scalar2=None, op0=ALU.mult)
            P.op("scalar", "activation", ["gc"], ["egc"], out=egc[:], in_=gc[:], func=AF.Exp)
            P.op("scalar", "activation", ["gl"], ["egl"], out=egl[:], in_=gl[:], func=AF.Exp)
            P.op("vector", "tensor_tensor", ["gl", "gc"], ["ekd"], out=ekd[:], in0=gl[:], in1=gc[:], op=ALU.subtract)
            P.op("scalar", "activation", ["ekd"], ["ekd"], out=ekd[:], in_=ekd[:], func=AF.Exp)
            P.op("vector", "tensor_tensor", ["egc", "beta"], ["bgc"], out=bgc[:], in0=egc[:],
                 in1=beta[:].rearrange("p t h -> p (t h)"), op=ALU.mult)
            P.flush()
            st_g.close()
            if DN_CUT == 2:
                return

            NB = 4
            pA = [ps("dn_pA%d" % i) for i in range(2)]
            pB = [ps("dn_pB%d" % i) for i in range(2)]
            pS = [ps("dn_pS%d" % i) for i in range(2)]
            pTk = ps("dn_pTk")
            pTv = ps("dn_pTv")
            Dg_ = [Ring(sb, "dn_Dg%d_" % p_, [128, 4, 128], F32, 1) for p_ in range(2)]
            dec_ = [Ring(sb, "dn_dec%d_" % p_, [128, 4, 128], F32, 1) for p_ in range(2)]
            decT = Ring(sb, "dn_decT", [128, 4, 128], F32, NB)
            Abuf_ = [Ring(sb, "dn_A%d_" % p_, [128, 4, 128], F32, 2) for p_ in range(2)]
            Bbuf_ = [Ring(sb, "dn_B%d_" % p_, [128, 4, 128], F32, 2) for p_ in range(2)]
            Xbuf_ = [Ring(sb, "dn_X%d_" % p_, [128, 4, 128], F32, 2) for p_ in range(2)]
            bvb_ = [Ring(sb, "dn_bv%d_" % p_, [128, 4, 128], F32, 1) for p_ in range(2)]
            kbgb_ = [Ring(sb, "dn_kbg%d_" % p_, [128, 4, 128], F32, 1) for p_ in range(2)]
            u4b = Ring(sb, "dn_u4", [128, 4, 128], F32, NB)
            wT4b = Ring(sb, "dn_wT4", [128, 4, 128], BF16, NB)
            aqk4b = Ring(sb, "dn_aqk", [128, 4, 128], BF16, NB)
            kd4b = Ring(sb, "dn_kd4", [128, 4, 128], BF16, NB)

            def slot(ring, t):
                i_ = t % len(ring.bufs)
                return ring.bufs[i_], ring.keys[i_]
            S4 = sb("dn_S4", [128, 4, 128])
            S4b = sb("dn_S4b", [128, 4, 128], BF16)
            P.op("vector", "memset", [], ["S4"], S4[:], 0.0)
            P.op("gpsimd", "memset", [], ["S4b"], S4b[:], 0.0)
            vn4b = Ring(sb, "dn_vn4", [128, 4, 128], BF16, 2)
            qs4b = Ring(sb, "dn_qs4", [128, 4, 128], F32, 2)
            o4b = Ring(sb, "dn_o4", [128, 4, 128], F32, 2)
            ztb = Ring(sb, "dn_zt", [128, 512], F32, 2)
            ogb = Ring(sb, "dn_og", [128, 512], BF16, 2)
            ssb = Ring(sb, "dn_ss", [128, 8], F32, 2)
            junk = sb("dn_junk", [128, 128])
            odT = sb("dn_odT", [128, 4, LP], BF16)
            ng = sb("dn_ng", [128, 512])
            P.dma("sync", [], ["ng"], out=ng[:], in_=ngr[l, :, :])
            prep_out = {}

            def F(ap):
                return ap[:].rearrange("p h c -> p (h c)")

            def prep(t):
                par = t % 2
                Dg, dec, Abuf, Bbuf, Xbuf, bvb, kbgb = Dg_[par], dec_[par], Abuf_[par], Bbuf_[par], Xbuf_[par], bvb_[par], kbgb_[par]
                cs = slice(t * 128, (t + 1) * 128)
                col = lambda a, h: a[:, t * 4 + h:t * 4 + h + 1]
                dg, dgk = Dg.next()
                for h in range(4):
                    P.op("gpsimd", "tensor_scalar", ["ident", "gc"], [dgk], out=dg[:, h, :], in0=ident[:],
                         scalar1=col(gc, h), scalar2=None, op0=ALU.mult)
                a_ps, ak_ps = pA[par], "dn_pA%d" % par
                b_ps, bk_ps = pB[par], "dn_pB%d" % par
                x_ps, xk_ps = b_ps, bk_ps
                P.op("tensor", "matmul", [dgk, "negones"], [ak_ps], a_ps[:], lhsT=negones[:], rhs=F(dg), start=True, stop=False)
                P.op("tensor", "matmul", ["Mst", "ident"], [ak_ps], a_ps[:], lhsT=ident[:], rhs=F(Mst), start=False, stop=True)
                P.op("tensor", "matmul", [dgk, "ones"], [bk_ps], b_ps[:], lhsT=ones[:], rhs=F(dg), start=True, stop=False)
                P.op("tensor", "matmul", ["Mup", "ident"], [bk_ps], b_ps[:], lhsT=ident[:], rhs=F(Mup), start=False, stop=True)
                de, dek = dec.next()
                deT, deTk = slot(decT, t)
                for h in range(4):
                    P.op("scalar", "activation", [ak_ps, "gc"], [dek], out=de[:, h, :], in_=a_ps[:, h * 128:(h + 1) * 128],
                         func=AF.Exp, bias=col(gc, h), scale=1.0)
                    P.op("scalar", "activation", [bk_ps, "ngc"], [deTk], out=deT[:, h, :], in_=b_ps[:, h * 128:(h + 1) * 128],
                         func=AF.Exp, bias=col(ngc, h), scale=1.0)
                yield
                if PREP_CUT == 1:
                    return
                for h in range(4):
                    P.op("tensor", "matmul", KK, [xk_ps], x_ps[:, h * 128:(h + 1) * 128], lhsT=kT[:, h, cs], rhs=kT[:, h, cs],
                         start=True, stop=True)
                A, Ak = Abuf.next()
                for h in range(4):
                    P.op("vector", "scalar_tensor_tensor", [xk_ps, "beta", dek], [Ak], out=A[:, h, :],
                         in0=x_ps[:, h * 128:(h + 1) * 128], scalar=beta[:, t, h:h + 1], in1=de[:, h, :],
                         op0=ALU.mult, op1=ALU.mult)
                for h in range(4):
                    P.op("tensor", "transpose", [Ak, "ident"], [ak_ps], out=a_ps[:, h * 128:(h + 1) * 128], in_=A[:, h, :],
                         identity=ident[:])
                Bm, Bk = Bbuf.next()
                P.op("scalar", "copy", [ak_ps], [Bk], out=F(Bm), in_=a_ps[:])
                X, Xk = Xbuf.next()
                for h in range(4):
                    P.op("gpsimd", "tensor_tensor", ["ident", Bk], [Xk], out=X[:, h, :], in0=ident[:], in1=Bm[:, h, :],
                         op=ALU.subtract)
                if PREP_CUT == 2:
                    return
                for h in range(4):
                    P.op("tensor", "matmul", KK + ["identb"], ["dn_pTk"], pTk[:, h * 128:(h + 1) * 128], lhsT=kT[:, h, cs],
                         rhs=identb[:], start=True, stop=True)
                    P.op("tensor", "matmul", VK + ["identb"], ["dn_pTv"], pTv[:, h * 128:(h + 1) * 128], lhsT=vT[:, h, cs],
                         rhs=identb[:], start=True, stop=True)
                kbg, kbgk = kbgb.next()
                kd4, kd4k = slot(kd4b, t)
                bv, bvk = bvb.next()
                if PREP_CUT == 31:
                    return
                for h in range(4):
                    P.op("vector", "tensor_scalar", ["dn_pTk", "bgc"], [kbgk], out=kbg[:, h, :], in0=pTk[:, h * 128:(h + 1) * 128],
                         scalar1=col(bgc, h), scalar2=None, op0=ALU.mult)
                    if PREP_CUT == 32:
                        continue
                    P.op("scalar", "activation", ["dn_pTk", "ekd"], [kd4k], out=kd4[:, h, :], in_=pTk[:, h * 128:(h + 1) * 128],
                         func=AF.Copy, scale=col(ekd, h))
                    if PREP_CUT == 33:
                        continue
                    P.op("vector", "tensor_scalar", ["dn_pTv", "beta"], [bvk], out=bv[:, h, :],
                         in0=pTv[:, h * 128:(h + 1) * 128], scalar1=beta[:, t, h:h + 1], scalar2=None, op0=ALU.mult)
                if PREP_CUT in (32, 33):
                    return
                yield
                if PREP_CUT == 3:
                    return
                for n in range(1, 7):
                    A2, A2k = Abuf.next()
                    for h in range(4):
                        P.op("tensor", "matmul", [Ak, Bk], [ak_ps], a_ps[:, h * 128:(h + 1) * 128], lhsT=Bm[:, h, :], rhs=A[:, h, :],
                             start=True, stop=True)
                    if n < 6:
                        B2, B2k = Bbuf.next()
                        for h in range(4):
                            P.op("tensor", "matmul", [Ak, Bk], [bk_ps], b_ps[:, h * 128:(h + 1) * 128], lhsT=A[:, h, :], rhs=Bm[:, h, :],
                                 start=True, stop=True)
                    P.op("scalar", "copy", [ak_ps], [A2k], out=F(A2), in_=a_ps[:])
                    if n < 6:
                        P.op("vector", "tensor_copy", [bk_ps], [B2k], out=F(B2), in_=b_ps[:])
                    for h in range(4):
                        P.op("tensor", "matmul", [A2k, Xk], [ak_ps], a_ps[:, h * 128:(h + 1) * 128], lhsT=A2[:, h, :], rhs=X[:, h, :],
                             start=True, stop=True)
                    X2, X2k = Xbuf.next()
                    P.op("vector", "tensor_tensor", [ak_ps, Xk], [X2k], out=F(X2), in0=a_ps[:], in1=F(X), op=ALU.add)
                    A, Ak = A2, A2k
                    if n < 6:
                        Bm, Bk = B2, B2k
                    X, Xk = X2, X2k
                    yield
                if PREP_CUT == 4:
                    return
                u4, u4k = slot(u4b, t)
                wT4, wT4k = slot(wT4b, t)
                aqk, aqkk = slot(aqk4b, t)
                for h in range(4):
                    P.op("tensor", "matmul", [Xk, bvk], [ak_ps], a_ps[:, h * 128:(h + 1) * 128], lhsT=X[:, h, :], rhs=bv[:, h, :],
                         start=True, stop=True)
                    P.op("tensor", "matmul", [Xk, kbgk], [bk_ps], b_ps[:, h * 128:(h + 1) * 128], lhsT=kbg[:, h, :], rhs=X[:, h, :],
                         start=True, stop=True)
                P.op("scalar", "copy", [ak_ps], [u4k], out=F(u4), in_=a_ps[:])
                P.op("scalar", "copy", [bk_ps], [wT4k], out=F(wT4), in_=b_ps[:])
                for h in range(4):
                    P.op("tensor", "matmul", KK + QK, [ak_ps], a_ps[:, h * 128:(h + 1) * 128], lhsT=kT[:, h, cs], rhs=qT[:, h, cs],
                         start=True, stop=True)
                P.op("vector", "tensor_tensor", [ak_ps, deTk], [aqkk], out=F(aqk), in0=a_ps[:], in1=F(deT), op=ALU.mult)
                prep_out[t] = (u4, u4k, wT4, wT4k, aqk, aqkk, kd4, kd4k)
                yield

            def scan(ts):
                for t in ts:
                    u4, u4k, wT4, wT4k, aqk, aqkk, kd4, kd4k = prep_out.pop(t)
                    cs = slice(t * 128, (t + 1) * 128)
                    col = lambda a, h: a[:, t * 4 + h:t * 4 + h + 1]
                    p1, p1k = pS[0], "dn_pS0"
                    p2, p2k = pS[1], "dn_pS1"
                    for h in range(4):
                        P.op("tensor", "matmul", [wT4k, "S4b"], [p1k], p1[:, h * 128:(h + 1) * 128], lhsT=wT4[:, h, :], rhs=S4b[:, h, :],
                             start=True, stop=True)
                    for h in range(4):
                        P.op("tensor", "matmul", QK + ["S4b"], [p2k], p2[:, h * 128:(h + 1) * 128], lhsT=qT[:, h, cs], rhs=S4b[:, h, :],
                             start=True, stop=True)
                    vn, vnk = vn4b.next()
                    P.op("vector", "tensor_tensor", [u4k, p1k], [vnk], out=F(vn), in0=F(u4), in1=p1[:], op=ALU.subtract)
                    qs, qsk = qs4b.next()
                    for h in range(4):
                        P.op("scalar", "activation", [p2k, "egc"], [qsk], out=qs[:, h, :], in_=p2[:, h * 128:(h + 1) * 128],
                             func=AF.Copy, scale=col(egc, h))
                    yield
                    for h in range(4):
                        P.op("tensor", "matmul", [aqkk, vnk], [p1k], p1[:, h * 128:(h + 1) * 128], lhsT=aqk[:, h, :], rhs=vn[:, h, :],
                             start=True, stop=True)
                    for h in range(4):
                        P.op("tensor", "matmul", [kd4k, vnk], [p2k], p2[:, h * 128:(h + 1) * 128], lhsT=kd4[:, h, :], rhs=vn[:, h, :],
                             start=True, stop=True)
                    o4, o4k = o4b.next()
                    P.op("vector", "tensor_tensor", [p1k, qsk], [o4k], out=F(o4), in0=p1[:], in1=F(qs), op=ALU.add)
                    for h in range(4):
                        P.op("vector", "scalar_tensor_tensor", ["S4", "egl", p2k], ["S4"], out=S4[:, h, :], in0=S4[:, h, :],
                             scalar=col(egl, h), in1=p2[:, h * 128:(h + 1) * 128], op0=ALU.mult, op1=ALU.add)
                    P.op("gpsimd", "tensor_copy", ["S4"], ["S4b"], out=F(S4b), in_=F(S4))
                    yield
                    zt, ztk = ztb.next()
                    P.dma("sync", [("z", t)], [ztk], out=zt[:], in_=z_d[t * 128:(t + 1) * 128, :])
                    ss, ssk = ssb.next()
                    for h in range(4):
                        P.op("scalar", "activation", [o4k], ["dn_junk", ssk], out=junk[:], in_=o4[:, h, :], func=AF.Square,
                             accum_out=ss[:, h:h + 1])
                    P.op("vector", "tensor_scalar", [ssk], [ssk], out=ss[:, 4:8], in0=ss[:, 0:4], scalar1=1.0 / 128, scalar2=1e-6,
                         op0=ALU.mult, op1=ALU.add)
                    P.op("scalar", "activation", [ssk], [ssk], out=ss[:, 4:8], in_=ss[:, 4:8], func=AF.Sqrt)
                    P.op("vector", "reciprocal", [ssk], [ssk], out=ss[:, 4:8], in_=ss[:, 4:8])
                    P.op("scalar", "activation", [ztk], [ztk], out=zt[:], in_=zt[:], func=AF.Silu)
                    P.op("gpsimd", "tensor_tensor", [ztk, "ng"], [ztk], out=zt[:], in0=zt[:], in1=ng[:], op=ALU.mult)
                    og, ogk = ogb.next()
                    for h in range(4):
                        P.op("gpsimd", "tensor_scalar", [o4k, ssk], [o4k], out=o4[:, h, :], in0=o4[:, h, :],
                             scalar1=ss[:, 4 + h:5 + h], scalar2=None, op0=ALU.mult)
                        P.op("gpsimd", "tensor_tensor", [o4k, ztk], [ogk], out=og[:, h * 128:(h + 1) * 128], in0=o4[:, h, :],
                             in1=zt[:, h * 128:(h + 1) * 128], op=ALU.mult)
                    for h in range(4):
                        P.op("tensor", "matmul", [ogk, "identb"], ["dn_pTk"], pTk[:, h * 128:(h + 1) * 128],
                             lhsT=og[:, h * 128:(h + 1) * 128], rhs=identb[:], start=True, stop=True)
                    P.op("scalar", "copy", ["dn_pTk"], [("odT", t)], out=odT[:, :, cs],
                         in_=pTk[:, 0:512].rearrange("p (h c) -> p h c", h=4))
                    yield

            for s_ in range(0, NT + 2, 2):
                if DN_CUT == 3 and s_ >= 2:
                    break
                gens = [prep(t) for t in (s_, s_ + 1) if t < NT]
                prev = [t for t in (s_ - 2, s_ - 1) if 0 <= t < NT]
                if prev:
                    gens.append(scan(prev))
                interleave(gens)
            for h in range(4):
                P.dma("sync", [("odT", t) for t in range(NT)], [("mixT", h)], out=mixT_d[h, :, :], in_=odT[:, h, :])
            P.flush()

    def stage_dsa(l):
        with ExitStack() as st:
            sb, ps = stage_allocs(st)
            saq = sb("sa_q", [128, 4, LP], BF16)
            sak = sb("sa_k", [128, 4, LP], BF16)
            iq = sb("sa_iq", [128, 4, LP], BF16)
            ik = sb("sa_ik", [128, LP], BF16)
            for c in range(4):
                P.dma("sync", [("ropeT", c)], ["saq"], out=saq[:, c, :], in_=ropeT_d[c, :, :])
                P.dma("sync", [("ropeT", 4 + c)], ["sak"], out=sak[:, c, :], in_=ropeT_d[4 + c, :, :])
                P.dma("sync", [("ropeT", 8 + c)], ["iq"], out=iq[:, c, :], in_=ropeT_d[8 + c, :, :])
            P.dma("sync", [("ropeT", 12)], ["ik"], out=ik[:], in_=ropeT_d[12, :, :])
            va = sb("sa_va", [128, NT, 8, 65], BF16)
            P.op("gpsimd", "memset", [], ["va"], va[:], 1.0)
            for t in range(NT):
                P.dma("sync", [("sv", t)], ["va"], out=va[:, t, :, 0:64],
                      in_=sv_d[t * 128:(t + 1) * 128, :].rearrange("p (h d) -> p h d", d=64))
            sm = sb("sa_sm", [128, NT, 16])
            P.dma("sync", [("sm", t) for t in range(NT)], ["sasm"], out=sm[:], in_=sm_d.rearrange("(t p) c -> p t c", p=128))
            cmask = sb("sa_cmask", [128, 128], BF16)
            P.op("gpsimd", "memset", [], ["cmask"], cmask[:], 0.0)
            P.op("gpsimd", "affine_select", ["cmask"], ["cmask"], out=cmask[:], in_=cmask[:], pattern=[[-1, 128]],
                 compare_op=ALU.is_ge, fill=NEG, base=0, channel_multiplier=1)
            osT = sb("sa_osT", [128, 4, LP], BF16)
            sc_ps = Ring(ps, "sa_scps", [128, 512], F32, 2)
            s_ps = Ring(ps, "sa_sps", [128, 512], F32, 3)
            o_ps = [ps("sa_ops%d" % i, [128, 512]) for i in range(2)]
            pTb = ps("sa_pTb")
            relu_b = Ring(sb, "sa_relu", [128, 512], F32, 3)
            accb = Ring(sb, "sa_acc", [128, LP], F32, 2)
            mbb = Ring(sb, "sa_mb", [128, LP], BF16, 4)
            junkb = Ring(sb, "sa_junk", [128, LP], BF16, 2)
            ptb = Ring(sb, "sa_pt", [128, 512], BF16, 3)
            stt = Ring(sb, "sa_st", [128, 8], F32, 2)
            wtb = Ring(sb, "sa_wt", [128, NIT + 2], F32, 2)
            ftab = sb("sa_ftab", [128, NIT + 2])
            for n_ in range(NIT + 2):
                P.op("gpsimd", "memset", [], ["ftab"], ftab[:, n_:n_ + 1], 2.0 ** (-n_))
            osb = Ring(sb, "sa_os", [128, 512], BF16, 2)
            rdb = Ring(sb, "sa_rd", [128, 8], F32, 2)

            def hrows(h):
                return slice(0, 64) if h % 2 == 0 else slice(64, 128)

            mb_of = {}

            def scores(i):
                nk = 128 * (i + 1)
                qs_ = slice(i * 128, (i + 1) * 128)
                acc, acck = accb.next()
                for h in range(8):
                    for k0 in range(0, nk, 512):
                        kn = min(512, nk - k0)
                        pt, pk = sc_ps.next()
                        P.op("tensor", "matmul", ["iq", "ik"], [pk], pt[:, 0:kn], lhsT=iq[hrows(h), h // 2, qs_],
                             rhs=ik[hrows(h), k0:k0 + kn], start=True, stop=True)
                        if h == 0:
                            r, rk = relu_b.next()
                            P.op("scalar", "activation", [pk], [rk], out=r[:, 0:kn], in_=pt[:, 0:kn], func=AF.Relu)
                            P.op("vector", "tensor_scalar", [rk, "sasm"], [acck], out=acc[:, k0:k0 + kn], in0=r[:, 0:kn],
                                 scalar1=sm[:, i, 8:9], scalar2=None, op0=ALU.mult)
                        else:
                            r, rk = relu_b.next()
                            P.op("scalar", "activation", [pk], [rk], out=r[:, 0:kn], in_=pt[:, 0:kn], func=AF.Relu)
                            P.op("vector", "scalar_tensor_tensor", [rk, "sasm", acck], [acck], out=acc[:, k0:k0 + kn], in0=r[:, 0:kn],
                                 scalar=sm[:, i, 8 + h:9 + h], in1=acc[:, k0:k0 + kn], op0=ALU.mult, op1=ALU.add)
                    yield
                P.op("gpsimd", "affine_select", [acck], [acck], out=acc[:, i * 128:nk], in_=acc[:, i * 128:nk], pattern=[[-1, 128]],
                     compare_op=ALU.is_ge, fill=-1e30, base=0, channel_multiplier=1)
                mb, mbk = mbb.next()
                s8, s8k = stt.next()
                if nk <= KTOP:
                    P.op("vector", "memset", [], [s8k], s8[:, 0:1], -1e29)
                else:
                    jk, jkk = junkb.next()
                    P.op("vector", "tensor_reduce", [acck], [s8k], out=s8[:, 5:6], in_=acc[:, 0:nk], axis=mybir.AxisListType.X, op=ALU.max)
                    P.op("vector", "tensor_reduce", [acck], [s8k], out=s8[:, 0:1], in_=acc[:, 0:i * 128], axis=mybir.AxisListType.X, op=ALU.min)
                    P.op("vector", "tensor_tensor", [s8k], [s8k], out=s8[:, 1:2], in0=s8[:, 5:6], in1=s8[:, 0:1], op=ALU.subtract)
                    P.op("vector", "tensor_scalar", [s8k], [s8k], out=s8[:, 1:2], in0=s8[:, 1:2], scalar1=1.0001, scalar2=1e-6,
                         op0=ALU.mult, op1=ALU.add)
                    wt, wtk = wtb.next()
                    P.op("vector", "tensor_scalar", ["ftab", s8k], [wtk], out=wt[:], in0=ftab[:], scalar1=s8[:, 1:2], scalar2=None,
                         op0=ALU.mult)
                    P.op("vector", "tensor_tensor", [s8k, wtk], [s8k], out=s8[:, 2:3], in0=s8[:, 0:1], in1=wt[:, 1:2], op=ALU.add)
                    for it in range(1, NIT + 1):
                        P.op("vector", "tensor_scalar", [acck, s8k], [jkk, s8k], out=jk[:, 0:nk], in0=acc[:, 0:nk], scalar1=s8[:, 2:3],
                             scalar2=None, op0=ALU.is_ge, op1=ALU.add, accum_out=s8[:, 3:4])
                        P.op("vector", "tensor_scalar", [s8k], [s8k], out=s8[:, 4:5], in0=s8[:, 3:4], scalar1=KTOP - 0.5, scalar2=0.5,
                             op0=ALU.is_ge, op1=ALU.subtract)
                        P.op("vector", "scalar_tensor_tensor", [s8k, wtk], [s8k], out=s8[:, 2:3], in0=s8[:, 4:5], scalar=wt[:, it:it + 1],
                             in1=s8[:, 2:3], op0=ALU.mult, op1=ALU.add)
                        yield
                    P.op("vector", "tensor_tensor", [s8k, wtk], [s8k], out=s8[:, 0:1], in0=s8[:, 2:3], in1=wt[:, NIT + 1:NIT + 2],
                         op=ALU.subtract)
                P.op("vector", "tensor_scalar", [acck, s8k], [mbk], out=mb[:, 0:nk], in0=acc[:, 0:nk], scalar1=s8[:, 0:1], scalar2=NEG,
                     op0=ALU.is_lt, op1=ALU.mult)
                mb_of[i] = (mb, mbk)
                yield

            def attention(i):
                mb, mbk = mb_of.pop(i)
                qs_ = slice(i * 128, (i + 1) * 128)
                groups = [(kt, hg) for kt in range(i + 1) for hg in range(2)]
                pend = None
                for g in groups + [None]:
                    cur = None
                    if g is not None:
                        kt, hg = g
                        ks_ = slice(kt * 128, (kt + 1) * 128)
                        sp, spk = s_ps.next()
                        for pair in ((0, 2), (1, 3)):
                            for hh in pair:
                                h = hg * 4 + hh
                                P.op("tensor", "matmul", ["sak", "saq"], [spk], sp[:, hh * 128:(hh + 1) * 128],
                                     lhsT=sak[hrows(h), h // 2, ks_], rhs=saq[hrows(h), h // 2, qs_], start=(hh == 0), stop=False,
                                     skip_group_check=True)
                            for hh in pair:
                                P.op("tensor", "matmul", [mbk, "identb"], [spk], sp[:, hh * 128:(hh + 1) * 128],
                                     lhsT=mb[:, ks_], rhs=identb[:], start=False, stop=True, skip_group_check=True)
                        pt, ptk = ptb.next()
                        P.op("scalar", "activation", [spk], [ptk], out=pt[:], in_=sp[:], func=AF.Exp, scale=0.125)
                        cur = (kt, hg, pt, ptk)
                    if pend is not None:
                        kt, hg, pt, ptk = pend
                        for hh in range(4):
                            h = hg * 4 + hh
                            P.op("tensor", "matmul", [ptk, "va"], ["sa_ops%d" % hg], o_ps[hg][:, hh * 65:(hh + 1) * 65],
                                 lhsT=pt[:, hh * 128:(hh + 1) * 128], rhs=va[:, kt, h, :], start=(kt == 0 and hh == 0), stop=(kt == i),
                                 skip_group_check=True)
                    pend = cur
                    yield
                rd, rdk = rdb.next()
                for hg in range(2):
                    P.op("vector", "reciprocal", ["sa_ops%d" % hg], [rdk], out=rd[:, hg * 4:(hg + 1) * 4],
                         in_=o_ps[hg][:, 0:260].rearrange("p (h d) -> p h d", d=65)[:, :, 64])
                os_, osk = osb.next()
                for h in range(8):
                    hg, hh = h // 4, h % 4
                    P.op("vector", "tensor_scalar", ["sa_ops%d" % hg, rdk], [osk], out=os_[:, h * 64:(h + 1) * 64],
                         in0=o_ps[hg][:, hh * 65:hh * 65 + 64], scalar1=rd[:, h:h + 1], scalar2=None, op0=ALU.mult)
                for c in range(4):
                    P.op("tensor", "matmul", [osk, "identb"], ["sa_pTb"], pTb[:, c * 128:(c + 1) * 128],
                         lhsT=os_[:, c * 128:(c + 1) * 128], rhs=identb[:], start=True, stop=True)
                P.op("scalar", "copy", ["sa_pTb"], [("osT", i)], out=osT[:, :, qs_],
                     in_=pTb[:, 0:512].rearrange("p (h c) -> p h c", h=4))
                yield

            def att_chain(ts):
                for t_ in ts:
                    yield from attention(t_)

            for i in range(0, NT + 2, 2):
                gens = [scores(t_) for t_ in (i, i + 1) if t_ < NT]
                prev = [t_ for t_ in (i - 2, i - 1) if 0 <= t_ < NT]
                if prev:
                    gens.append(att_chain(prev))
                interleave(gens)
            for c in range(4):
                P.dma("sync", [("osT", t) for t in range(NT)], [("mixT", 4 + c)], out=mixT_d[4 + c, :, :], in_=osT[:, c, :])
            P.flush()

    def stage_mix_ln1(l, src, hT, comb):
        with ExitStack() as st:
            sb, ps = stage_allocs(st)
            mixT = sb("mx_mixT", [128, 8, LP], BF16)
            for c in range(8):
                P.dma("sync", [("mixT", c)], ["mixT"], out=mixT[:, c, :], in_=mixT_d[c, :, :])
            wo = sb("mx_wo", [128, 8, D], BF16)
            wol = w_out[l].rearrange("(c p) n -> p c n", p=128)
            for c in range(8):
                P.dma("gpsimd", [], ["wo"], out=wo[:, c, :], in_=wol[:, c, :])
            g_rep = sb("mx_g", [128, D])
            b_rep = sb("mx_b", [128, D])
            P.dma("sync", [], ["lng"], out=g_rep[:], in_=ln1g[l, :, :])
            P.dma("sync", [], ["lnb"], out=b_rep[:], in_=ln1b[l, :, :])
            wrs = sb("mx_wr", [128, 8, 36])
            P.dma("sync", [], ["wrs"], out=wrs[:], in_=wr[l].rearrange("(c p) n -> p c n", p=128))
            br = sb("mx_br", [128, 36])
            P.dma("sync", [], ["br"], out=br[:], in_=brep[l, :, :])
            hin = Ring(sb, "mx_hin", [128, D], F32, 2)
            tb = Ring(sb, "mx_t", [128, D], F32, 2)
            ob = Ring(sb, "mx_o", [128, D], F32, 2)
            scr = sb("mx_scr", [128, D])
            stb = Ring(sb, "mx_st", [128, 4], F32, 2)
            hTf = Ring(sb, "mx_hTf", [128, 8, 128], F32, 2)
            pM = [ps("mx_pM%d" % i, [128, 1024]) for i in range(2)]
            pT = [ps("mx_pT%d" % i, [128, 1024]) for i in range(1)]
            pLs = [ps("mx_pL%d" % i, [128, 512]) for i in range(2)]
            rt = Ring(sb, "mx_rt", [128, 160], F32, 2)
            def mtile(t):
                cs = slice(t * 128, (t + 1) * 128)
                pL, pLk = pLs[t % 2], "mx_pL%d" % (t % 2)
                pm, pmk = pM[t % 2], "mx_pM%d" % (t % 2)
                for n in range(2):
                    for c in range(8):
                        P.op("tensor", "matmul", ["mixT", "wo"], [pmk], pm[:, n * 512:(n + 1) * 512], lhsT=mixT[:, c, cs],
                             rhs=wo[:, c, n * 512:(n + 1) * 512], start=(c == 0), stop=(c == 7))
                hi_, hik = hin.next()
                P.dma("sync", [("h", t)], [hik], out=hi_[:], in_=src[t * 128:(t + 1) * 128, :])
                tt, ttk = tb.next()
                P.op("vector", "scalar_tensor_tensor", [hik, pmk], [ttk], out=tt[:], in0=hi_[:], scalar=ALPHA, in1=pm[:],
                     op0=ALU.mult, op1=ALU.add)
                o, ok = ob.next()
                s4, s4k = stb.next()
                yield
                yield from ln_tile(tt, ttk, g_rep, b_rep, o, ok, scr, "mx_scr", s4, s4k)
                P.dma("sync", [ok], [("h", t)], out=h_d[t * 128:(t + 1) * 128, :], in_=o[:])
                yield
                pt, ptk = pT[0], "mx_pT0"
                for c in range(8):
                    P.op("tensor", "transpose", [ok, "ident"], [ptk], out=pt[:, c * 128:(c + 1) * 128], in_=o[:, c * 128:(c + 1) * 128],
                         identity=ident[:])
                hf, hfk = hTf.next()
                P.op("scalar", "copy", [ptk], [hfk], out=hf[:].rearrange("p c t -> p (c t)"), in_=pt[:])
                P.op("gpsimd", "tensor_copy", [hfk], [("hT", t)], out=hT[:, :, cs], in_=hf[:])
                yield
                for c in range(8):
                    P.op("tensor", "matmul", [hfk, "wrs"], [pLk], pL[:, 0:36], lhsT=hf[:, c, :], rhs=wrs[:, c, :],
                         start=(c == 0), stop=(c == 7))
                yield
                r, rk = rt.next()
                P.op("vector", "tensor_tensor", [pLk, "br"], [rk], out=r[:, 0:36], in0=pL[:, 0:36], in1=br[:], op=ALU.add)
                P.op("vector", "tensor_reduce", [rk], [rk], out=r[:, 148:149], in_=r[:, 0:4], axis=mybir.AxisListType.X, op=ALU.max)
                P.op("vector", "tensor_scalar", [rk], [rk], out=r[:, 149:150], in0=r[:, 148:149], scalar1=-1.0, scalar2=None, op0=ALU.mult)
                P.op("scalar", "activation", [rk], [rk], out=r[:, 36:40], in_=r[:, 0:4], func=AF.Exp, bias=r[:, 149:150], scale=1.0,
                     accum_out=r[:, 150:151])
                P.op("vector", "reciprocal", [rk], [rk], out=r[:, 151:152], in_=r[:, 150:151])
                P.op("vector", "tensor_scalar", [rk], [rk], out=r[:, 44:48], in0=r[:, 0:4], scalar1=r[:, 148:149], scalar2=None,
                     op0=ALU.is_ge)
                P.op("vector", "tensor_scalar", [rk], [rk], out=r[:, 48:52], in0=r[:, 44:48], scalar1=-1.0, scalar2=1e30,
                     op0=ALU.add, op1=ALU.mult)
                for gq in range(4):
                    P.op("vector", "tensor_scalar", [rk], [rk], out=r[:, 52 + gq * 8:60 + gq * 8], in0=r[:, 4 + gq * 8:12 + gq * 8],
                         scalar1=r[:, 48 + gq:49 + gq], scalar2=None, op0=ALU.add)
                yield
                P.op("vector", "max", [rk], [rk], out=r[:, 36:44], in_=r[:, 52:84])
                P.op("vector", "tensor_scalar", [rk], [rk], out=r[:, 84:116], in0=r[:, 52:84], scalar1=r[:, 36:37], scalar2=None,
                     op0=ALU.is_equal)
                P.op("vector", "tensor_scalar", [rk], [rk], out=r[:, 116:148], in0=r[:, 52:84], scalar1=r[:, 37:38], scalar2=None,
                     op0=ALU.is_equal)
                P.op("vector", "tensor_tensor", [rk], [rk], out=r[:, 152:153], in0=r[:, 37:38], in1=r[:, 36:37], op=ALU.subtract)
                yield
                P.op("scalar", "activation", [rk], [rk], out=r[:, 153:154], in_=r[:, 152:153], func=AF.Exp)
                P.op("vector", "tensor_scalar", [rk], [rk], out=r[:, 154:155], in0=r[:, 153:154], scalar1=1.0, scalar2=None, op0=ALU.add)
                P.op("vector", "reciprocal", [rk], [rk], out=r[:, 154:155], in_=r[:, 154:155])
                P.op("vector", "tensor_tensor", [rk], [rk], out=r[:, 155:156], in0=r[:, 154:155], in1=r[:, 151:152], op=ALU.mult)
                P.op("vector", "tensor_tensor", [rk], [rk], out=r[:, 156:157], in0=r[:, 155:156], in1=r[:, 153:154], op=ALU.mult)
                P.op("vector", "tensor_scalar", [rk], [("comb", t)], out=comb[:, t, :], in0=r[:, 84:116], scalar1=r[:, 155:156],
                     scalar2=None, op0=ALU.mult)
                P.op("vector", "scalar_tensor_tensor", [rk, ("comb", t)], [("comb", t)], out=comb[:, t, :], in0=r[:, 116:148],
                     scalar=r[:, 156:157], in1=comb[:, t, :], op0=ALU.mult, op1=ALU.add)
            for t0 in range(0, NT, 2):
                interleave([mtile(t) for t in (t0, t0 + 1) if t < NT])
            P.flush()

    def stage_moe(l, hT, comb, yacc):
        with ExitStack() as st:
            sb, ps = stage_allocs(st)
            w1b = Ring(sb, "mo_w1", [128, 8, 256], BF16, 3)
            w3b = Ring(sb, "mo_w3", [128, 8, 256], BF16, 3)
            w2b = Ring(sb, "mo_w2", [128, 2, D], BF16, 3)
            hidb = Ring(sb, "mo_hid", [128, 2, LP], BF16, 2)
            silb = Ring(sb, "mo_sil", [128, 512], F32, 3)
            stg = Ring(sb, "mo_stg", [128, 2048], F32, 3)
            p1 = Ring(ps, "mo_p1", [128, 512], F32, 2)
            p3 = Ring(ps, "mo_p3", [128, 512], F32, 2)
            pY = [ps("mo_pY%d" % i, [128, 1024]) for i in range(2)]
            HT = [("hT", t) for t in range(NT)]
            yi = [0]
            wts = {}

            def hphase(e):
                a1, a1k = w1b.next()
                a3, a3k = w3b.next()
                a2, a2k = w2b.next()
                for (dst_, dkey_, src_, c_) in ((a1, a1k, w1[l, e].rearrange("(c p) f -> p c f", p=128), 8),
                                                (a3, a3k, w3[l, e].rearrange("(c p) f -> p c f", p=128), 8),
                                                (a2, a2k, w2[l, e].rearrange("(c p) n -> p c n", p=128), 2)):
                    sg, sgk = stg.next()
                    sv_ = sg[:].rearrange("p (c f) -> p c f", c=c_)
                    P.dma("sync", [], [sgk], out=sv_, in_=src_)
                    P.op("gpsimd", "tensor_copy", [sgk], [dkey_], out=dst_[:], in_=sv_)
                hid, hidk = hidb.next()
                for fc in range(2):
                    for (n0, nn) in NTILES:
                        q1, q1k = p1.next()
                        q3, q3k = p3.next()
                        for k in range(8):
                            P.op("tensor", "matmul", [a1k] + HT, [q1k], q1[:, 0:nn], lhsT=a1[:, k, fc * 128:(fc + 1) * 128],
                                 rhs=hT[:, k, n0:n0 + nn], start=(k == 0), stop=(k == 7))
                        yield
                        for k in range(8):
                            P.op("tensor", "matmul", [a3k] + HT, [q3k], q3[:, 0:nn], lhsT=a3[:, k, fc * 128:(fc + 1) * 128],
                                 rhs=hT[:, k, n0:n0 + nn], start=(k == 0), stop=(k == 7))
                        s, sk = silb.next()
                        P.op("scalar", "activation", [q1k], [sk], out=s[:, 0:nn], in_=q1[:, 0:nn], func=AF.Silu)
                        P.op("vector", "tensor_tensor", [sk, q3k], [hidk], out=hid[:, fc, n0:n0 + nn], in0=s[:, 0:nn], in1=q3[:, 0:nn],
                             op=ALU.mult)
                        yield
                wts[e] = (hid, hidk, a2, a2k)

            def yphase(e):
                hid, hidk, a2, a2k = wts.pop(e)
                for t in range(NT):
                    cs = slice(t * 128, (t + 1) * 128)
                    py, pyk = pY[yi[0] % 2], "mo_pY%d" % (yi[0] % 2)
                    yi[0] += 1
                    for n in range(2):
                        for fc in range(2):
                            P.op("tensor", "matmul", [hidk, a2k], [pyk], py[:, n * 512:(n + 1) * 512], lhsT=hid[:, fc, cs],
                                 rhs=a2[:, fc, n * 512:(n + 1) * 512], start=(fc == 0), stop=(fc == 1))
                    if e == 0:
                        P.op("vector", "tensor_scalar", [pyk, ("comb", t)], [("yacc", t)], out=yacc[:, t, :], in0=py[:],
                             scalar1=comb[:, t, e:e + 1], scalar2=None, op0=ALU.mult)
                    else:
                        P.op("vector", "scalar_tensor_tensor", [pyk, ("comb", t), ("yacc", t)], [("yacc", t)], out=yacc[:, t, :],
                             in0=py[:], scalar=comb[:, t, e:e + 1], in1=yacc[:, t, :], op0=ALU.mult, op1=ALU.add)
                    yield

            interleave([hphase(0)])
            for e in range(NE):
                gens = [yphase(e)]
                if e + 1 < NE:
                    gens.append(hphase(e + 1))
                interleave(gens)
            P.flush()

    def stage_ln2(l, yacc, last):
        with ExitStack() as st:
            sb, ps = stage_allocs(st)
            g_rep = sb("l2_g", [128, D])
            b_rep = sb("l2_b", [128, D])
            P.dma("sync", [], ["lng"], out=g_rep[:], in_=ln2g[l, :, :])
            P.dma("sync", [], ["lnb"], out=b_rep[:], in_=ln2b[l, :, :])
            hin = Ring(sb, "l2_hin", [128, D], F32, 2)
            tb = Ring(sb, "l2_t", [128, D], F32, 2)
            ob = Ring(sb, "l2_o", [128, D], F32, 2)
            scr = sb("l2_scr", [128, D])
            stb = Ring(sb, "l2_st", [128, 4], F32, 2)
            def ltile(t):
                hi_, hik = hin.next()
                P.dma("sync", [("h", t)], [hik], out=hi_[:], in_=h_d[t * 128:(t + 1) * 128, :])
                tt, ttk = tb.next()
                P.op("vector", "scalar_tensor_tensor", [hik, ("yacc", t)], [ttk], out=tt[:], in0=hi_[:], scalar=ALPHA,
                     in1=yacc[:, t, :], op0=ALU.mult, op1=ALU.add)
                o, ok = ob.next()
                s4, s4k = stb.next()
                yield
                yield from ln_tile(tt, ttk, g_rep, b_rep, o, ok, scr, "l2_scr", s4, s4k)
                if not last:
                    P.dma("sync", [ok], [("h", t)], out=h_d[t * 128:(t + 1) * 128, :], in_=o[:])
                else:
                    if t == 0:
                        P.dma("sync", [ok], [("y", t)], out=y[0:112, :], in_=o[16:128, :])
                    elif t < 16:
                        P.dma("sync", [ok], [("y", t)], out=y[t * 128 - 16:t * 128 + 112, :], in_=o[:])
                    else:
                        P.dma("sync", [ok], [("y", t)], out=y[2032:2048, :], in_=o[0:16, :])
            for t0 in range(0, NT, 2):
                interleave([ltile(t) for t in (t0, t0 + 1) if t < NT])
            P.flush()

    stages = []
    res = ExitStack()
    rsb, _ = stage_allocs(res)
    hT = rsb("hT", [128, 8, LP], BF16)
    done = False

    def want(name, l):
        nonlocal done
        if done:
            return False
        if only is not None and (name, l) not in only:
            return False
        if stop_after is not None and stop_after == (name, l):
            done = True
        return True

    for l in range(n_layers):
        src = h0 if l == 0 else h_d
        if want("hT", l):
            stage_hT(src, hT)
        if want("proj", l):
            stage_proj(l, hT)
        if want("dn", l):
            stage_dn(l)
        if want("dsa", l):
            stage_dsa(l)
        moe_st = ExitStack()
        msb, _ = stage_allocs(moe_st)
        comb = msb("comb", [128, NT, NE])
        if want("mix", l):
            stage_mix_ln1(l, src, hT, comb)
        yacc = msb("yacc", [128, NT, D])
        if want("moe", l):
            stage_moe(l, hT, comb, yacc)
        if want("ln2", l):
            stage_ln2(l, yacc, last=(l == n_layers - 1))
        moe_st.close()
    P.finish([("y", t) for t in range(NT)])
    res.close()
    top.close()
    return nc


def _rope_tables():
    inv = 1.0 / (10000.0 ** (np.arange(0, 64, 2, dtype=np.float32) / np.float32(64)))
    pos = np.arange(LP, dtype=np.float32)
    ang = pos[:, None] * inv[None, :].astype(np.float32)
    ang = np.concatenate([ang, ang], -1)
    cos = np.cos(ang).astype(np.float32)
    sin = np.sin(ang).astype(np.float32)
    sgn = np.concatenate([-np.ones(32, np.float32), np.ones(32, np.float32)])
    sins = sin * sgn[None, :]
    cosT = np.ascontiguousarray(np.concatenate([cos.T, cos.T], 0))
    sinT = np.ascontiguousarray(np.concatenate([sins.T, sins.T], 0))
    return cosT, sinT


def make_shared(inp):
    f = lambda a: np.ascontiguousarray(np.asarray(a, dtype=np.float32))
    rep = lambda a: f(np.broadcast_to(np.asarray(a, np.float32)[:, None, :], (DEPTH, 128, np.asarray(a).shape[-1])))
    cosT, sinT = _rope_tables()
    cw = np.asarray(inp["conv_w"], np.float32)
    cwT = f(cw.reshape(DEPTH, 4, 12, 128).transpose(0, 3, 2, 1).reshape(DEPTH, 128, 48))
    sh = {
        "w_in": f(inp["w_in"]), "w_out": f(inp["w_out"]),
        "w1": f(inp["w1"]), "w3": f(inp["w3"]), "w2": f(inp["w2"]),
        "wr": f(np.concatenate([np.asarray(inp["w_grp"], np.float32), np.asarray(inp["w_rtr"], np.float32)], -1)),
        "brep": rep(np.concatenate([np.asarray(inp["b_grp"], np.float32), np.asarray(inp["b_rtr"], np.float32)], -1)),
        "cwT": cwT,
        "alog": rep(np.tile(np.asarray(inp["a_log"], np.float32), (1, NT))),
        "dtb": rep(np.tile(np.asarray(inp["dt_bias"], np.float32), (1, NT))),
        "ngr": rep(np.tile(np.asarray(inp["dn_norm_g"], np.float32), (1, 4))),
        "ln1g": rep(inp["ln1_g"]), "ln1b": rep(inp["ln1_b"]), "ln2g": rep(inp["ln2_g"]), "ln2b": rep(inp["ln2_b"]),
        "ropec": cosT, "ropes": sinT,
    }
    return sh


def make_h0(x_b, meta):
    h0 = np.zeros((LP, D), np.float32)
    h0[:NMETA] = meta
    h0[NMETA:L] = x_b
    return h0


_NC_CACHE = {}


def kernel(**inputs):
    x = np.asarray(inputs["x"], np.float32)
    meta = np.asarray(inputs["meta_tokens"], np.float32)
    sh = make_shared(inputs)
    if "nc" not in _NC_CACHE:
        _NC_CACHE["nc"] = build()
    nc = _NC_CACHE["nc"]
    in_maps = []
    for b in range(8):
        m = dict(sh)
        m["h0"] = make_h0(x[b], meta)
        in_maps.append(m)
    res = run_bass_kernel_spmd(nc, in_maps, core_ids=list(range(8)))
    return np.stack([np.asarray(r["y"], np.float32) for r in res.results], 0)
```
